# Optimizing a Trainium2 kernel written in Bass

```python
import math
import jax, jax.numpy as jnp
from jax import lax
import numpy as np

D_MODEL = 1024
BATCH = 8
SEQ = 4096
DEPTH = 4

SSD_INNER = D_MODEL
SSD_HEAD_DIM = 64
SSD_HEADS = SSD_INNER // SSD_HEAD_DIM
SSD_GROUPS = 4
SSD_STATE = 128
SSD_CHUNK = 128
SSD_CONV = 4
SSD_BC = SSD_GROUPS * SSD_STATE
SSD_CONV_CH = SSD_INNER + 2 * SSD_BC
DT_MIN = 0.001
DT_MAX = 0.1
ATT_HEAD_DIM = 64
ATT_HEADS = D_MODEL // 128
ATT_INNER = ATT_HEADS * ATT_HEAD_DIM
Q_BLOCK = 128
N_GROUPS = 4
EXPERTS_PER_GROUP = 4
N_EXPERTS = N_GROUPS * EXPERTS_PER_GROUP
TOP_K = 2
EXPERT_FF = D_MODEL // 2
IN_SIZES = (SSD_INNER, SSD_CONV_CH, SSD_HEADS, ATT_INNER, ATT_INNER, ATT_INNER, ATT_HEADS, D_MODEL, D_MODEL)
N_IN = sum(IN_SIZES)
N_MOD = 6
DEEPNORM_ALPHA = (2 * DEPTH) ** 0.25
DEEPNORM_BETA = (8 * DEPTH) ** -0.25
LN_EPS = 1e-5

kernel_name = 'hybrid_ssd_fox_hmoe_deepnorm_adaln'


def layer_norm(x, gain=None, bias=None):
    xf = x.astype(jnp.float32)
    mu = jnp.mean(xf, axis=-1, keepdims=True)
    var = jnp.mean(jnp.square(xf - mu), axis=-1, keepdims=True)
    y = ((xf - mu) * lax.rsqrt(var + LN_EPS)).astype(x.dtype)
    if gain is None:
        return y
    return y * gain + bias


def gated_rms_norm(y, z, w):
    g = (y * jax.nn.silu(z)).astype(jnp.float32)
    g = g * lax.rsqrt(jnp.mean(jnp.square(g), axis=-1, keepdims=True) + LN_EPS)
    return g.astype(y.dtype) * w


def causal_depthwise_conv(u, w, bias):
    k_width, ch = w.shape
    out = lax.conv_general_dilated(
        u, w[:, None, :].astype(u.dtype), window_strides=(1,),
        padding=((k_width - 1, 0),), dimension_numbers=('NWC', 'WIO', 'NWC'),
        feature_group_count=ch)
    return out + bias


def ssd_chunked(xh, dt, a_log, bmat, cmat):
    b, s, h, p = xh.shape
    g, n = bmat.shape[-2:]
    r = h // g
    L = SSD_CHUNK
    nc = s // L
    f32 = jnp.float32
    A = -jnp.exp(a_log.astype(f32))
    a = (dt * A).reshape(b, nc, L, g, r)
    xdt = (xh.astype(f32) * dt[..., None]).reshape(b, nc, L, g, r, p)
    bc = bmat.astype(f32).reshape(b, nc, L, g, n)
    cc = cmat.astype(f32).reshape(b, nc, L, g, n)
    a_cs = jnp.cumsum(a, axis=2)
    causal = jnp.tril(jnp.ones((L, L), dtype=bool))[None, None, :, :, None, None]
    seg = a_cs[:, :, :, None] - a_cs[:, :, None, :]
    decay = jnp.exp(jnp.where(causal, seg, -jnp.inf))
    cb = jnp.einsum('bclgn,bcsgn->bclsg', cc, bc)
    y_diag = jnp.einsum('bclsgr,bcsgrp->bclgrp', cb[..., None] * decay, xdt)
    to_end = jnp.exp(a_cs[:, :, -1:] - a_cs)
    states = jnp.einsum('bclgn,bclgrp->bcgrpn', bc, xdt * to_end[..., None])
    chunk_decay = jnp.exp(a_cs[:, :, -1])

    def carry_state(state, inp):
        st, dec = inp
        return state * dec[..., None, None] + st, state

    _, prev = lax.scan(carry_state, jnp.zeros_like(states[:, 0]),
                       (jnp.moveaxis(states, 1, 0), jnp.moveaxis(chunk_decay, 1, 0)))
    prev = jnp.moveaxis(prev, 0, 1)
    y_off = jnp.einsum('bclgn,bcgrpn->bclgrp', cc, prev) * jnp.exp(a_cs)[..., None]
    return (y_diag + y_off).reshape(b, s, h, p)


def forgetting_attention(q, k, v, log_f):
    b, s, h, d = q.shape
    fcum = jnp.cumsum(log_f.astype(jnp.float32), axis=1).transpose(0, 2, 1)
    scale = d ** -0.5
    q_pos = jnp.arange(Q_BLOCK)
    outs = []
    for i in range(s // Q_BLOCK):
        lo, hi = i * Q_BLOCK, (i + 1) * Q_BLOCK
        logits = jnp.einsum('bqhd,bkhd->bhqk', q[:, lo:hi], k[:, :hi]).astype(jnp.float32) * scale
        logits = logits + fcum[:, :, lo:hi, None] - fcum[:, :, None, :hi]
        causal = jnp.arange(hi)[None, :] <= (lo + q_pos)[:, None]
        logits = jnp.where(causal, logits, -jnp.inf)
        probs = jax.nn.softmax(logits, axis=-1).astype(v.dtype)
        outs.append(jnp.einsum('bhqk,bkhd->bqhd', probs, v[:, :hi]))
    return jnp.concatenate(outs, axis=1)


def hybrid_mixer(h, w_in, conv_w, conv_b, dt_bias, a_log, d_skip, ssd_norm_w, forget_b,
                 w_ssd_o, w_att_o, w_o):
    b, s, _ = h.shape
    split_points = np.cumsum(IN_SIZES)[:-1].tolist()
    proj = h @ w_in
    z, xbc, dt_raw, q, k, v, f_raw, g_ssd, g_att = jnp.split(proj, split_points, axis=-1)
    xbc = jax.nn.silu(causal_depthwise_conv(xbc, conv_w, conv_b))
    xs, bm, cm = jnp.split(xbc, [SSD_INNER, SSD_INNER + SSD_BC], axis=-1)
    dt = jax.nn.softplus((dt_raw + dt_bias).astype(jnp.float32))
    xh = xs.reshape(b, s, SSD_HEADS, SSD_HEAD_DIM)
    y = ssd_chunked(xh, dt, a_log,
                    bm.reshape(b, s, SSD_GROUPS, SSD_STATE),
                    cm.reshape(b, s, SSD_GROUPS, SSD_STATE))
    y = (y + d_skip[:, None] * xh).astype(h.dtype).reshape(b, s, SSD_INNER)
    y_ssd = gated_rms_norm(y, z, ssd_norm_w) @ w_ssd_o
    log_f = jax.nn.log_sigmoid((f_raw + forget_b).astype(jnp.float32))
    o = forgetting_attention(q.reshape(b, s, ATT_HEADS, ATT_HEAD_DIM),
                             k.reshape(b, s, ATT_HEADS, ATT_HEAD_DIM),
                             v.reshape(b, s, ATT_HEADS, ATT_HEAD_DIM), log_f)
    y_att = o.reshape(b, s, ATT_INNER) @ w_att_o
    merged = jax.nn.sigmoid(g_ssd) * y_ssd + jax.nn.sigmoid(g_att) * y_att
    return merged @ w_o


def hierarchical_moe(h, w_rg, b_rg, w_re, b_re, w_gate, w_up, w_down):
    b, s, d = h.shape
    t = h.reshape(-1, d)
    f32 = jnp.float32
    g_prob = jax.nn.softmax((t @ w_rg + b_rg).astype(f32), axis=-1)
    g_p, g_idx = lax.top_k(g_prob, 1)
    onehot_g = jax.nn.one_hot(g_idx[:, 0], N_GROUPS, dtype=f32)
    e_logits = (t @ w_re + b_re).astype(f32).reshape(-1, N_GROUPS, EXPERTS_PER_GROUP)
    e_in = jnp.einsum('tg,tge->te', onehot_g, e_logits)
    top_l, top_i = lax.top_k(e_in, TOP_K)
    w_k = jax.nn.softmax(top_l, axis=-1) * g_p
    w_group = jnp.einsum('tk,tke->te', w_k, jax.nn.one_hot(top_i, EXPERTS_PER_GROUP, dtype=f32))
    gates = (onehot_g[:, :, None] * w_group[:, None, :]).reshape(-1, N_EXPERTS).astype(t.dtype)
    out = jnp.zeros_like(t)
    for e in range(N_EXPERTS):
        hid = jax.nn.silu(t @ w_gate[e]) * (t @ w_up[e])
        out = out + gates[:, e:e + 1] * (hid @ w_down[e])
    return out.reshape(b, s, d)


def setup_inputs(seed: int = 0) -> dict:
    key = jax.random.key(seed)
    ks = iter(jax.random.split(key, 32))
    f32 = jnp.float32

    def nrm(shape, scale):
        return jax.random.normal(next(ks), shape, f32) * scale

    offs = np.concatenate([[0], np.cumsum(IN_SIZES)])
    col_scale = np.ones((N_IN,), np.float32)
    col_scale[offs[1]:offs[1] + SSD_INNER] = DEEPNORM_BETA
    col_scale[offs[5]:offs[6]] = DEEPNORM_BETA
    dt0 = jnp.exp(jax.random.uniform(next(ks), (DEPTH, SSD_HEADS), f32)
                  * (math.log(DT_MAX) - math.log(DT_MIN)) + math.log(DT_MIN))
    return {
        'x': nrm((BATCH, SEQ, D_MODEL), 1.0),
        'c': nrm((BATCH, D_MODEL), 1.0),
        'w_mod': nrm((DEPTH, D_MODEL, N_MOD * D_MODEL), 0.5 * D_MODEL ** -0.5),
        'b_mod': nrm((DEPTH, N_MOD * D_MODEL), 0.02),
        'w_in': nrm((DEPTH, D_MODEL, N_IN), D_MODEL ** -0.5) * jnp.asarray(col_scale),
        'conv_w': nrm((DEPTH, SSD_CONV, SSD_CONV_CH), SSD_CONV ** -0.5),
        'conv_b': nrm((DEPTH, SSD_CONV_CH), 0.02),
        'dt_bias': dt0 + jnp.log(-jnp.expm1(-dt0)),
        'a_log': jnp.log(jax.random.uniform(next(ks), (DEPTH, SSD_HEADS), f32, minval=1.0, maxval=16.0)),
        'd_skip': 1.0 + nrm((DEPTH, SSD_HEADS), 0.1),
        'ssd_norm_w': 1.0 + nrm((DEPTH, SSD_INNER), 0.05),
        'forget_b': jax.random.uniform(next(ks), (DEPTH, ATT_HEADS), f32, minval=1.0, maxval=4.0),
        'w_ssd_o': nrm((DEPTH, SSD_INNER, D_MODEL), DEEPNORM_BETA * SSD_INNER ** -0.5),
        'w_att_o': nrm((DEPTH, ATT_INNER, D_MODEL), DEEPNORM_BETA * ATT_INNER ** -0.5),
        'w_o': nrm((DEPTH, D_MODEL, D_MODEL), DEEPNORM_BETA * D_MODEL ** -0.5),
        'ln1_g': 1.0 + nrm((DEPTH, D_MODEL), 0.05),
        'ln1_b': nrm((DEPTH, D_MODEL), 0.02),
        'w_router_group': nrm((DEPTH, D_MODEL, N_GROUPS), D_MODEL ** -0.5),
        'b_router_group': nrm((DEPTH, N_GROUPS), 0.01),
        'w_router_expert': nrm((DEPTH, D_MODEL, N_EXPERTS), D_MODEL ** -0.5),
        'b_router_expert': nrm((DEPTH, N_EXPERTS), 0.01),
        'w_gate': nrm((DEPTH, N_EXPERTS, D_MODEL, EXPERT_FF), D_MODEL ** -0.5),
        'w_up': nrm((DEPTH, N_EXPERTS, D_MODEL, EXPERT_FF), DEEPNORM_BETA * D_MODEL ** -0.5),
        'w_down': nrm((DEPTH, N_EXPERTS, EXPERT_FF, D_MODEL), DEEPNORM_BETA * EXPERT_FF ** -0.5),
        'ln2_g': 1.0 + nrm((DEPTH, D_MODEL), 0.05),
        'ln2_b': nrm((DEPTH, D_MODEL), 0.02),
    }


def reference(x, c, w_mod, b_mod, w_in, conv_w, conv_b, dt_bias, a_log, d_skip, ssd_norm_w,
              forget_b, w_ssd_o, w_att_o, w_o, ln1_g, ln1_b, w_router_group, b_router_group,
              w_router_expert, b_router_expert, w_gate, w_up, w_down, ln2_g, ln2_b):
    c_act = jax.nn.silu(c)
    for l in range(DEPTH):
        mod = (c_act @ w_mod[l] + b_mod[l])[:, None, :]
        sh1, sc1, gt1, sh2, sc2, gt2 = jnp.split(mod, N_MOD, axis=-1)
        h = layer_norm(x) * (1.0 + sc1) + sh1
        y = hybrid_mixer(h, w_in[l], conv_w[l], conv_b[l], dt_bias[l], a_log[l], d_skip[l],
                         ssd_norm_w[l], forget_b[l], w_ssd_o[l], w_att_o[l], w_o[l])
        x = layer_norm(DEEPNORM_ALPHA * x + (1.0 + gt1) * y, ln1_g[l], ln1_b[l])
        h = layer_norm(x) * (1.0 + sc2) + sh2
        y = hierarchical_moe(h, w_router_group[l], b_router_group[l], w_router_expert[l],
                             b_router_expert[l], w_gate[l], w_up[l], w_down[l])
        x = layer_norm(DEEPNORM_ALPHA * x + (1.0 + gt2) * y, ln2_g[l], ln2_b[l])
    return x
```

```python
import numpy as np
from contextlib import contextmanager, ExitStack
import concourse.bass as bass
import concourse.mybir as mybir
from concourse.bass_utils import run_bass_kernel_spmd

F32 = mybir.dt.float32
BF16 = mybir.dt.bfloat16
AF = mybir.ActivationFunctionType
ALU = mybir.AluOpType
AX = mybir.AxisListType

D = 1024
NIN = 6680
ALPHA = 8 ** 0.25
EPS = 1e-5
OZ, OXBC, ODT, OQ, OK_, OV, OF, OGS, OGA = 0, 1024, 3072, 3088, 3600, 4112, 4624, 4632, 5656


class Buf:
    def __init__(self, t, name):
        self.t = t
        self.name = name
        self.w = {}
        self.r = {}
        self.dsem = None

    def __getitem__(self, key):
        return self.t[key]


class FW:
    ENGS = ('pe', 'act', 'dve', 'pool', 'sp')

    def __init__(self, nc, n_dma_sems=48):
        self.nc = nc
        self.e = {'pe': nc.tensor, 'act': nc.scalar, 'dve': nc.vector, 'pool': nc.gpsimd, 'sp': nc.sync}
        self.esem = {k: nc.alloc_semaphore("es_" + k) for k in self.ENGS}
        self.ecount = {k: 0 for k in self.ENGS}
        self.known = {k: {} for k in self.ENGS}
        self.pending = {k: [] for k in self.ENGS}
        self.dsems = [nc.alloc_semaphore("ds_%d" % i) for i in range(n_dma_sems)]
        self.dtotal = {s: 0 for s in self.dsems}
        self.free_dsems = list(self.dsems)
        self.stack = None
        self.phase_bufs = []
        self.bar = nc.alloc_semaphore("bar")
        self.barcount = 0
        self.nbuf = 0
        self.ninstr = 0

    def sb(self, shape, dtype, name=None):
        self.nbuf += 1
        name = (name or "t") + "_%d" % self.nbuf
        if self.stack is not None:
            t = self.stack.enter_context(self.nc.sbuf_tensor(name, list(shape), dtype))
        else:
            t = self.nc.alloc_sbuf_tensor(name, list(shape), dtype)
        b = Buf(t, name)
        if self.stack is not None:
            self.phase_bufs.append(b)
        return b

    def ring(self, n, shape, dtype, name=None):
        return [self.sb(shape, dtype, name) for _ in range(n)]

    def token(self, name):
        b = Buf(None, name)
        if self.stack is not None:
            self.phase_bufs.append(b)
        return b

    def _dsem(self, b):
        if b.dsem is None:
            b.dsem = self.free_dsems.pop()
        return b.dsem

    @contextmanager
    def phase(self, name=""):
        assert self.stack is None
        self.stack = ExitStack()
        self.phase_bufs = []
        try:
            yield
            self.barrier()
        finally:
            for b in self.phase_bufs:
                if b.dsem is not None:
                    self.free_dsems.append(b.dsem)
                    b.dsem = None
            self.stack.close()
            self.stack = None
            self.phase_bufs = []

    def _waits(self, eng, reads, writes):
        waits = {}
        for b in reads:
            for s, v in b.w.items():
                if waits.get(s, 0) < v:
                    waits[s] = v
        for b in writes:
            for d in (b.w, b.r):
                for s, v in d.items():
                    if waits.get(s, 0) < v:
                        waits[s] = v
        e = self.e[eng]
        kn = self.known[eng]
        for s, v in waits.items():
            if eng == 'pe' and s is self.esem['pe']:
                continue
            if kn.get(s, 0) < v:
                e.wait_ge(s, v)
                self.ninstr += 1
                kn[s] = v

    def _record(self, ev, reads, writes):
        s, v = ev
        for b in writes:
            b.w = {s: v}
            b.r = {}
        for b in reads:
            if b.r.get(s, 0) < v:
                b.r[s] = v

    def op(self, eng, fn, reads=(), writes=(), inc=True):
        self._waits(eng, reads, writes)
        ins = fn(self.e[eng])
        self.ninstr += 1
        if not inc:
            self.pending[eng].append((tuple(reads), tuple(writes)))
            return ins
        self.ecount[eng] += 1
        ins.then_inc(self.esem[eng], 1)
        ev = (self.esem[eng], self.ecount[eng])
        for (r, w) in self.pending[eng]:
            self._record(ev, r, w)
        self.pending[eng] = []
        self._record(ev, reads, writes)
        return ins

    def dma(self, q, fn, reads=(), writes=(), sem_buf=None):
        self._waits(q, reads, writes)
        sbuf = sem_buf or (writes[0] if writes else reads[0])
        sem = self._dsem(sbuf)
        inss = fn(self.e[q])
        if not isinstance(inss, (list, tuple)):
            inss = [inss]
        for ins in inss:
            ins.then_inc(sem, 16)
            self.dtotal[sem] += 16
            self.ninstr += 1
        ev = (sem, self.dtotal[sem])
        self._record(ev, reads, writes)

    def barrier(self):
        sp = self.e['sp']
        kn = self.known['sp']
        for k in self.ENGS:
            assert not self.pending[k], "pending non-inc instrs at barrier on " + k
            s, v = self.esem[k], self.ecount[k]
            if k != 'sp' and kn.get(s, 0) < v:
                sp.wait_ge(s, v)
                kn[s] = v
        for s, v in self.dtotal.items():
            if kn.get(s, 0) < v:
                sp.wait_ge(s, v)
                kn[s] = v
        self.barcount += 1
        sp.sem_inc(self.bar, 1)
        for k in self.ENGS:
            if k == 'sp':
                continue
            self.e[k].wait_ge(self.bar, self.barcount)
            self.known[k] = dict(kn)
            self.known[k][self.esem[k]] = self.ecount[k]


def build_program(S=4096, L=4, debug=False, stop_after=None):
    nc = bass.Bass("TRN2", target_bir_lowering=False)
    NT = S // 128
    NB = S // 512
    fw = FW(nc)

    def inp(name, shape, dt=F32):
        return nc.dram_tensor(name, list(shape), dt, kind="ExternalInput").ap()

    dbg_kind = "ExternalOutput" if debug else "Internal"

    def scr(name, shape, dt, dbg=True):
        return nc.dram_tensor(name, list(shape), dt, kind=(dbg_kind if dbg else "Internal")).ap()

    x_in = inp("x", [S, D])
    cT = inp("cT", [128, 8])
    consts = inp("consts", [128, 512])
    w_mod = inp("w_mod", [L, D, 6 * D])
    b_mod = inp("b_mod", [L, 6 * D])
    w_in = inp("w_in", [L, D, NIN])
    w_small = inp("w_small", [L, D, 24])
    conv_wT = inp("conv_wT", [L, 128, 16, 4])
    conv_bT = inp("conv_bT", [L, 128, 16])
    dt_bias = inp("dt_bias", [L, 16])
    a_log = inp("a_log", [L, 16])
    d_skip = inp("d_skip", [L, 16])
    ssd_norm_wT = inp("ssd_norm_wT", [L, 128, 8])
    forget_b = inp("forget_b", [L, 8])
    w_ssd_o = inp("w_ssd_o", [L, D, D])
    w_att_o = inp("w_att_o", [L, 512, D])
    w_o = inp("w_o", [L, D, D])
    ln1_g = inp("ln1_g", [L, D])
    ln1_b = inp("ln1_b", [L, D])
    w_router = inp("w_router", [L, D, 20])
    b_router = inp("b_router", [L, 20])
    w_gate = inp("w_gate", [L, 16, D, 512])
    w_up = inp("w_up", [L, 16, D, 512])
    w_down = inp("w_down", [L, 16, 512, D])
    ln2_g = inp("ln2_g", [L, D])
    ln2_b = inp("ln2_b", [L, D])
    out = nc.dram_tensor("out", [S, D], F32, kind="ExternalOutput").ap()

    mod_row = scr("mod_row", [L, 6 * D], F32)
    wb_in = scr("wb_in", [2, D, NIN], BF16, dbg=False)
    wb_so = scr("wb_so", [2, D, D], BF16, dbg=False)
    wb_ao = scr("wb_ao", [2, 512, D], BF16, dbg=False)
    wb_o = scr("wb_o", [2, D, D], BF16, dbg=False)
    wb_r = scr("wb_r", [2, D, 20], BF16, dbg=False)
    wb_g = scr("wb_g", [2, 16, D, 512], BF16, dbg=False)
    wb_u = scr("wb_u", [2, 16, D, 512], BF16, dbg=False)
    wb_d = scr("wb_d", [2, 16, 512, D], BF16, dbg=False)
    hT_d = scr("hT_d", [8, 128, S], BF16)
    zs_d = scr("zs_d", [S, D], BF16)
    xs_d = scr("xs_d", [S, D], BF16)
    Btm_d = scr("Btm_d", [S, 512], BF16)
    BT_d = scr("BT_d", [4, 128, S], BF16)
    CT_d = scr("CT_d", [4, 128, S], BF16)
    small_d = scr("small_d", [S, 24], F32)
    qT_d = scr("qT_d", [8, 66, S], BF16)
    kT_d = scr("kT_d", [8, 66, S], BF16)
    va_d = scr("va_d", [S, 8, 128], BF16)
    sgT_d = scr("sgT_d", [16, 128, S], BF16)
    gnT_d = scr("gnT_d", [8, 128, S], BF16)
    oT_d = scr("oT_d", [4, 128, S], BF16)
    y_d = scr("y_d", [S, D], F32)
    xres_d = scr("xres_d", [S, D], F32)
    fbias_d = scr("fbias_d", [8, 128, NB * NT], F32)

    psall = nc.alloc_psum_tensor("psall", [128, 4096], F32)
    psall_bf = psall.bitcast(BF16)
    ps = [Buf(psall[:, i * 512:(i + 1) * 512], "ps%d" % i) for i in range(8)]

    def psbf(i):
        return psall_bf[:, i * 1024:(i + 1) * 1024]

    cst = fw.sb([128, 512], F32, "cst")
    identb = fw.sb([128, 128], BF16, "identb")
    ident = cst[:, 0:128]
    Uinc = cst[:, 128:256]
    Lstr = cst[:, 256:384]
    ones = cst[:, 384:512]

    def TT(eng, o, a, b, op, R, W):
        fw.op(eng, lambda e: e.tensor_tensor(out=o, in0=a, in1=b, op=op), reads=R, writes=W)

    def TS(eng, o, a, s1, s2, op0, op1, R, W):
        if s2 is None:
            fw.op(eng, lambda e: e.tensor_scalar(out=o, in0=a, scalar1=s1, scalar2=None, op0=op0), reads=R, writes=W)
        else:
            fw.op(eng, lambda e: e.tensor_scalar(out=o, in0=a, scalar1=s1, scalar2=s2, op0=op0, op1=op1), reads=R, writes=W)

    def STT(eng, o, a, s, b, op0, op1, R, W):
        fw.op(eng, lambda e: e.scalar_tensor_tensor(out=o, in0=a, scalar=s, in1=b, op0=op0, op1=op1), reads=R, writes=W)

    def ACT(o, a, func, R, W, bias=None, scale=None, accum=None):
        kw = {}
        if bias is not None:
            kw['bias'] = bias
        if scale is not None:
            kw['scale'] = scale
        if accum is not None:
            kw['accum_out'] = accum
        fw.op('act', lambda e: e.activation(out=o, in_=a, func=func, **kw), reads=R, writes=W)

    def CP(eng, o, a, R, W):
        if eng == 'act':
            ACT(o, a, AF.Copy, R, W)
        else:
            fw.op(eng, lambda e: e.tensor_copy(out=o, in_=a), reads=R, writes=W)

    def MM(o, lhsT, rhs, start, stop, R, W, inc=True):
        fw.op('pe', lambda e: e.matmul(o, lhsT=lhsT, rhs=rhs, start=start, stop=stop), reads=R, writes=W, inc=inc)

    def TR(o, a, R, W, inc=True):
        fw.op('pe', lambda e: e.transpose(o, a, identb[:, :]), reads=list(R) + [identb], writes=W, inc=inc)

    def LD(q, o, a, W, R=(), **kw):
        fw.dma(q, lambda e: e.dma_start(out=o, in_=a, **kw), reads=R, writes=W)

    def ST(q, o, a, R, **kw):
        fw.dma(q, lambda e: e.dma_start(out=o, in_=a, **kw), reads=R)

    def MEMSET(eng, o, val, W):
        fw.op(eng, lambda e: e.memset(o, val), writes=W)

    NC_OK = dict(allow_slow_non_contiguous=True)

    LD('sp', cst[:, :], consts, [cst])
    CP('dve', identb[:, :], ident, [cst], [identb])

    wtok = [fw.token("wtok%d" % l) for l in range(L)]

    def cast_weights(l):
        s = l % 2
        tk = wtok[l]

        def go(e):
            r = []
            for k in range(8):
                r.append(e.dma_start(out=wb_in[s, k * 128:(k + 1) * 128, :], in_=w_in[l, k * 128:(k + 1) * 128, :]))
                r.append(e.dma_start(out=wb_so[s, k * 128:(k + 1) * 128, :], in_=w_ssd_o[l, k * 128:(k + 1) * 128, :]))
                r.append(e.dma_start(out=wb_o[s, k * 128:(k + 1) * 128, :], in_=w_o[l, k * 128:(k + 1) * 128, :]))
            for k in range(4):
                r.append(e.dma_start(out=wb_ao[s, k * 128:(k + 1) * 128, :], in_=w_att_o[l, k * 128:(k + 1) * 128, :]))
            r.append(e.dma_start(out=wb_r[s], in_=w_router[l]))
            for ex in range(16):
                for k in range(4):
                    r.append(e.dma_start(out=wb_g[s, ex, k * 256:(k + 1) * 256, :], in_=w_gate[l, ex, k * 256:(k + 1) * 256, :]))
                    r.append(e.dma_start(out=wb_u[s, ex, k * 256:(k + 1) * 256, :], in_=w_up[l, ex, k * 256:(k + 1) * 256, :]))
                    r.append(e.dma_start(out=wb_d[s, ex, k * 128:(k + 1) * 128, :], in_=w_down[l, ex, k * 128:(k + 1) * 128, :]))
            return r
        fw.dma('pool', go, writes=[tk])

    cast_weights(0)
    with fw.phase("mod"):
        ct = fw.sb([128, 8], F32, "ct")
        cact = fw.sb([128, 8], F32, "cact")
        LD('sp', ct[:, :], cT, [ct])
        ACT(cact[:, :], ct[:, :], AF.Silu, [ct], [cact])
        brow = fw.sb([1, 6 * D], F32, "brow")
        row = fw.sb([1, 6 * D], F32, "row")
        wring = fw.ring(3, [128, 8, 512], F32, "wmod")
        i = 0
        for l in range(L):
            LD('sp', brow[0:1, :], b_mod[l:l + 1, :], [brow])
            for nb in range(12):
                wbuf = wring[i % 3]
                P = ps[i % 2]
                i += 1
                LD('sp', wbuf[:, :, :], w_mod[l].rearrange("(kc p) n -> p kc n", p=128)[:, :, nb * 512:(nb + 1) * 512], [wbuf])
                for kc in range(8):
                    MM(P[0:1, :], cact[:, kc:kc + 1], wbuf[:, kc, :], kc == 0, kc == 7, [cact, wbuf], [P], inc=(kc == 7))
                TT('dve', row[0:1, nb * 512:(nb + 1) * 512], P[0:1, :], brow[0:1, nb * 512:(nb + 1) * 512], ALU.add, [P, brow], [row])
            ST('sp', mod_row[l:l + 1, :], row[0:1, :], [row])

    def ln_phase(l, x_src, y_src, x_dst, gt_idx, g_ap, b_ap, sc_idx, sh_idx, lp=None):
        lp = l if lp is None else lp
        with fw.phase("ln"):
            post = y_src is not None
            pre = sc_idx is not None
            if post:
                gtb = fw.sb([128, D], F32, "gtb")
                gb = fw.sb([128, D], F32, "gb")
                bb = fw.sb([128, D], F32, "bb")
                LD('sp', gtb[:, :], mod_row[l, gt_idx * D:(gt_idx + 1) * D].partition_broadcast(128), [gtb])
                LD('sp', gb[:, :], g_ap.partition_broadcast(128), [gb])
                LD('sp', bb[:, :], b_ap.partition_broadcast(128), [bb])
                TS('dve', gtb[:, :], gtb[:, :], 1.0, None, ALU.add, None, [gtb], [gtb])
            if pre:
                scp = fw.sb([128, 8], F32, "scp")
                shp = fw.sb([128, 8], F32, "shp")
                LD('sp', scp[:, :], mod_row[lp, sc_idx * D:(sc_idx + 1) * D].rearrange("(c p) -> p c", p=128), [scp], **NC_OK)
                LD('sp', shp[:, :], mod_row[lp, sh_idx * D:(sh_idx + 1) * D].rearrange("(c p) -> p c", p=128), [shp], **NC_OK)
                TS('dve', scp[:, :], scp[:, :], 1.0, None, ALU.add, None, [scp], [scp])
            xr = fw.ring(3, [128, D], F32, "xt")
            yr = fw.ring(3, [128, D], F32, "yt") if post else None
            ur = fw.ring(2, [128, D], F32, "u") if post else None
            xnr = fw.ring(2, [128, D], F32, "xnew") if post else None
            smr = fw.ring(2, [128, 32], F32, "sm")
            xbr = fw.ring(2, [128, D], BF16, "xnb") if pre else None
            hr = fw.ring(2, [128, 8, 128], BF16, "hTt") if pre else None
            hr32 = fw.ring(2, [128, 8, 128], F32, "hT32") if pre else None

            def loads(t):
                LD('sp', xr[t % 3][:, :], x_src[t * 128:(t + 1) * 128, :], [xr[t % 3]])
                if post:
                    LD('sp', yr[t % 3][:, :], y_src[t * 128:(t + 1) * 128, :], [yr[t % 3]])

            def stats(src, srcb, sm, o):
                for hh in range(2):
                    fw.op('dve', lambda e: e.bn_stats(out=sm[:, o + 6 * hh:o + 6 * hh + 6], in_=src[:, hh * 512:(hh + 1) * 512]), reads=[srcb], writes=[sm])
                fw.op('dve', lambda e: e.bn_aggr(out=sm[:, o + 12:o + 14], in_=sm[:, o:o + 12]), reads=[sm], writes=[sm])
                ACT(sm[:, o + 14:o + 15], sm[:, o + 13:o + 14], AF.Ln, [sm], [sm], bias=EPS)
                ACT(sm[:, o + 14:o + 15], sm[:, o + 14:o + 15], AF.Exp, [sm], [sm], scale=-0.5)
                STT('dve', sm[:, o + 15:o + 16], sm[:, o + 12:o + 13], -1.0, sm[:, o + 14:o + 15], ALU.mult, ALU.mult, [sm], [sm])

            loads(0)
            if NT > 1:
                loads(1)
            for t in range(NT):
                if t + 2 < NT:
                    loads(t + 2)
                xt = xr[t % 3]
                sm = smr[t % 2]
                if post:
                    yt = yr[t % 3]
                    u = ur[t % 2]
                    xn = xnr[t % 2]
                    TT('pool', u[:, :], yt[:, :], gtb[:, :], ALU.mult, [yt, gtb], [u])
                    STT('dve', u[:, :], xt[:, :], ALPHA, u[:, :], ALU.mult, ALU.add, [xt, u], [u])
                    stats(u[:, :], u, sm, 0)
                    ACT(xn[:, :], u[:, :], AF.Identity, [u, sm], [xn], bias=sm[:, 15:16], scale=sm[:, 14:15])
                    TT('pool', xn[:, :], xn[:, :], gb[:, :], ALU.mult, [xn, gb], [xn])
                    TT('pool', xn[:, :], xn[:, :], bb[:, :], ALU.add, [xn, bb], [xn])
                    ST('sp', x_dst[t * 128:(t + 1) * 128, :], xn[:, :], [xn])
                    cur = xn
                else:
                    cur = xt
                if pre:
                    xb = xbr[t % 2]
                    ht = hr[t % 2]
                    h32 = hr32[t % 2]
                    stats(cur[:, :], cur, sm, 16)
                    ACT(xb[:, :], cur[:, :], AF.Identity, [cur, sm], [xb], bias=sm[:, 31:32], scale=sm[:, 30:31])
                    P = ps[t % 2]
                    for c in range(8):
                        TR(psbf(t % 2)[:, c * 128:(c + 1) * 128], xb[:, c * 128:(c + 1) * 128], [xb], [P], inc=(c == 7))
                    pv = psbf(t % 2)[:, :].rearrange("p (c s) -> p c s", c=8)
                    TT('dve', h32[:, :, :], pv, scp[:, :].unsqueeze(2).to_broadcast([128, 8, 128]), ALU.mult, [P, scp], [h32])
                    TT('pool', ht[:, :, :], h32[:, :, :], shp[:, :].unsqueeze(2).to_broadcast([128, 8, 128]), ALU.add, [h32, shp], [ht])
                    ST('sp', hT_d.rearrange("c p s -> p c s")[:, :, t * 128:(t + 1) * 128], ht[:, :, :], [ht])

    def win_phase(l):
        s = l % 2
        wsrc = wb_in[s].rearrange("(kc p) n -> p kc n", p=128)
        with fw.phase("win"):
            hT = fw.sb([128, 8, S], BF16, "hT")
            fw.dma('sp', lambda e: [e.dma_start(out=hT[:, c, :], in_=hT_d[c]) for c in range(8)], writes=[hT])
            wA = fw.sb([128, 8, 1560], BF16, "wA")
            LD('sp', wA[:, :, 0:1024], wsrc[:, :, OZ:OZ + 1024], [wA], R=[wtok[l]])
            LD('sp', wA[:, :, 1024:1536], wsrc[:, :, OV:OV + 512], [wA])
            LD('pool', wA[:, :, 1536:1560], w_small[l].rearrange("(kc p) n -> p kc n", p=128), [wA])
            cw = fw.sb([128, 16, 4], F32, "cw")
            cb = fw.sb([128, 16], F32, "cb")
            LD('sp', cw[:, :, :], conv_wT[l], [cw])
            LD('sp', cb[:, :], conv_bT[l], [cb])
            zr = fw.ring(2, [128, D], BF16, "zt")
            vr = fw.ring(2, [128, 8, 128], BF16, "vt")
            sr = fw.ring(2, [128, 24], F32, "smt")
            for v in vr:
                MEMSET('pool', v[:, :, :], 1.0, [v])
            for t in range(NT):
                zt, vt, st = zr[t % 2], vr[t % 2], sr[t % 2]
                tok = slice(t * 128, (t + 1) * 128)
                for cbk in range(2):
                    P = ps[cbk]
                    for kc in range(8):
                        MM(P[:, :], hT[:, kc, tok], wA[:, kc, cbk * 512:(cbk + 1) * 512], kc == 0, kc == 7, [hT, wA], [P], inc=(kc == 7))
                    ACT(zt[:, cbk * 512:(cbk + 1) * 512], P[:, :], AF.Silu, [P], [zt])
                ST('sp', zs_d[tok, :], zt[:, :], [zt])
                P = ps[2]
                for kc in range(8):
                    MM(P[:, :], hT[:, kc, tok], wA[:, kc, 1024:1536], kc == 0, kc == 7, [hT, wA], [P], inc=(kc == 7))
                CP('dve', vt[:, :, 0:64], P[:, :].rearrange("p (h d) -> p h d", h=8), [P], [vt])
                ST('sp', va_d[tok, :, :], vt[:, :, :], [vt])
                P = ps[3]
                for kc in range(8):
                    MM(P[:, 0:24], hT[:, kc, tok], wA[:, kc, 1536:1560], kc == 0, kc == 7, [hT, wA], [P], inc=(kc == 7))
                CP('dve', st[:, :], P[:, 0:24], [P], [st])
                ST('sp', small_d[tok, :], st[:, :], [st])
            chunks = [('xbc', m, OXBC + 128 * m) for m in range(16)]
            chunks += [('q', j, OQ + 128 * j) for j in range(4)] + [('k', j, OK_ + 128 * j) for j in range(4)]
            chunks += [('g', j, OGS + 128 * j) for j in range(16)]
            wBr = fw.ring(3, [128, 8, 128], BF16, "wB")
            rawr = fw.ring(2, [128, 520], F32, "raw")
            accr = fw.ring(2, [128, 512], F32, "acc")
            otr = fw.ring(3, [128, 512], BF16, "ot")
            xtr = fw.ring(2, [128, 4, 128], BF16, "xtt")
            k8 = fw.sb([128, 512], BF16, "k8")
            MEMSET('pool', k8[:, :], 1.0, [k8])
            for h in range(8):
                for nb in range(NB):
                    ST('sp', kT_d[h, 64:66, nb * 512:(nb + 1) * 512], k8[0:2, :], [k8])
            it = 0

            def ldw(ci):
                LD('sp', wBr[ci % 3][:, :, :], wsrc[:, :, chunks[ci][2]:chunks[ci][2] + 128], [wBr[ci % 3]])
            ldw(0)
            ldw(1)
            for ci, (kind, m, col) in enumerate(chunks):
                if ci + 2 < len(chunks):
                    ldw(ci + 2)
                wB = wBr[ci % 3]
                for nb in range(NB):
                    cols = slice(nb * 512, (nb + 1) * 512)
                    P = ps[4 + it % 2]
                    ot = otr[it % 3]
                    for kc in range(8):
                        MM(P[:, :], wB[:, kc, :], hT[:, kc, cols], kc == 0, kc == 7, [wB, hT], [P], inc=(kc == 7))
                    if kind == 'xbc':
                        raw, acc = rawr[it % 2], accr[it % 2]
                        praw = rawr[(it + 1) % 2]
                        CP('act', raw[:, 3:515], P[:, :], [P], [raw])
                        if nb == 0:
                            MEMSET('pool', raw[:, 0:3], 0.0, [raw])
                        else:
                            CP('pool', raw[:, 0:3], praw[:, 512:515], [praw], [raw])
                        ACT(acc[:, :], P[:, :], AF.Identity, [P, cw, cb], [acc], bias=cb[:, m:m + 1], scale=cw[:, m, 3:4])
                        STT('dve', acc[:, :], raw[:, 2:514], cw[:, m, 2:3], acc[:, :], ALU.mult, ALU.add, [raw, cw, acc], [acc])
                        STT('dve', acc[:, :], raw[:, 1:513], cw[:, m, 1:2], acc[:, :], ALU.mult, ALU.add, [raw, cw, acc], [acc])
                        STT('dve', acc[:, :], raw[:, 0:512], cw[:, m, 0:1], acc[:, :], ALU.mult, ALU.add, [raw, cw, acc], [acc])
                        ACT(ot[:, :], acc[:, :], AF.Silu, [acc], [ot])
                        if m >= 8:
                            g = (m - 8) % 4
                            dst = BT_d if m < 12 else CT_d
                            ST('sp', dst[g, :, cols], ot[:, :], [ot])
                        if m < 12:
                            xt = xtr[it % 2]
                            PT = ps[6 + it % 2]
                            for j in range(4):
                                TR(psbf(6 + it % 2)[:, j * 128:(j + 1) * 128], ot[:, j * 128:(j + 1) * 128], [ot], [PT], inc=(j == 3))
                            CP('dve', xt[:, :, :], psbf(6 + it % 2)[:, 0:512].rearrange("p (j c) -> p j c", j=4), [PT], [xt])
                            if m < 8:
                                dd = xs_d[cols, m * 128:(m + 1) * 128]
                            else:
                                dd = Btm_d[cols, (m - 8) * 128:(m - 7) * 128]
                            ST('sp', dd.rearrange("(j p) c -> p j c", p=128), xt[:, :, :], [xt])
                    elif kind in ('q', 'k'):
                        CP('dve', ot[:, :], P[:, :], [P], [ot])
                        dst = qT_d if kind == 'q' else kT_d
                        ST('sp', dst[2 * m, 0:64, cols], ot[0:64, :], [ot])
                        ST('sp', dst[2 * m + 1, 0:64, cols], ot[64:128, :], [ot])
                    else:
                        ACT(ot[:, :], P[:, :], AF.Sigmoid, [P], [ot])
                        ST('sp', sgT_d[m, :, cols], ot[:, :], [ot])
                    it += 1

    def attprep_phase(l):
        with fw.phase("attprep"):
            fr = fw.sb([128, NT, 8], F32, "fr")
            LD('sp', fr[:, :, :], small_d[:, 16:24].rearrange("(b p) h -> p b h", p=128), [fr])
            fbb = fw.sb([128, 8], F32, "fbb")
            LD('sp', fbb[:, :], forget_b[l].partition_broadcast(128), [fbb])
            lf = fw.sb([128, 8, NT], F32, "lf")
            TT('dve', lf[:, :, :], fr[:, :, :].rearrange("p b h -> p h b"), fbb[:, :].unsqueeze(2).to_broadcast([128, 8, NT]), ALU.add, [fr, fbb], [lf])
            ACT(lf[:, :, :], lf[:, :, :], AF.Exp, [lf], [lf], scale=-1.0)
            ACT(lf[:, :, :], lf[:, :, :], AF.Ln, [lf], [lf], bias=1.0)
            TS('dve', lf[:, :, :], lf[:, :, :], -1.0, None, ALU.mult, None, [lf], [lf])
            lf2 = lf[:, :, :].rearrange("p h b -> p (h b)")
            NHB = 8 * NT
            P = ps[0]
            MM(P[:, 0:NHB], Uinc, lf2, True, True, [cst, lf], [P])
            P1 = ps[1]
            MM(P1[:, 0:NHB], ones, lf2, True, True, [cst, lf], [P1])
            W = fw.sb([128, 8, NT], F32, "W")
            T = fw.sb([128, 8, NT], F32, "T")
            CP('dve', W[:, :, :].rearrange("p h b -> p (h b)"), P[:, 0:NHB], [P], [W])
            CP('dve', T[:, :, :].rearrange("p h b -> p (h b)"), P1[:, 0:NHB], [P1], [T])
            Pf = fw.sb([128, 8, NT], F32, "Pf")
            MEMSET('dve', Pf[:, :, 0:1], 0.0, [Pf])
            for b in range(1, NT):
                TT('dve', Pf[:, :, b:b + 1], Pf[:, :, b - 1:b], T[:, :, b - 1:b], ALU.add, [Pf, T], [Pf])
            Fm = fw.sb([128, 8, NT], F32, "Fm")
            TT('dve', Fm[:, :, :], W[:, :, :], Pf[:, :, :], ALU.add, [W, Pf], [Fm])
            bt = fw.sb([128, 8, NB, NT], F32, "bt")
            for h in range(8):
                TT('dve', bt[:, h, :, :], Pf[:, h, :].rearrange("p (j r) -> p j r", r=4)[:, :, 0:1].to_broadcast([128, NB, NT]),
                   Fm[:, h, :].unsqueeze(1).to_broadcast([128, NB, NT]), ALU.subtract, [Pf, Fm], [bt])
                ST('sp', fbias_d[h], bt[:, h, :, :].rearrange("p j i -> p (j i)"), [bt])
            fl = fw.sb([128, 8, NT], F32, "fl")
            for h in range(8):
                TT('dve', fl[:, h, :].rearrange("p (j r) -> p j r", r=4), Fm[:, h, :].rearrange("p (j r) -> p j r", r=4),
                   Pf[:, h, :].rearrange("p (j r) -> p j r", r=4)[:, :, 0:1].to_broadcast([128, NB, 4]), ALU.subtract, [Fm, Pf], [fl])
            TS('dve', fl[:, :, :], fl[:, :, :], 8.0, None, ALU.mult, None, [fl], [fl])
            hi = fw.sb([128, 8, NT], BF16, "hi")
            hi32 = fw.sb([128, 8, NT], F32, "hi32")
            lo = fw.sb([128, 8, NT], BF16, "lo")
            CP('dve', hi[:, :, :], fl[:, :, :], [fl], [hi])
            CP('dve', hi32[:, :, :], hi[:, :, :], [hi], [hi32])
            TT('dve', lo[:, :, :], fl[:, :, :], hi32[:, :, :], ALU.subtract, [fl, hi32], [lo])
            rows = fw.sb([128, 2, 2, 128], BF16, "rows")
            HB = 128 // NT
            for wi, src in enumerate((hi, lo)):
                s2 = src[:, :, :].rearrange("p h b -> p (h b)")
                nhalf = (8 * NT + 127) // 128
                for hf in range(nhalf):
                    n = min(128, 8 * NT - hf * 128)
                    PT = ps[2 + (wi * 2 + hf) % 2]
                    TR(psbf(2 + (wi * 2 + hf) % 2)[0:n, 0:128], s2[:, hf * 128:hf * 128 + n], [src], [PT])
                    CP('dve', rows[0:n, wi, hf, :], psbf(2 + (wi * 2 + hf) % 2)[0:n, 0:128], [PT], [rows])
            for h in range(8):
                for wi in range(2):
                    g = h * NT
                    hf, p0 = g // 128, g % 128
                    ST('sp', qT_d[h, 64 + wi, :].rearrange("(b p) -> b p", p=128), rows[p0:p0 + NT, wi, hf, :], [rows])

    def att_phase(l):
        with fw.phase("att"):
            qr = fw.ring(2, [66, S], BF16, "qa")
            kr = fw.ring(2, [66, S], BF16, "ka")
            vr = fw.ring(2, [128, NT, 128], BF16, "va")
            br = fw.ring(2, [128, NB * NT], F32, "fb")
            pr = fw.ring(3, [128, 512], BF16, "pt")
            mr = fw.ring(2, [128, 128], F32, "msk")
            rr = fw.ring(2, [64, 512], F32, "rs")
            orr = fw.ring(2, [64, 512], BF16, "on")
            negm = fw.sb([128, 128], F32, "negm")
            TS('dve', negm[:, :], Lstr, -30000.0, None, ALU.mult, None, [cst], [negm])

            def loads(h):
                LD('sp', qr[h % 2][:, :], qT_d[h], [qr[h % 2]])
                LD('sp', kr[h % 2][:, :], kT_d[h], [kr[h % 2]])
                LD('sp', vr[h % 2][:, :, :], va_d[:, h, :].rearrange("(b p) d -> p b d", p=128), [vr[h % 2]])
                LD('sp', br[h % 2][:, :], fbias_d[h], [br[h % 2]])
            loads(0)
            it = 0
            for h in range(8):
                if h + 1 < 8:
                    loads(h + 1)
                qa, ka, va, fb = qr[h % 2], kr[h % 2], vr[h % 2], br[h % 2]
                for j in range(NB):
                    PO = ps[6 + j % 2]
                    nk = 4 * j + 4
                    for i in range(nk):
                        r = i - 4 * j
                        c0 = 128 * r if r > 0 else 0
                        n = 512 - c0
                        PS_ = ps[it % 4]
                        pt = pr[it % 3]
                        it += 1
                        bias = fb[:, j * NT + i:j * NT + i + 1]
                        MM(PS_[:, 0:n], ka[:, i * 128:(i + 1) * 128], qa[:, j * 512 + c0:(j + 1) * 512], True, True, [ka, qa], [PS_])
                        if r >= 0:
                            mk = mr[i % 2]
                            TT('dve', mk[:, :], PS_[:, 0:128], negm[:, :], ALU.add, [PS_, negm], [mk])
                            ACT(pt[:, 0:128], mk[:, :], AF.Exp, [mk, fb], [pt], bias=bias, scale=0.125)
                            if n > 128:
                                ACT(pt[:, 128:n], PS_[:, 128:n], AF.Exp, [PS_, fb], [pt], bias=bias, scale=0.125)
                        else:
                            ACT(pt[:, 0:n], PS_[:, 0:n], AF.Exp, [PS_, fb], [pt], bias=bias, scale=0.125)
                        MM(PO[:, c0:512], va[:, i, :], pt[:, 0:n], i == 0, i == nk - 1, [va, pt], [PO], inc=(i == nk - 1))
                    rs = rr[j % 2]
                    on = orr[j % 2]
                    CP('dve', rs[:, :], PO[64:128, :], [PO], [rs])
                    fw.op('dve', lambda e: e.reciprocal(out=rs[:, :], in_=rs[:, :]), reads=[rs], writes=[rs])
                    TT('dve', on[:, :], PO[0:64, :], rs[:, :], ALU.mult, [PO, rs], [on])
                    ST('sp', oT_d[h // 2, (h % 2) * 64:(h % 2) * 64 + 64, j * 512:(j + 1) * 512], on[:, :], [on])

    def ssd_phase(l):
        with fw.phase("ssd"):
            dtb = fw.sb([128, 16], F32, "dtb")
            Ab = fw.sb([128, 16], F32, "Ab")
            dsk = fw.sb([128, 16], F32, "dsk")
            LD('sp', dtb[:, :], dt_bias[l].partition_broadcast(128), [dtb])
            LD('sp', Ab[:, :], a_log[l].partition_broadcast(128), [Ab])
            LD('sp', dsk[:, :], d_skip[l].partition_broadcast(128), [dsk])
            ACT(Ab[:, :], Ab[:, :], AF.Exp, [Ab], [Ab])
            TS('dve', Ab[:, :], Ab[:, :], -1.0, None, ALU.mult, None, [Ab], [Ab])
            st32 = fw.sb([128, D], F32, "st32")
            stb = fw.sb([128, D], BF16, "stb")
            MEMSET('dve', st32[:, :], 0.0, [st32])
            MEMSET('dve', stb[:, :], 0.0, [stb])
            xsr = fw.ring(2, [128, D], BF16, "xs")
            zsr = fw.ring(2, [128, D], BF16, "zs")
            btr = fw.ring(2, [128, 512], BF16, "btm")
            bTr = fw.ring(2, [128, 4, 128], BF16, "bT")
            cTr = fw.ring(2, [128, 4, 128], BF16, "cT")
            dtr = fw.ring(2, [128, 16], F32, "dtraw")
            smr = fw.ring(2, [128, 160], F32, "ssm")
            xdr = fw.ring(2, [128, D], BF16, "xdt")
            xer = fw.ring(2, [128, D], BF16, "xdte")
            AMr = fw.ring(2, [128, 16, 128], F32, "AM")
            Dmr = fw.ring(2, [128, 16, 128], F32, "Dm")
            cbr = fw.ring(2, [128, 4, 128], F32, "cbm")
            Mtr = fw.ring(2, [128, 16, 128], BF16, "Mt")
            t1r = fw.ring(2, [128, D], F32, "t1")
            t2r = fw.ring(2, [128, D], F32, "t2")
            gnr = fw.ring(2, [128, D], BF16, "gn")
            gTr = fw.ring(2, [128, 8, 128], BF16, "gT")
            junk = fw.sb([128, D], BF16, "junk")

            def loads(c):
                tok = slice(c * 128, (c + 1) * 128)
                k = c % 2
                LD('sp', xsr[k][:, :], xs_d[tok, :], [xsr[k]])
                LD('sp', zsr[k][:, :], zs_d[tok, :], [zsr[k]])
                LD('sp', btr[k][:, :], Btm_d[tok, :], [btr[k]])
                LD('sp', bTr[k][:, :, :], BT_d.rearrange("g p s -> p g s")[:, :, tok], [bTr[k]])
                LD('sp', cTr[k][:, :, :], CT_d.rearrange("g p s -> p g s")[:, :, tok], [cTr[k]])
                LD('sp', dtr[k][:, :], small_d[tok, 0:16], [dtr[k]])
            loads(0)
            for c in range(NT):
                if c + 1 < NT:
                    loads(c + 1)
                k = c % 2
                tok = slice(c * 128, (c + 1) * 128)
                xs, zs, btm, bT, cTt, dtraw, sm = xsr[k], zsr[k], btr[k], bTr[k], cTr[k], dtr[k], smr[k]
                xdt, xdte, AM, Dm, cbm, Mt, t1, t2, gn, gT = xdr[k], xer[k], AMr[k], Dmr[k], cbr[k], Mtr[k], t1r[k], t2r[k], gnr[k], gTr[k]
                dt_, a_, acs, tot, ea, te, cd, dte = [sm[:, 16 * i:16 * i + 16] for i in range(8)]
                TT('dve', dt_, dtraw[:, :], dtb[:, :], ALU.add, [dtraw, dtb], [sm])
                ACT(dt_, dt_, AF.Exp, [sm], [sm])
                ACT(dt_, dt_, AF.Ln, [sm], [sm], bias=1.0)
                TT('dve', a_, dt_, Ab[:, :], ALU.mult, [sm, Ab], [sm])
                P0 = ps[0]
                MM(P0[:, 0:16], Uinc, a_, True, True, [cst, sm], [P0], inc=False)
                MM(P0[:, 16:32], ones, a_, True, True, [cst, sm], [P0])
                CP('dve', sm[:, 32:64], P0[:, 0:32], [P0], [sm])
                ACT(ea, acs, AF.Exp, [sm], [sm])
                TT('dve', te, tot, acs, ALU.subtract, [sm], [sm])
                ACT(te, te, AF.Exp, [sm], [sm])
                ACT(cd, tot, AF.Exp, [sm], [sm])
                TT('dve', dte, dt_, te, ALU.mult, [sm], [sm])
                xs3 = xs[:, :].rearrange("p (h d) -> p h d", h=16)
                TT('dve', xdt[:, :].rearrange("p (h d) -> p h d", h=16), xs3, dt_.unsqueeze(2).to_broadcast([128, 16, 64]), ALU.mult, [xs, sm], [xdt])
                TT('pool', xdte[:, :].rearrange("p (h d) -> p h d", h=16), xs3, dte.unsqueeze(2).to_broadcast([128, 16, 64]), ALU.mult, [xs, sm], [xdte])
                TT('pool', AM[:, :, :], Lstr.unsqueeze(1).to_broadcast([128, 16, 128]), a_.unsqueeze(2).to_broadcast([128, 16, 128]), ALU.mult, [cst, sm], [AM])
                for hh in range(16):
                    PSg = ps[1 + hh // 4]
                    MM(PSg[:, (hh % 4) * 128:(hh % 4 + 1) * 128], AM[:, hh, :], Uinc, True, True, [AM, cst], [PSg], inc=(hh % 4 == 3))
                for q4 in range(4):
                    ACT(Dm[:, 4 * q4:4 * q4 + 4, :].rearrange("p h s -> p (h s)"), ps[1 + q4][:, :], AF.Exp, [ps[1 + q4]], [Dm])
                for g in range(4):
                    MM(P0[:, g * 128:(g + 1) * 128], bT[:, g, :], cTt[:, g, :], True, True, [bT, cTt], [P0], inc=(g == 3))
                TT('dve', cbm[:, :, :], P0[:, :].rearrange("p (g s) -> p g s", g=4), Uinc.unsqueeze(1).to_broadcast([128, 4, 128]), ALU.mult, [P0, cst], [cbm])
                TT('dve', Mt[:, :, :].rearrange("p (g r) s -> p g r s", g=4), Dm[:, :, :].rearrange("p (g r) s -> p g r s", g=4),
                   cbm[:, :, :].unsqueeze(2).to_broadcast([128, 4, 4, 128]), ALU.mult, [Dm, cbm], [Mt])
                for hh in range(16):
                    PY = ps[5 + hh // 8]
                    MM(PY[:, (hh % 8) * 64:(hh % 8 + 1) * 64], Mt[:, hh, :], xdt[:, hh * 64:(hh + 1) * 64], True, True, [Mt, xdt], [PY], inc=(hh % 8 == 7))
                for g in range(4):
                    PF = ps[1 + g // 2]
                    MM(PF[:, (g % 2) * 256:(g % 2 + 1) * 256], cTt[:, g, :], stb[:, g * 256:(g + 1) * 256], True, True, [cTt, stb], [PF], inc=(g % 2 == 1))
                for g in range(4):
                    PSt = ps[3 + g // 2]
                    MM(PSt[:, (g % 2) * 256:(g % 2 + 1) * 256], btm[:, g * 128:(g + 1) * 128], xdte[:, g * 256:(g + 1) * 256], True, True, [btm, xdte], [PSt], inc=(g % 2 == 1))
                pyoff = psall[:, 512:1536].rearrange("p (h d) -> p h d", h=16)
                pydia = psall[:, 2560:3584]
                TT('dve', t1[:, :].rearrange("p (h d) -> p h d", h=16), pyoff, ea.unsqueeze(2).to_broadcast([128, 16, 64]), ALU.mult, [ps[1], ps[2], sm], [t1])
                TT('pool', t2[:, :].rearrange("p (h d) -> p h d", h=16), xs3, dsk[:, :].unsqueeze(2).to_broadcast([128, 16, 64]), ALU.mult, [xs, dsk], [t2])
                TT('dve', t1[:, :], t1[:, :], pydia, ALU.add, [t1, ps[5], ps[6]], [t1])
                TT('pool', t1[:, :], t1[:, :], t2[:, :], ALU.add, [t1, t2], [t1])
                TT('pool', t1[:, :], t1[:, :], zs[:, :], ALU.mult, [t1, zs], [t1])
                MEMSET('dve', sm[:, 128:129], 0.0, [sm])
                ACT(junk[:, :], t1[:, :], AF.Square, [t1], [junk, sm], accum=sm[:, 128:129])
                TS('dve', sm[:, 129:130], sm[:, 128:129], 1.0 / D, EPS, ALU.mult, ALU.add, [sm], [sm])
                ACT(sm[:, 129:130], sm[:, 129:130], AF.Ln, [sm], [sm])
                ACT(sm[:, 129:130], sm[:, 129:130], AF.Exp, [sm], [sm], scale=-0.5)
                ACT(gn[:, :], t1[:, :], AF.Copy, [t1, sm], [gn], scale=sm[:, 129:130])
                PT = ps[7]
                for cc in range(8):
                    TR(psbf(7)[:, cc * 128:(cc + 1) * 128], gn[:, cc * 128:(cc + 1) * 128], [gn], [PT], inc=(cc == 7))
                CP('act', gT[:, :, :].rearrange("p c s -> p (c s)"), psbf(7)[:, :], [PT], [gT])
                ST('sp', gnT_d.rearrange("c p s -> p c s")[:, :, tok], gT[:, :, :], [gT])
                TT('pool', st32[:, :].rearrange("p (h d) -> p h d", h=16), st32[:, :].rearrange("p (h d) -> p h d", h=16),
                   cd.unsqueeze(2).to_broadcast([128, 16, 64]), ALU.mult, [st32, sm], [st32])
                TT('dve', st32[:, :], st32[:, :], psall[:, 1536:2560], ALU.add, [st32, ps[3], ps[4], stb], [st32])
                CP('act', stb[:, :], st32[:, :], [st32], [stb])

    def out_phase(l):
        s = l % 2
        with fw.phase("out"):
            wso = fw.sb([128, 8, D], BF16, "wso")
            wao = fw.sb([128, 4, D], BF16, "wao")
            wo = fw.sb([128, 8, D], BF16, "wo")
            nw = fw.sb([128, 8], F32, "nw")
            LD('sp', wso[:, :, :], wb_so[s].rearrange("(kc p) n -> p kc n", p=128), [wso], R=[wtok[l]])
            LD('sp', wao[:, :, :], wb_ao[s].rearrange("(kc p) n -> p kc n", p=128), [wao])
            LD('sp', wo[:, :, :], wb_o[s].rearrange("(kc p) n -> p kc n", p=128), [wo])
            LD('sp', nw[:, :], ssd_norm_wT[l], [nw])
            for kc in range(8):
                TS('dve' if kc % 2 else 'pool', wso[:, kc, :], wso[:, kc, :], nw[:, kc:kc + 1], None, ALU.mult, None, [wso, nw], [wso])
            gr = fw.ring(2, [128, 8, 512], BF16, "gTi")
            orr = fw.ring(2, [128, 4, 512], BF16, "oTi")
            sr = fw.ring(2, [128, 16, 512], BF16, "sgi")
            mr = fw.ring(2, [128, 8, 512], BF16, "mT")
            t1r = fw.ring(2, [128, 512], F32, "ot1")
            t2r = fw.ring(2, [128, 512], F32, "ot2")
            yr = fw.ring(2, [128, D], F32, "yo")

            def loads(nb):
                cols = slice(nb * 512, (nb + 1) * 512)
                LD('sp', gr[nb % 2][:, :, :], gnT_d.rearrange("c p s -> p c s")[:, :, cols], [gr[nb % 2]])
                LD('sp', orr[nb % 2][:, :, :], oT_d.rearrange("c p s -> p c s")[:, :, cols], [orr[nb % 2]])
                LD('sp', sr[nb % 2][:, :, :], sgT_d.rearrange("c p s -> p c s")[:, :, cols], [sr[nb % 2]])
            loads(0)
            it = 0
            for nb in range(NB):
                if nb + 1 < NB:
                    loads(nb + 1)
                gT, oT, sg, mT = gr[nb % 2], orr[nb % 2], sr[nb % 2], mr[nb % 2]
                for m in range(8):
                    P1, P2 = ps[(it % 2) * 2], ps[(it % 2) * 2 + 1]
                    t1, t2 = t1r[it % 2], t2r[it % 2]
                    it += 1
                    for kc in range(8):
                        MM(P1[:, :], wso[:, kc, m * 128:(m + 1) * 128], gT[:, kc, :], kc == 0, kc == 7, [wso, gT], [P1], inc=(kc == 7))
                    for kc in range(4):
                        MM(P2[:, :], wao[:, kc, m * 128:(m + 1) * 128], oT[:, kc, :], kc == 0, kc == 3, [wao, oT], [P2], inc=(kc == 3))
                    TT('dve', t1[:, :], P1[:, :], sg[:, m, :], ALU.mult, [P1, sg], [t1])
                    TT('dve', t2[:, :], P2[:, :], sg[:, 8 + m, :], ALU.mult, [P2, sg], [t2])
                    TT('pool', mT[:, m, :], t1[:, :], t2[:, :], ALU.add, [t1, t2], [mT])
                for j in range(4):
                    yo = yr[j % 2]
                    for hf in range(2):
                        P3 = ps[4 + (2 * j + hf) % 4]
                        for kc in range(8):
                            MM(P3[:, :], mT[:, kc, j * 128:(j + 1) * 128], wo[:, kc, hf * 512:(hf + 1) * 512], kc == 0, kc == 7, [mT, wo], [P3], inc=(kc == 7))
                        CP('act', yo[:, hf * 512:(hf + 1) * 512], P3[:, :], [P3], [yo])
                    ST('sp', y_d[(nb * 4 + j) * 128:(nb * 4 + j + 1) * 128, :], yo[:, :], [yo])

    def moe_phase(l):
        s = l % 2
        NH = 2 if S >= 2048 else 1
        SH = S // NH
        NTH = SH // 128
        NBH = SH // 512
        for half in range(NH):
            with fw.phase("moe"):
                hT = fw.sb([128, 8, SH], BF16, "h2T")
                fw.dma('sp', lambda e: [e.dma_start(out=hT[:, c, :], in_=hT_d[c, :, half * SH:(half + 1) * SH]) for c in range(8)], writes=[hT])
                wr = fw.sb([128, 8, 20], BF16, "wr")
                LD('sp', wr[:, :, :], wb_r[s].rearrange("(kc p) n -> p kc n", p=128), [wr], R=[wtok[l]])
                brt = fw.sb([128, 20], F32, "brt")
                LD('sp', brt[:, :], b_router[l].partition_broadcast(128), [brt])
                lg = fw.sb([128, NTH, 20], F32, "lg")
                for t in range(NTH):
                    P = ps[t % 2]
                    for kc in range(8):
                        MM(P[:, 0:20], hT[:, kc, t * 128:(t + 1) * 128], wr[:, kc, :], kc == 0, kc == 7, [hT, wr], [P], inc=(kc == 7))
                    TT('dve', lg[:, t, :], P[:, 0:20], brt[:, :], ALU.add, [P, brt], [lg])
                gl = lg[:, :, 0:4]
                el = lg[:, :, 4:20].rearrange("p t (g e) -> p t g e", g=4)
                R_ = fw.sb([128, NTH, 64], F32, "rt")
                gm = R_[:, :, 0:1]
                oh = R_[:, :, 4:8]
                ex = R_[:, :, 8:12]
                gs = R_[:, :, 12:13]
                ein = R_[:, :, 16:20]
                m1 = R_[:, :, 20:21]
                k1 = R_[:, :, 24:28]
                e2 = R_[:, :, 28:32]
                m2 = R_[:, :, 32:33]
                k2 = R_[:, :, 36:40]
                w1 = R_[:, :, 40:41]
                w2 = R_[:, :, 41:42]
                wg = R_[:, :, 44:48]
                tmp = R_[:, :, 48:64].rearrange("p t (g e) -> p t g e", g=4)
                RR = [R_, lg]
                fw.op('dve', lambda e: e.tensor_reduce(out=gm, in_=gl, axis=AX.X, op=ALU.max), reads=RR, writes=[R_])
                TT('dve', oh, gl, gm.to_broadcast([128, NTH, 4]), ALU.is_equal, RR, [R_])
                TT('dve', ex, gl, gm.to_broadcast([128, NTH, 4]), ALU.subtract, RR, [R_])
                ACT(ex, ex, AF.Exp, [R_], [R_])
                fw.op('dve', lambda e: e.tensor_reduce(out=gs, in_=ex, axis=AX.X, op=ALU.add), reads=RR, writes=[R_])
                fw.op('dve', lambda e: e.reciprocal(out=gs, in_=gs), reads=[R_], writes=[R_])
                TT('dve', tmp, el, oh.unsqueeze(3).to_broadcast([128, NTH, 4, 4]), ALU.mult, RR, [R_])
                fw.op('dve', lambda e: e.tensor_reduce(out=ein, in_=R_[:, :, 48:64].rearrange("p t (g e) -> p t e g", g=4), axis=AX.X, op=ALU.add), reads=RR, writes=[R_])
                fw.op('dve', lambda e: e.tensor_reduce(out=m1, in_=ein, axis=AX.X, op=ALU.max), reads=RR, writes=[R_])
                TT('dve', k1, ein, m1.to_broadcast([128, NTH, 4]), ALU.is_equal, RR, [R_])
                STT('dve', e2, k1, -1e30, ein, ALU.mult, ALU.add, RR, [R_])
                fw.op('dve', lambda e: e.tensor_reduce(out=m2, in_=e2, axis=AX.X, op=ALU.max), reads=RR, writes=[R_])
                TT('dve', k2, e2, m2.to_broadcast([128, NTH, 4]), ALU.is_equal, RR, [R_])
                TT('dve', w1, m2, m1, ALU.subtract, RR, [R_])
                ACT(w1, w1, AF.Exp, [R_], [R_])
                TS('dve', w1, w1, 1.0, None, ALU.add, None, RR, [R_])
                fw.op('dve', lambda e: e.reciprocal(out=w1, in_=w1), reads=[R_], writes=[R_])
                TS('dve', w2, w1, -1.0, 1.0, ALU.mult, ALU.add, RR, [R_])
                TT('dve', w1, w1, gs, ALU.mult, RR, [R_])
                TT('dve', w2, w2, gs, ALU.mult, RR, [R_])
                TT('dve', k1, k1, w1.to_broadcast([128, NTH, 4]), ALU.mult, RR, [R_])
                TT('dve', k2, k2, w2.to_broadcast([128, NTH, 4]), ALU.mult, RR, [R_])
                TT('dve', wg, k1, k2, ALU.add, RR, [R_])
                gates = fw.sb([128, NTH, 16], F32, "gates")
                TT('dve', gates[:, :, :].rearrange("p t (g e) -> p t g e", g=4), oh.unsqueeze(3).to_broadcast([128, NTH, 4, 4]),
                   wg.unsqueeze(2).to_broadcast([128, NTH, 4, 4]), ALU.mult, RR, [gates])
                acc = fw.sb([128, NTH, D], F32, "acc")
                wgr = fw.ring(2, [128, 8, 512], BF16, "wg")
                wur = fw.ring(2, [128, 8, 512], BF16, "wu")
                wdr = fw.ring(2, [128, 4, D], BF16, "wd")
                sgr = fw.ring(2, [128, 512], F32, "sgl")
                hdr = fw.ring(2, [128, 4, 512], BF16, "hid")

                def ldw(e_):
                    k = e_ % 2
                    LD('sp', wgr[k][:, :, :], wb_g[s, e_].rearrange("(kc p) n -> p kc n", p=128), [wgr[k]])
                    LD('sp', wur[k][:, :, :], wb_u[s, e_].rearrange("(kc p) n -> p kc n", p=128), [wur[k]])
                    LD('sp', wdr[k][:, :, :], wb_d[s, e_].rearrange("(kc p) n -> p kc n", p=128), [wdr[k]])
                ldw(0)
                it = 0
                for e_ in range(16):
                    if e_ + 1 < 16:
                        ldw(e_ + 1)
                    wgt, wut, wdt = wgr[e_ % 2], wur[e_ % 2], wdr[e_ % 2]
                    for nb in range(NBH):
                        cols = slice(nb * 512, (nb + 1) * 512)
                        hid = hdr[nb % 2]
                        for c in range(4):
                            PG, PU = ps[(it % 2) * 2], ps[(it % 2) * 2 + 1]
                            sgl = sgr[it % 2]
                            it += 1
                            for kc in range(8):
                                MM(PG[:, :], wgt[:, kc, c * 128:(c + 1) * 128], hT[:, kc, cols], kc == 0, kc == 7, [wgt, hT], [PG], inc=(kc == 7))
                            for kc in range(8):
                                MM(PU[:, :], wut[:, kc, c * 128:(c + 1) * 128], hT[:, kc, cols], kc == 0, kc == 7, [wut, hT], [PU], inc=(kc == 7))
                            ACT(sgl[:, :], PG[:, :], AF.Silu, [PG], [sgl])
                            TT('dve', hid[:, c, :], PU[:, :], sgl[:, :], ALU.mult, [PU, sgl], [hid])
                        for j in range(4):
                            t = nb * 4 + j
                            for hf in range(2):
                                PD = ps[4 + (2 * j + hf) % 4]
                                for c in range(4):
                                    MM(PD[:, :], hid[:, c, j * 128:(j + 1) * 128], wdt[:, c, hf * 512:(hf + 1) * 512], c == 0, c == 3, [hid, wdt], [PD], inc=(c == 3))
                                av = acc[:, t, hf * 512:(hf + 1) * 512]
                                if e_ == 0:
                                    TS('dve', av, PD[:, :], gates[:, t, e_:e_ + 1], None, ALU.mult, None, [PD, gates], [acc])
                                else:
                                    STT('dve', av, PD[:, :], gates[:, t, e_:e_ + 1], av, ALU.mult, ALU.add, [PD, gates, acc], [acc])
                for t in range(NTH):
                    ST('sp', y_d[half * SH + t * 128:half * SH + (t + 1) * 128, :], acc[:, t, :], [acc])

    stages = []
    for l in range(L):
        if l + 1 < L:
            cast_weights(l + 1)
        xs_src = x_in if l == 0 else xres_d
        if l == 0:
            ln_phase(l, x_in, None, None, None, None, None, 1, 0)
        win_phase(l)
        if stop_after == 'win':
            break
        attprep_phase(l)
        att_phase(l)
        if stop_after == 'att':
            break
        ssd_phase(l)
        if stop_after == 'ssd':
            break
        out_phase(l)
        if stop_after == 'out':
            break
        ln_phase(l, xs_src, y_d, xres_d, 2, ln1_g[l], ln1_b[l], 4, 3)
        if stop_after == 'ln1':
            break
        moe_phase(l)
        if stop_after == 'moe':
            break
        last = (l == L - 1)
        ln_phase(l, xres_d, y_d, out if last else xres_d, 5, ln2_g[l], ln2_b[l], None if last else 1, None if last else 0, lp=l + 1)
    fw.barrier()
    return nc, fw


def host_inputs(inputs, S=4096, L=4):
    f = lambda a: np.ascontiguousarray(np.asarray(a, dtype=np.float32))
    w_in = f(inputs['w_in'])[:L]
    ident = np.eye(128, dtype=np.float32)
    U = np.triu(np.ones((128, 128), np.float32))
    Ls = np.tril(np.ones((128, 128), np.float32), -1)
    consts = np.concatenate([ident, U, Ls, np.ones((128, 128), np.float32)], axis=1)
    shared = {
        'consts': f(consts),
        'w_mod': f(inputs['w_mod'])[:L], 'b_mod': f(inputs['b_mod'])[:L],
        'w_in': w_in,
        'w_small': f(np.concatenate([w_in[:, :, ODT:ODT + 16], w_in[:, :, OF:OF + 8]], axis=2)),
        'conv_wT': f(np.asarray(inputs['conv_w'])[:L].reshape(L, 4, 16, 128).transpose(0, 3, 2, 1)),
        'conv_bT': f(np.asarray(inputs['conv_b'])[:L].reshape(L, 16, 128).transpose(0, 2, 1)),
        'dt_bias': f(inputs['dt_bias'])[:L], 'a_log': f(inputs['a_log'])[:L], 'd_skip': f(inputs['d_skip'])[:L],
        'ssd_norm_wT': f(np.asarray(inputs['ssd_norm_w'])[:L].reshape(L, 8, 128).transpose(0, 2, 1)),
        'forget_b': f(inputs['forget_b'])[:L],
        'w_ssd_o': f(inputs['w_ssd_o'])[:L], 'w_att_o': f(inputs['w_att_o'])[:L], 'w_o': f(inputs['w_o'])[:L],
        'ln1_g': f(inputs['ln1_g'])[:L], 'ln1_b': f(inputs['ln1_b'])[:L],
        'w_router': f(np.concatenate([np.asarray(inputs['w_router_group'])[:L], np.asarray(inputs['w_router_expert'])[:L]], axis=2)),
        'b_router': f(np.concatenate([np.asarray(inputs['b_router_group'])[:L], np.asarray(inputs['b_router_expert'])[:L]], axis=1)),
        'w_gate': f(inputs['w_gate'])[:L], 'w_up': f(inputs['w_up'])[:L], 'w_down': f(inputs['w_down'])[:L],
        'ln2_g': f(inputs['ln2_g'])[:L], 'ln2_b': f(inputs['ln2_b'])[:L],
    }
    x = np.asarray(inputs['x'], dtype=np.float32)
    c = np.asarray(inputs['c'], dtype=np.float32)
    maps = []
    for b in range(x.shape[0]):
        m = dict(shared)
        m['x'] = np.ascontiguousarray(x[b, :S])
        m['cT'] = np.ascontiguousarray(c[b].reshape(8, 128).T)
        maps.append(m)
    return maps


_PROG = {}


def kernel(**inputs):
    S, L = 4096, 4
    if 'p' not in _PROG:
        _PROG['p'] = build_program(S, L)[0]
    nc = _PROG['p']
    maps = host_inputs(inputs, S, L)
    res = run_bass_kernel_spmd(nc, maps, core_ids=list(range(8)))
    return np.stack([np.asarray(r['out'], dtype=np.float32) for r in res.results], axis=0)
```

```python
import numpy as np
from contextlib import contextmanager, ExitStack
import concourse.bass as bass
import concourse.mybir as mybir
from concourse.bass_utils import run_bass_kernel_spmd

F32 = mybir.dt.float32
BF16 = mybir.dt.bfloat16
AF = mybir.ActivationFunctionType
ALU = mybir.AluOpType
AX = mybir.AxisListType

D = 1024
NIN = 6680
ALPHA = 8 ** 0.25
EPS = 1e-5
OZ, OXBC, ODT, OQ, OK_, OV, OF, OGS, OGA = 0, 1024, 3072, 3088, 3600, 4112, 4624, 4632, 5656


class Buf:
    def __init__(self, t, name):
        self.t = t
        self.name = name
        self.w = {}
        self.r = {}
        self.dsem = None

    def __getitem__(self, key):
        return self.t[key]


class FW:
    ENGS = ('pe', 'act', 'dve', 'pool', 'sp')

    def __init__(self, nc, n_dma_sems=48):
        self.nc = nc
        self.e = {'pe': nc.tensor, 'act': nc.scalar, 'dve': nc.vector, 'pool': nc.gpsimd, 'sp': nc.sync}
        self.esem = {k: nc.alloc_semaphore("es_" + k) for k in self.ENGS}
        self.ecount = {k: 0 for k in self.ENGS}
        self.known = {k: {} for k in self.ENGS}
        self.pending = {k: [] for k in self.ENGS}
        self.dsems = [nc.alloc_semaphore("ds_%d" % i) for i in range(n_dma_sems)]
        self.dtotal = {s: 0 for s in self.dsems}
        self.free_dsems = list(self.dsems)
        self.stack = None
        self.phase_bufs = []
        self.bar = nc.alloc_semaphore("bar")
        self.barcount = 0
        self.nbuf = 0
        self.ninstr = 0

    def sb(self, shape, dtype, name=None):
        self.nbuf += 1
        name = (name or "t") + "_%d" % self.nbuf
        if self.stack is not None:
            t = self.stack.enter_context(self.nc.sbuf_tensor(name, list(shape), dtype))
        else:
            t = self.nc.alloc_sbuf_tensor(name, list(shape), dtype)
        b = Buf(t, name)
        if self.stack is not None:
            self.phase_bufs.append(b)
        return b

    def ring(self, n, shape, dtype, name=None):
        return [self.sb(shape, dtype, name) for _ in range(n)]

    def token(self, name):
        b = Buf(None, name)
        if self.stack is not None:
            self.phase_bufs.append(b)
        return b

    def _dsem(self, b):
        if b.dsem is None:
            b.dsem = self.free_dsems.pop()
        return b.dsem

    @contextmanager
    def phase(self, name=""):
        assert self.stack is None
        self.stack = ExitStack()
        self.phase_bufs = []
        try:
            yield
            self.barrier()
        finally:
            for b in self.phase_bufs:
                if b.dsem is not None:
                    self.free_dsems.append(b.dsem)
                    b.dsem = None
            self.stack.close()
            self.stack = None
            self.phase_bufs = []

    def _waits(self, eng, reads, writes):
        waits = {}
        for b in reads:
            for s, v in b.w.items():
                if waits.get(s, 0) < v:
                    waits[s] = v
        for b in writes:
            for d in (b.w, b.r):
                for s, v in d.items():
                    if waits.get(s, 0) < v:
                        waits[s] = v
        e = self.e[eng]
        kn = self.known[eng]
        for s, v in waits.items():
            if eng == 'pe' and s is self.esem['pe']:
                continue
            if kn.get(s, 0) < v:
                e.wait_ge(s, v)
                self.ninstr += 1
                kn[s] = v

    def _record(self, ev, reads, writes):
        s, v = ev
        for b in writes:
            b.w = {s: v}
            b.r = {}
        for b in reads:
            if b.r.get(s, 0) < v:
                b.r[s] = v

    def op(self, eng, fn, reads=(), writes=(), inc=True):
        self._waits(eng, reads, writes)
        ins = fn(self.e[eng])
        self.ninstr += 1
        if not inc:
            self.pending[eng].append((tuple(reads), tuple(writes)))
            return ins
        self.ecount[eng] += 1
        ins.then_inc(self.esem[eng], 1)
        ev = (self.esem[eng], self.ecount[eng])
        for (r, w) in self.pending[eng]:
            self._record(ev, r, w)
        self.pending[eng] = []
        self._record(ev, reads, writes)
        return ins

    def dma(self, q, fn, reads=(), writes=(), sem_buf=None):
        self._waits(q, reads, writes)
        sbuf = sem_buf or (writes[0] if writes else reads[0])
        sem = self._dsem(sbuf)
        inss = fn(self.e[q])
        if not isinstance(inss, (list, tuple)):
            inss = [inss]
        for ins in inss:
            ins.then_inc(sem, 16)
            self.dtotal[sem] += 16
            self.ninstr += 1
        ev = (sem, self.dtotal[sem])
        self._record(ev, reads, writes)

    def barrier(self):
        sp = self.e['sp']
        kn = self.known['sp']
        for k in self.ENGS:
            assert not self.pending[k], "pending non-inc instrs at barrier on " + k
            s, v = self.esem[k], self.ecount[k]
            if k != 'sp' and kn.get(s, 0) < v:
                sp.wait_ge(s, v)
                kn[s] = v
        for s, v in self.dtotal.items():
            if kn.get(s, 0) < v:
                sp.wait_ge(s, v)
                kn[s] = v
        self.barcount += 1
        sp.sem_inc(self.bar, 1)
        for k in self.ENGS:
            if k == 'sp':
                continue
            self.e[k].wait_ge(self.bar, self.barcount)
            self.known[k] = dict(kn)
            self.known[k][self.esem[k]] = self.ecount[k]


def build_program(S=4096, L=4, debug=False, stop_after=None):
    nc = bass.Bass("TRN2", target_bir_lowering=False)
    NT = S // 128
    NB = S // 512
    fw = FW(nc)

    def inp(name, shape, dt=F32):
        return nc.dram_tensor(name, list(shape), dt, kind="ExternalInput").ap()

    dbg_kind = "ExternalOutput" if debug else "Internal"

    def scr(name, shape, dt, dbg=True):
        return nc.dram_tensor(name, list(shape), dt, kind=(dbg_kind if dbg else "Internal")).ap()

    x_in = inp("x", [S, D])
    cT = inp("cT", [128, 8])
    consts = inp("consts", [128, 512])
    w_mod = inp("w_mod", [L, D, 6 * D])
    b_mod = inp("b_mod", [L, 6 * D])
    w_in = inp("w_in", [L, D, NIN])
    w_small = inp("w_small", [L, D, 24])
    conv_wT = inp("conv_wT", [L, 128, 16, 4])
    conv_bT = inp("conv_bT", [L, 128, 16])
    dt_bias = inp("dt_bias", [L, 16])
    a_log = inp("a_log", [L, 16])
    d_skip = inp("d_skip", [L, 16])
    ssd_norm_wT = inp("ssd_norm_wT", [L, 128, 8])
    forget_b = inp("forget_b", [L, 8])
    w_ssd_o = inp("w_ssd_o", [L, D, D])
    w_att_o = inp("w_att_o", [L, 512, D])
    w_o = inp("w_o", [L, D, D])
    ln1_g = inp("ln1_g", [L, D])
    ln1_b = inp("ln1_b", [L, D])
    w_router = inp("w_router", [L, D, 20])
    b_router = inp("b_router", [L, 20])
    w_gate = inp("w_gate", [L, 16, D, 512])
    w_up = inp("w_up", [L, 16, D, 512])
    w_down = inp("w_down", [L, 16, 512, D])
    ln2_g = inp("ln2_g", [L, D])
    ln2_b = inp("ln2_b", [L, D])
    out = nc.dram_tensor("out", [S, D], F32, kind="ExternalOutput").ap()

    mod_row = scr("mod_row", [L, 6 * D], F32)
    wb_in = scr("wb_in", [2, D, NIN], BF16, dbg=False)
    wb_so = scr("wb_so", [2, D, D], BF16, dbg=False)
    wb_ao = scr("wb_ao", [2, 512, D], BF16, dbg=False)
    wb_o = scr("wb_o", [2, D, D], BF16, dbg=False)
    wb_r = scr("wb_r", [2, D, 20], BF16, dbg=False)
    wb_g = scr("wb_g", [2, 16, D, 512], BF16, dbg=False)
    wb_u = scr("wb_u", [2, 16, D, 512], BF16, dbg=False)
    wb_d = scr("wb_d", [2, 16, 512, D], BF16, dbg=False)
    hT_d = scr("hT_d", [8, 128, S], BF16)
    zs_d = scr("zs_d", [S, D], BF16)
    xs_d = scr("xs_d", [S, D], BF16)
    Btm_d = scr("Btm_d", [S, 512], BF16)
    BT_d = scr("BT_d", [4, 128, S], BF16)
    CT_d = scr("CT_d", [4, 128, S], BF16)
    small_d = scr("small_d", [S, 24], F32)
    qT_d = scr("qT_d", [8, 66, S], BF16)
    kT_d = scr("kT_d", [8, 66, S], BF16)
    va_d = scr("va_d", [S, 8, 128], BF16)
    sgT_d = scr("sgT_d", [16, 128, S], BF16)
    gnT_d = scr("gnT_d", [8, 128, S], BF16)
    oT_d = scr("oT_d", [4, 128, S], BF16)
    y_d = scr("y_d", [S, D], F32)
    xres_d = scr("xres_d", [S, D], F32)
    fbias_d = scr("fbias_d", [8, 128, NB * NT], F32)

    psall = nc.alloc_psum_tensor("psall", [128, 4096], F32)
    psall_bf = psall.bitcast(BF16)
    ps = [Buf(psall[:, i * 512:(i + 1) * 512], "ps%d" % i) for i in range(8)]

    def psbf(i):
        return psall_bf[:, i * 1024:(i + 1) * 1024]

    cst = fw.sb([128, 512], F32, "cst")
    identb = fw.sb([128, 128], BF16, "identb")
    ident = cst[:, 0:128]
    Uinc = cst[:, 128:256]
    Lstr = cst[:, 256:384]
    ones = cst[:, 384:512]

    def TT(eng, o, a, b, op, R, W):
        fw.op(eng, lambda e: e.tensor_tensor(out=o, in0=a, in1=b, op=op), reads=R, writes=W)

    def TS(eng, o, a, s1, s2, op0, op1, R, W):
        if s2 is None:
            fw.op(eng, lambda e: e.tensor_scalar(out=o, in0=a, scalar1=s1, scalar2=None, op0=op0), reads=R, writes=W)
        else:
            fw.op(eng, lambda e: e.tensor_scalar(out=o, in0=a, scalar1=s1, scalar2=s2, op0=op0, op1=op1), reads=R, writes=W)

    def STT(eng, o, a, s, b, op0, op1, R, W):
        fw.op(eng, lambda e: e.scalar_tensor_tensor(out=o, in0=a, scalar=s, in1=b, op0=op0, op1=op1), reads=R, writes=W)

    def ACT(o, a, func, R, W, bias=None, scale=None, accum=None):
        kw = {}
        if bias is not None:
            kw['bias'] = bias
        if scale is not None:
            kw['scale'] = scale
        if accum is not None:
            kw['accum_out'] = accum
        fw.op('act', lambda e: e.activation(out=o, in_=a, func=func, **kw), reads=R, writes=W)

    def CP(eng, o, a, R, W):
        if eng == 'act':
            ACT(o, a, AF.Copy, R, W)
        else:
            fw.op(eng, lambda e: e.tensor_copy(out=o, in_=a), reads=R, writes=W)

    def MM(o, lhsT, rhs, start, stop, R, W, inc=True):
        fw.op('pe', lambda e: e.matmul(o, lhsT=lhsT, rhs=rhs, start=start, stop=stop), reads=R, writes=W, inc=inc)

    def TR(o, a, R, W, inc=True):
        fw.op('pe', lambda e: e.transpose(o, a, identb[:, :]), reads=list(R) + [identb], writes=W, inc=inc)

    def LD(q, o, a, W, R=(), **kw):
        fw.dma(q, lambda e: e.dma_start(out=o, in_=a, **kw), reads=R, writes=W)

    def ST(q, o, a, R, **kw):
        fw.dma(q, lambda e: e.dma_start(out=o, in_=a, **kw), reads=R)

    def MEMSET(eng, o, val, W):
        fw.op(eng, lambda e: e.memset(o, val), writes=W)

    NC_OK = dict(allow_slow_non_contiguous=True)

    LD('sp', cst[:, :], consts, [cst])
    CP('dve', identb[:, :], ident, [cst], [identb])

    wtok = [fw.token("wtok%d" % l) for l in range(L)]

    def cast_weights(l):
        s = l % 2
        tk = wtok[l]

        def go(e):
            r = []
            for k in range(8):
                r.append(e.dma_start(out=wb_in[s, k * 128:(k + 1) * 128, :], in_=w_in[l, k * 128:(k + 1) * 128, :]))
                r.append(e.dma_start(out=wb_so[s, k * 128:(k + 1) * 128, :], in_=w_ssd_o[l, k * 128:(k + 1) * 128, :]))
                r.append(e.dma_start(out=wb_o[s, k * 128:(k + 1) * 128, :], in_=w_o[l, k * 128:(k + 1) * 128, :]))
            for k in range(4):
                r.append(e.dma_start(out=wb_ao[s, k * 128:(k + 1) * 128, :], in_=w_att_o[l, k * 128:(k + 1) * 128, :]))
            r.append(e.dma_start(out=wb_r[s], in_=w_router[l]))
            for ex in range(16):
                for k in range(4):
                    r.append(e.dma_start(out=wb_g[s, ex, k * 256:(k + 1) * 256, :], in_=w_gate[l, ex, k * 256:(k + 1) * 256, :]))
                    r.append(e.dma_start(out=wb_u[s, ex, k * 256:(k + 1) * 256, :], in_=w_up[l, ex, k * 256:(k + 1) * 256, :]))
                    r.append(e.dma_start(out=wb_d[s, ex, k * 128:(k + 1) * 128, :], in_=w_down[l, ex, k * 128:(k + 1) * 128, :]))
            return r
        fw.dma('pool', go, writes=[tk])

    cast_weights(0)
    with fw.phase("mod"):
        ct = fw.sb([128, 8], F32, "ct")
        cact = fw.sb([128, 8], F32, "cact")
        LD('sp', ct[:, :], cT, [ct])
        ACT(cact[:, :], ct[:, :], AF.Silu, [ct], [cact])
        brow = fw.sb([1, 6 * D], F32, "brow")
        row = fw.sb([1, 6 * D], F32, "row")
        wring = fw.ring(3, [128, 8, 512], F32, "wmod")
        i = 0
        for l in range(L):
            LD('sp', brow[0:1, :], b_mod[l:l + 1, :], [brow])
            for nb in range(12):
                wbuf = wring[i % 3]
                P = ps[i % 2]
                i += 1
                LD('sp', wbuf[:, :, :], w_mod[l].rearrange("(kc p) n -> p kc n", p=128)[:, :, nb * 512:(nb + 1) * 512], [wbuf])
                for kc in range(8):
                    MM(P[0:1, :], cact[:, kc:kc + 1], wbuf[:, kc, :], kc == 0, kc == 7, [cact, wbuf], [P], inc=(kc == 7))
                TT('dve', row[0:1, nb * 512:(nb + 1) * 512], P[0:1, :], brow[0:1, nb * 512:(nb + 1) * 512], ALU.add, [P, brow], [row])
            ST('sp', mod_row[l:l + 1, :], row[0:1, :], [row])

    def ln_phase(l, x_src, y_src, x_dst, gt_idx, g_ap, b_ap, sc_idx, sh_idx, lp=None):
        lp = l if lp is None else lp
        with fw.phase("ln"):
            post = y_src is not None
            pre = sc_idx is not None
            if post:
                gtb = fw.sb([128, D], F32, "gtb")
                gb = fw.sb([128, D], F32, "gb")
                bb = fw.sb([128, D], F32, "bb")
                LD('sp', gtb[:, :], mod_row[l, gt_idx * D:(gt_idx + 1) * D].partition_broadcast(128), [gtb])
                LD('sp', gb[:, :], g_ap.partition_broadcast(128), [gb])
                LD('sp', bb[:, :], b_ap.partition_broadcast(128), [bb])
                TS('dve', gtb[:, :], gtb[:, :], 1.0, None, ALU.add, None, [gtb], [gtb])
            if pre:
                scp = fw.sb([128, 8], F32, "scp")
                shp = fw.sb([128, 8], F32, "shp")
                LD('sp', scp[:, :], mod_row[lp, sc_idx * D:(sc_idx + 1) * D].rearrange("(c p) -> p c", p=128), [scp], **NC_OK)
                LD('sp', shp[:, :], mod_row[lp, sh_idx * D:(sh_idx + 1) * D].rearrange("(c p) -> p c", p=128), [shp], **NC_OK)
                TS('dve', scp[:, :], scp[:, :], 1.0, None, ALU.add, None, [scp], [scp])
            xr = fw.ring(4, [128, D], F32, "xt")
            yr = fw.ring(4, [128, D], F32, "yt") if post else None
            ur = fw.ring(2, [128, D], F32, "u") if post else None
            xnr = fw.ring(3, [128, D], F32, "xnew") if post else None
            sm1 = fw.ring(3, [128, 16], F32, "sm1")
            sm2 = fw.ring(3, [128, 16], F32, "sm2")
            xbr = fw.ring(3, [128, D], BF16, "xnb") if pre else None
            hr = fw.ring(3, [128, 8, 128], BF16, "hTt") if pre else None

            def loads(t):
                LD('sp', xr[t % 4][:, :], x_src[t * 128:(t + 1) * 128, :], [xr[t % 4]])
                if post:
                    LD('sp', yr[t % 4][:, :], y_src[t * 128:(t + 1) * 128, :], [yr[t % 4]])

            def stats(src, srcb, sm):
                for hh in range(2):
                    fw.op('dve', lambda e: e.bn_stats(out=sm[:, 6 * hh:6 * hh + 6], in_=src[:, hh * 512:(hh + 1) * 512]), reads=[srcb], writes=[sm])
                fw.op('dve', lambda e: e.bn_aggr(out=sm[:, 12:14], in_=sm[:, 0:12]), reads=[sm], writes=[sm])
                ACT(sm[:, 14:15], sm[:, 13:14], AF.Ln, [sm], [sm], bias=EPS)
                ACT(sm[:, 14:15], sm[:, 14:15], AF.Exp, [sm], [sm], scale=-0.5)
                STT('dve', sm[:, 15:16], sm[:, 12:13], -1.0, sm[:, 14:15], ALU.mult, ALU.mult, [sm], [sm])

            def S1(t):
                if not post:
                    return
                xt, yt, u, xn, sm = xr[t % 4], yr[t % 4], ur[t % 2], xnr[t % 3], sm1[t % 3]
                TT('pool', u[:, :], yt[:, :], gtb[:, :], ALU.mult, [yt, gtb], [u])
                STT('dve', u[:, :], xt[:, :], ALPHA, u[:, :], ALU.mult, ALU.add, [xt, u], [u])
                stats(u[:, :], u, sm)
                ACT(xn[:, :], u[:, :], AF.Identity, [u, sm], [xn], bias=sm[:, 15:16], scale=sm[:, 14:15])

            def S2(t):
                if post:
                    xn = xnr[t % 3]
                    TT('pool', xn[:, :], xn[:, :], gb[:, :], ALU.mult, [xn, gb], [xn])
                    TT('dve', xn[:, :], xn[:, :], bb[:, :], ALU.add, [xn, bb], [xn])
                    ST('sp', x_dst[t * 128:(t + 1) * 128, :], xn[:, :], [xn])
                    cur = xn
                else:
                    cur = xr[t % 4]
                if pre:
                    xb, sm = xbr[t % 3], sm2[t % 3]
                    stats(cur[:, :], cur, sm)
                    ACT(xb[:, :], cur[:, :], AF.Identity, [cur, sm], [xb], bias=sm[:, 15:16], scale=sm[:, 14:15])

            def S3(t):
                if not pre:
                    return
                xb, ht = xbr[t % 3], hr[t % 3]
                P = ps[t % 2]
                for c in range(8):
                    TR(psbf(t % 2)[:, c * 128:(c + 1) * 128], xb[:, c * 128:(c + 1) * 128], [xb], [P], inc=(c == 7))
                for c in range(8):
                    ACT(ht[:, c, :], psbf(t % 2)[:, c * 128:(c + 1) * 128], AF.Identity, [P, scp, shp], [ht], bias=shp[:, c:c + 1], scale=scp[:, c:c + 1])
                ST('sp', hT_d.rearrange("c p s -> p c s")[:, :, t * 128:(t + 1) * 128], ht[:, :, :], [ht])

            loads(0)
            if NT > 1:
                loads(1)
            for step in range(NT + 2):
                if step + 2 < NT:
                    loads(step + 2)
                if step < NT:
                    S1(step)
                if 0 <= step - 1 < NT:
                    S2(step - 1)
                if 0 <= step - 2 < NT:
                    S3(step - 2)

    def win_phase(l):
        s = l % 2
        wsrc = wb_in[s].rearrange("(kc p) n -> p kc n", p=128)
        with fw.phase("win"):
            hT = fw.sb([128, 8, S], BF16, "hT")
            fw.dma('sp', lambda e: [e.dma_start(out=hT[:, c, :], in_=hT_d[c]) for c in range(8)], writes=[hT])
            wA = fw.sb([128, 8, 1560], BF16, "wA")
            LD('sp', wA[:, :, 0:1024], wsrc[:, :, OZ:OZ + 1024], [wA], R=[wtok[l]])
            LD('sp', wA[:, :, 1024:1536], wsrc[:, :, OV:OV + 512], [wA])
            LD('pool', wA[:, :, 1536:1560], w_small[l].rearrange("(kc p) n -> p kc n", p=128), [wA])
            cw = fw.sb([128, 16, 4], F32, "cw")
            cb = fw.sb([128, 16], F32, "cb")
            LD('sp', cw[:, :, :], conv_wT[l], [cw])
            LD('sp', cb[:, :], conv_bT[l], [cb])
            zr = fw.ring(2, [128, D], BF16, "zt")
            vr = fw.ring(2, [128, 8, 128], BF16, "vt")
            sr = fw.ring(2, [128, 24], F32, "smt")
            for v in vr:
                MEMSET('pool', v[:, :, :], 1.0, [v])
            for t in range(NT):
                zt, vt, st = zr[t % 2], vr[t % 2], sr[t % 2]
                tok = slice(t * 128, (t + 1) * 128)
                for cbk in range(2):
                    P = ps[cbk]
                    for kc in range(8):
                        MM(P[:, :], hT[:, kc, tok], wA[:, kc, cbk * 512:(cbk + 1) * 512], kc == 0, kc == 7, [hT, wA], [P], inc=(kc == 7))
                    ACT(zt[:, cbk * 512:(cbk + 1) * 512], P[:, :], AF.Silu, [P], [zt])
                ST('sp', zs_d[tok, :], zt[:, :], [zt])
                P = ps[2]
                for kc in range(8):
                    MM(P[:, :], hT[:, kc, tok], wA[:, kc, 1024:1536], kc == 0, kc == 7, [hT, wA], [P], inc=(kc == 7))
                CP('dve', vt[:, :, 0:64], P[:, :].rearrange("p (h d) -> p h d", h=8), [P], [vt])
                ST('sp', va_d[tok, :, :], vt[:, :, :], [vt])
                P = ps[3]
                for kc in range(8):
                    MM(P[:, 0:24], hT[:, kc, tok], wA[:, kc, 1536:1560], kc == 0, kc == 7, [hT, wA], [P], inc=(kc == 7))
                CP('dve', st[:, :], P[:, 0:24], [P], [st])
                ST('sp', small_d[tok, :], st[:, :], [st])
            chunks = [('xbc', m, OXBC + 128 * m) for m in range(16)]
            chunks += [('q', j, OQ + 128 * j) for j in range(4)] + [('k', j, OK_ + 128 * j) for j in range(4)]
            chunks += [('g', j, OGS + 128 * j) for j in range(16)]
            wBr = fw.ring(3, [128, 8, 128], BF16, "wB")
            rawr = fw.ring(2, [128, 520], F32, "raw")
            accr = fw.ring(2, [128, 512], F32, "acc")
            otr = fw.ring(3, [128, 512], BF16, "ot")
            xtr = fw.ring(2, [128, 4, 128], BF16, "xtt")
            k8 = fw.sb([128, 512], BF16, "k8")
            MEMSET('pool', k8[:, :], 1.0, [k8])
            for h in range(8):
                for nb in range(NB):
                    ST('sp', kT_d[h, 64:66, nb * 512:(nb + 1) * 512], k8[0:2, :], [k8])
            it = 0

            def ldw(ci):
                LD('sp', wBr[ci % 3][:, :, :], wsrc[:, :, chunks[ci][2]:chunks[ci][2] + 128], [wBr[ci % 3]])
            ldw(0)
            ldw(1)
            for ci, (kind, m, col) in enumerate(chunks):
                if ci + 2 < len(chunks):
                    ldw(ci + 2)
                wB = wBr[ci % 3]
                for nb in range(NB):
                    cols = slice(nb * 512, (nb + 1) * 512)
                    P = ps[4 + it % 2]
                    ot = otr[it % 3]
                    for kc in range(8):
                        MM(P[:, :], wB[:, kc, :], hT[:, kc, cols], kc == 0, kc == 7, [wB, hT], [P], inc=(kc == 7))
                    if kind == 'xbc':
                        raw, acc = rawr[it % 2], accr[it % 2]
                        praw = rawr[(it + 1) % 2]
                        CP('act', raw[:, 3:515], P[:, :], [P], [raw])
                        if nb == 0:
                            MEMSET('pool', raw[:, 0:3], 0.0, [raw])
                        else:
                            CP('pool', raw[:, 0:3], praw[:, 512:515], [praw], [raw])
                        ACT(acc[:, :], P[:, :], AF.Identity, [P, cw, cb], [acc], bias=cb[:, m:m + 1], scale=cw[:, m, 3:4])
                        STT('dve', acc[:, :], raw[:, 2:514], cw[:, m, 2:3], acc[:, :], ALU.mult, ALU.add, [raw, cw, acc], [acc])
                        STT('dve', acc[:, :], raw[:, 1:513], cw[:, m, 1:2], acc[:, :], ALU.mult, ALU.add, [raw, cw, acc], [acc])
                        STT('dve', acc[:, :], raw[:, 0:512], cw[:, m, 0:1], acc[:, :], ALU.mult, ALU.add, [raw, cw, acc], [acc])
                        ACT(ot[:, :], acc[:, :], AF.Silu, [acc], [ot])
                        if m >= 8:
                            g = (m - 8) % 4
                            dst = BT_d if m < 12 else CT_d
                            ST('sp', dst[g, :, cols], ot[:, :], [ot])
                        if m < 12:
                            xt = xtr[it % 2]
                            PT = ps[6 + it % 2]
                            for j in range(4):
                                TR(psbf(6 + it % 2)[:, j * 128:(j + 1) * 128], ot[:, j * 128:(j + 1) * 128], [ot], [PT], inc=(j == 3))
                            CP('dve', xt[:, :, :], psbf(6 + it % 2)[:, 0:512].rearrange("p (j c) -> p j c", j=4), [PT], [xt])
                            if m < 8:
                                dd = xs_d[cols, m * 128:(m + 1) * 128]
                            else:
                                dd = Btm_d[cols, (m - 8) * 128:(m - 7) * 128]
                            ST('sp', dd.rearrange("(j p) c -> p j c", p=128), xt[:, :, :], [xt])
                    elif kind in ('q', 'k'):
                        CP('dve', ot[:, :], P[:, :], [P], [ot])
                        dst = qT_d if kind == 'q' else kT_d
                        ST('sp', dst[2 * m, 0:64, cols], ot[0:64, :], [ot])
                        ST('sp', dst[2 * m + 1, 0:64, cols], ot[64:128, :], [ot])
                    else:
                        ACT(ot[:, :], P[:, :], AF.Sigmoid, [P], [ot])
                        ST('sp', sgT_d[m, :, cols], ot[:, :], [ot])
                    it += 1

    def attprep_phase(l):
        with fw.phase("attprep"):
            fr = fw.sb([128, NT, 8], F32, "fr")
            LD('sp', fr[:, :, :], small_d[:, 16:24].rearrange("(b p) h -> p b h", p=128), [fr])
            fbb = fw.sb([128, 8], F32, "fbb")
            LD('sp', fbb[:, :], forget_b[l].partition_broadcast(128), [fbb])
            lf = fw.sb([128, 8, NT], F32, "lf")
            TT('dve', lf[:, :, :], fr[:, :, :].rearrange("p b h -> p h b"), fbb[:, :].unsqueeze(2).to_broadcast([128, 8, NT]), ALU.add, [fr, fbb], [lf])
            ACT(lf[:, :, :], lf[:, :, :], AF.Exp, [lf], [lf], scale=-1.0)
            ACT(lf[:, :, :], lf[:, :, :], AF.Ln, [lf], [lf], bias=1.0)
            TS('dve', lf[:, :, :], lf[:, :, :], -1.0, None, ALU.mult, None, [lf], [lf])
            lf2 = lf[:, :, :].rearrange("p h b -> p (h b)")
            NHB = 8 * NT
            P = ps[0]
            MM(P[:, 0:NHB], Uinc, lf2, True, True, [cst, lf], [P])
            P1 = ps[1]
            MM(P1[:, 0:NHB], ones, lf2, True, True, [cst, lf], [P1])
            W = fw.sb([128, 8, NT], F32, "W")
            T = fw.sb([128, 8, NT], F32, "T")
            CP('dve', W[:, :, :].rearrange("p h b -> p (h b)"), P[:, 0:NHB], [P], [W])
            CP('dve', T[:, :, :].rearrange("p h b -> p (h b)"), P1[:, 0:NHB], [P1], [T])
            Pf = fw.sb([128, 8, NT], F32, "Pf")
            MEMSET('dve', Pf[:, :, 0:1], 0.0, [Pf])
            for b in range(1, NT):
                TT('dve', Pf[:, :, b:b + 1], Pf[:, :, b - 1:b], T[:, :, b - 1:b], ALU.add, [Pf, T], [Pf])
            Fm = fw.sb([128, 8, NT], F32, "Fm")
            TT('dve', Fm[:, :, :], W[:, :, :], Pf[:, :, :], ALU.add, [W, Pf], [Fm])
            bt = fw.sb([128, 8, NB, NT], F32, "bt")
            for h in range(8):
                TT('dve', bt[:, h, :, :], Pf[:, h, :].rearrange("p (j r) -> p j r", r=4)[:, :, 0:1].to_broadcast([128, NB, NT]),
                   Fm[:, h, :].unsqueeze(1).to_broadcast([128, NB, NT]), ALU.subtract, [Pf, Fm], [bt])
                ST('sp', fbias_d[h], bt[:, h, :, :].rearrange("p j i -> p (j i)"), [bt])
            fl = fw.sb([128, 8, NT], F32, "fl")
            for h in range(8):
                TT('dve', fl[:, h, :].rearrange("p (j r) -> p j r", r=4), Fm[:, h, :].rearrange("p (j r) -> p j r", r=4),
                   Pf[:, h, :].rearrange("p (j r) -> p j r", r=4)[:, :, 0:1].to_broadcast([128, NB, 4]), ALU.subtract, [Fm, Pf], [fl])
            TS('dve', fl[:, :, :], fl[:, :, :], 8.0, None, ALU.mult, None, [fl], [fl])
            hi = fw.sb([128, 8, NT], BF16, "hi")
            hi32 = fw.sb([128, 8, NT], F32, "hi32")
            lo = fw.sb([128, 8, NT], BF16, "lo")
            CP('dve', hi[:, :, :], fl[:, :, :], [fl], [hi])
            CP('dve', hi32[:, :, :], hi[:, :, :], [hi], [hi32])
            TT('dve', lo[:, :, :], fl[:, :, :], hi32[:, :, :], ALU.subtract, [fl, hi32], [lo])
            rows = fw.sb([128, 2, 2, 128], BF16, "rows")
            HB = 128 // NT
            for wi, src in enumerate((hi, lo)):
                s2 = src[:, :, :].rearrange("p h b -> p (h b)")
                nhalf = (8 * NT + 127) // 128
                for hf in range(nhalf):
                    n = min(128, 8 * NT - hf * 128)
                    PT = ps[2 + (wi * 2 + hf) % 2]
                    TR(psbf(2 + (wi * 2 + hf) % 2)[0:n, 0:128], s2[:, hf * 128:hf * 128 + n], [src], [PT])
                    CP('dve', rows[0:n, wi, hf, :], psbf(2 + (wi * 2 + hf) % 2)[0:n, 0:128], [PT], [rows])
            for h in range(8):
                for wi in range(2):
                    g = h * NT
                    hf, p0 = g // 128, g % 128
                    ST('sp', qT_d[h, 64 + wi, :].rearrange("(b p) -> b p", p=128), rows[p0:p0 + NT, wi, hf, :], [rows])

    def att_phase(l):
        with fw.phase("att"):
            qr = fw.ring(2, [66, S], BF16, "qa")
            kr = fw.ring(2, [66, S], BF16, "ka")
            vr = fw.ring(2, [128, NT, 128], BF16, "va")
            br = fw.ring(2, [128, NB * NT], F32, "fb")
            pr = fw.ring(4, [128, 512], BF16, "pt")
            mr = fw.ring(2, [128, 128], F32, "msk")
            rr = fw.ring(2, [64, 512], F32, "rs")
            orr = fw.ring(2, [64, 512], BF16, "on")
            negm = fw.sb([128, 128], F32, "negm")
            TS('dve', negm[:, :], Lstr, -30000.0, None, ALU.mult, None, [cst], [negm])

            def loads(h):
                LD('sp', qr[h % 2][:, :], qT_d[h], [qr[h % 2]])
                LD('sp', kr[h % 2][:, :], kT_d[h], [kr[h % 2]])
                LD('sp', vr[h % 2][:, :, :], va_d[:, h, :].rearrange("(b p) d -> p b d", p=128), [vr[h % 2]])
                LD('sp', br[h % 2][:, :], fbias_d[h], [br[h % 2]])
            loads(0)
            items = [(h, j, i) for h in range(8) for j in range(NB) for i in range(4 * j + 4)]
            LA = 2

            def s_part(n_, h, j, i):
                qa, ka, fb = qr[h % 2], kr[h % 2], br[h % 2]
                r = i - 4 * j
                c0 = 128 * r if r > 0 else 0
                n = 512 - c0
                PS_ = ps[n_ % 4]
                pt = pr[n_ % 4]
                bias = fb[:, j * NT + i:j * NT + i + 1]
                MM(PS_[:, 0:n], ka[:, i * 128:(i + 1) * 128], qa[:, j * 512 + c0:(j + 1) * 512], True, True, [ka, qa], [PS_])
                if r >= 0:
                    mk = mr[n_ % 2]
                    TT('dve', mk[:, :], PS_[:, 0:128], negm[:, :], ALU.add, [PS_, negm], [mk])
                    ACT(pt[:, 0:128], mk[:, :], AF.Exp, [mk, fb], [pt], bias=bias, scale=0.125)
                    if n > 128:
                        ACT(pt[:, 128:n], PS_[:, 128:n], AF.Exp, [PS_, fb], [pt], bias=bias, scale=0.125)
                else:
                    ACT(pt[:, 0:n], PS_[:, 0:n], AF.Exp, [PS_, fb], [pt], bias=bias, scale=0.125)

            def pv_part(n_, h, j, i):
                if j == 0 and i == 0 and h + 1 < 8:
                    loads(h + 1)
                va = vr[h % 2]
                g_ = h * NB + j
                r = i - 4 * j
                c0 = 128 * r if r > 0 else 0
                n = 512 - c0
                nk = 4 * j + 4
                PO = ps[6 + g_ % 2]
                pt = pr[n_ % 4]
                MM(PO[:, c0:512], va[:, i, :], pt[:, 0:n], i == 0, i == nk - 1, [va, pt], [PO], inc=(i == nk - 1))
                if i == nk - 1:
                    rs = rr[g_ % 2]
                    on = orr[g_ % 2]
                    CP('dve', rs[:, :], PO[64:128, :], [PO], [rs])
                    fw.op('dve', lambda e: e.reciprocal(out=rs[:, :], in_=rs[:, :]), reads=[rs], writes=[rs])
                    TT('dve', on[:, :], PO[0:64, :], rs[:, :], ALU.mult, [PO, rs], [on])
                    ST('sp', oT_d[h // 2, (h % 2) * 64:(h % 2) * 64 + 64, j * 512:(j + 1) * 512], on[:, :], [on])

            for n_ in range(len(items) + LA):
                if n_ < len(items):
                    s_part(n_, *items[n_])
                if n_ - LA >= 0:
                    pv_part(n_ - LA, *items[n_ - LA])

    def ssd_phase(l):
        with fw.phase("ssd"):
            dtb = fw.sb([128, 16], F32, "dtb")
            Ab = fw.sb([128, 16], F32, "Ab")
            dsk = fw.sb([128, 16], F32, "dsk")
            LD('sp', dtb[:, :], dt_bias[l].partition_broadcast(128), [dtb])
            LD('sp', Ab[:, :], a_log[l].partition_broadcast(128), [Ab])
            LD('sp', dsk[:, :], d_skip[l].partition_broadcast(128), [dsk])
            ACT(Ab[:, :], Ab[:, :], AF.Exp, [Ab], [Ab])
            TS('dve', Ab[:, :], Ab[:, :], -1.0, None, ALU.mult, None, [Ab], [Ab])
            st32 = fw.sb([128, D], F32, "st32")
            stb = fw.sb([128, D], BF16, "stb")
            MEMSET('dve', st32[:, :], 0.0, [st32])
            MEMSET('dve', stb[:, :], 0.0, [stb])
            xsr = fw.ring(3, [128, D], BF16, "xs")
            zsr = fw.ring(3, [128, D], BF16, "zs")
            btr = fw.ring(3, [128, 512], BF16, "btm")
            bTr = fw.ring(3, [128, 4, 128], BF16, "bT")
            cTr = fw.ring(3, [128, 4, 128], BF16, "cT")
            dtr = fw.ring(3, [128, 16], F32, "dtraw")
            smr = fw.ring(3, [128, 160], F32, "ssm")
            xdr = fw.ring(3, [128, D], BF16, "xdt")
            xer = fw.ring(3, [128, D], BF16, "xdte")
            AMr = fw.ring(2, [128, 16, 128], F32, "AM")
            Dmr = fw.ring(2, [128, 16, 128], F32, "Dm")
            cbr = fw.ring(2, [128, 4, 128], F32, "cbm")
            Mtr = fw.ring(3, [128, 16, 128], BF16, "Mt")
            t1r = fw.ring(2, [128, D], F32, "t1")
            t2r = fw.ring(2, [128, D], F32, "t2")
            gnr = fw.ring(2, [128, D], BF16, "gn")
            gTr = fw.ring(2, [128, 8, 128], BF16, "gT")
            junk = fw.sb([128, D], BF16, "junk")

            def loads(c):
                tok = slice(c * 128, (c + 1) * 128)
                k = c % 3
                LD('sp', xsr[k][:, :], xs_d[tok, :], [xsr[k]])
                LD('sp', zsr[k][:, :], zs_d[tok, :], [zsr[k]])
                LD('sp', btr[k][:, :], Btm_d[tok, :], [btr[k]])
                LD('sp', bTr[k][:, :, :], BT_d.rearrange("g p s -> p g s")[:, :, tok], [bTr[k]])
                LD('sp', cTr[k][:, :, :], CT_d.rearrange("g p s -> p g s")[:, :, tok], [cTr[k]])
                LD('sp', dtr[k][:, :], small_d[tok, 0:16], [dtr[k]])
            def SA(c):
                    k = c % 3
                    tok = slice(c * 128, (c + 1) * 128)
                    xs, zs, btm, bT, cTt, dtraw, sm = xsr[k], zsr[k], btr[k], bTr[k], cTr[k], dtr[k], smr[k]
                    xdt, xdte, AM, Dm, cbm, Mt, t1, t2, gn, gT = xdr[k], xer[k], AMr[c % 2], Dmr[c % 2], cbr[c % 2], Mtr[k], t1r[c % 2], t2r[c % 2], gnr[c % 2], gTr[c % 2]
                    dt_, a_, acs, tot, ea, te, cd, dte = [sm[:, 16 * i:16 * i + 16] for i in range(8)]
                    xs3 = xs[:, :].rearrange("p (h d) -> p h d", h=16)
                    TT('dve', dt_, dtraw[:, :], dtb[:, :], ALU.add, [dtraw, dtb], [sm])
                    ACT(dt_, dt_, AF.Exp, [sm], [sm])
                    ACT(dt_, dt_, AF.Ln, [sm], [sm], bias=1.0)
                    TT('dve', a_, dt_, Ab[:, :], ALU.mult, [sm, Ab], [sm])
                    P0 = ps[0]
                    MM(P0[:, 0:16], Uinc, a_, True, True, [cst, sm], [P0], inc=False)
                    MM(P0[:, 16:32], ones, a_, True, True, [cst, sm], [P0])
                    CP('dve', sm[:, 32:64], P0[:, 0:32], [P0], [sm])
                    ACT(ea, acs, AF.Exp, [sm], [sm])
                    TT('dve', te, tot, acs, ALU.subtract, [sm], [sm])
                    ACT(te, te, AF.Exp, [sm], [sm])
                    ACT(cd, tot, AF.Exp, [sm], [sm])
                    TT('dve', dte, dt_, te, ALU.mult, [sm], [sm])
                    TT('dve', xdt[:, :].rearrange("p (h d) -> p h d", h=16), xs3, dt_.unsqueeze(2).to_broadcast([128, 16, 64]), ALU.mult, [xs, sm], [xdt])
                    TT('pool', xdte[:, :].rearrange("p (h d) -> p h d", h=16), xs3, dte.unsqueeze(2).to_broadcast([128, 16, 64]), ALU.mult, [xs, sm], [xdte])
                    TT('pool', AM[:, :, :], Lstr.unsqueeze(1).to_broadcast([128, 16, 128]), a_.unsqueeze(2).to_broadcast([128, 16, 128]), ALU.mult, [cst, sm], [AM])
                    for half_ in range(2):
                        for hh in range(8 * half_, 8 * half_ + 8):
                            PSg = ps[1 + (hh % 8) // 4]
                            MM(PSg[:, (hh % 4) * 128:(hh % 4 + 1) * 128], AM[:, hh, :], Uinc, True, True, [AM, cst], [PSg], inc=(hh % 4 == 3))
                        for q4 in range(2):
                            hq = 8 * half_ + 4 * q4
                            ACT(Dm[:, hq:hq + 4, :].rearrange("p h s -> p (h s)"), ps[1 + q4][:, :], AF.Exp, [ps[1 + q4]], [Dm])
                    for g in range(4):
                        MM(P0[:, g * 128:(g + 1) * 128], bT[:, g, :], cTt[:, g, :], True, True, [bT, cTt], [P0], inc=(g == 3))
                    TT('dve', cbm[:, :, :], P0[:, :].rearrange("p (g s) -> p g s", g=4), Uinc.unsqueeze(1).to_broadcast([128, 4, 128]), ALU.mult, [P0, cst], [cbm])
                    TT('dve', Mt[:, :, :].rearrange("p (g r) s -> p g r s", g=4), Dm[:, :, :].rearrange("p (g r) s -> p g r s", g=4),
                       cbm[:, :, :].unsqueeze(2).to_broadcast([128, 4, 4, 128]), ALU.mult, [Dm, cbm], [Mt])

            def SB(c):
                    k = c % 3
                    tok = slice(c * 128, (c + 1) * 128)
                    xs, zs, btm, bT, cTt, dtraw, sm = xsr[k], zsr[k], btr[k], bTr[k], cTr[k], dtr[k], smr[k]
                    xdt, xdte, AM, Dm, cbm, Mt, t1, t2, gn, gT = xdr[k], xer[k], AMr[c % 2], Dmr[c % 2], cbr[c % 2], Mtr[k], t1r[c % 2], t2r[c % 2], gnr[c % 2], gTr[c % 2]
                    dt_, a_, acs, tot, ea, te, cd, dte = [sm[:, 16 * i:16 * i + 16] for i in range(8)]
                    xs3 = xs[:, :].rearrange("p (h d) -> p h d", h=16)
                    for hh in range(16):
                        PY = ps[5 + hh // 8]
                        MM(PY[:, (hh % 8) * 64:(hh % 8 + 1) * 64], Mt[:, hh, :], xdt[:, hh * 64:(hh + 1) * 64], True, True, [Mt, xdt], [PY], inc=(hh % 8 == 7))
                    for g in range(4):
                        PF = ps[3 + g // 2]
                        MM(PF[:, (g % 2) * 256:(g % 2 + 1) * 256], cTt[:, g, :], stb[:, g * 256:(g + 1) * 256], True, True, [cTt, stb], [PF], inc=(g % 2 == 1))
                    pyoff = psall[:, 1536:2560].rearrange("p (h d) -> p h d", h=16)
                    pydia = psall[:, 2560:3584]
                    TT('dve', t1[:, :].rearrange("p (h d) -> p h d", h=16), pyoff, ea.unsqueeze(2).to_broadcast([128, 16, 64]), ALU.mult, [ps[3], ps[4], sm], [t1])
                    for g in range(4):
                        PSt = ps[3 + g // 2]
                        MM(PSt[:, (g % 2) * 256:(g % 2 + 1) * 256], btm[:, g * 128:(g + 1) * 128], xdte[:, g * 256:(g + 1) * 256], True, True, [btm, xdte], [PSt], inc=(g % 2 == 1))
                    TT('pool', t2[:, :].rearrange("p (h d) -> p h d", h=16), xs3, dsk[:, :].unsqueeze(2).to_broadcast([128, 16, 64]), ALU.mult, [xs, dsk], [t2])
                    TT('dve', t1[:, :], t1[:, :], pydia, ALU.add, [t1, ps[5], ps[6]], [t1])
                    TT('pool', t1[:, :], t1[:, :], t2[:, :], ALU.add, [t1, t2], [t1])
                    TT('pool', t1[:, :], t1[:, :], zs[:, :], ALU.mult, [t1, zs], [t1])
                    MEMSET('dve', sm[:, 128:129], 0.0, [sm])
                    ACT(junk[:, :], t1[:, :], AF.Square, [t1], [junk, sm], accum=sm[:, 128:129])
                    TS('dve', sm[:, 129:130], sm[:, 128:129], 1.0 / D, EPS, ALU.mult, ALU.add, [sm], [sm])
                    ACT(sm[:, 129:130], sm[:, 129:130], AF.Ln, [sm], [sm])
                    ACT(sm[:, 129:130], sm[:, 129:130], AF.Exp, [sm], [sm], scale=-0.5)
                    ACT(gn[:, :], t1[:, :], AF.Copy, [t1, sm], [gn], scale=sm[:, 129:130])
                    PT = ps[7]
                    for cc in range(8):
                        TR(psbf(7)[:, cc * 128:(cc + 1) * 128], gn[:, cc * 128:(cc + 1) * 128], [gn], [PT], inc=(cc == 7))
                    CP('act', gT[:, :, :].rearrange("p c s -> p (c s)"), psbf(7)[:, :], [PT], [gT])
                    ST('sp', gnT_d.rearrange("c p s -> p c s")[:, :, tok], gT[:, :, :], [gT])
                    TT('pool', st32[:, :].rearrange("p (h d) -> p h d", h=16), st32[:, :].rearrange("p (h d) -> p h d", h=16),
                       cd.unsqueeze(2).to_broadcast([128, 16, 64]), ALU.mult, [st32, sm], [st32])
                    TT('dve', st32[:, :], st32[:, :], psall[:, 1536:2560], ALU.add, [st32, ps[3], ps[4], stb], [st32])
                    CP('act', stb[:, :], st32[:, :], [st32], [stb])


            loads(0)
            if NT > 1:
                loads(1)
            for step in range(NT + 1):
                if step < NT:
                    SA(step)
                if step - 1 >= 0:
                    SB(step - 1)
                if step + 2 < NT:
                    loads(step + 2)

    def out_phase(l):
        s = l % 2
        with fw.phase("out"):
            wso = fw.sb([128, 8, D], BF16, "wso")
            wao = fw.sb([128, 4, D], BF16, "wao")
            wo = fw.sb([128, 8, D], BF16, "wo")
            nw = fw.sb([128, 8], F32, "nw")
            LD('sp', wso[:, :, :], wb_so[s].rearrange("(kc p) n -> p kc n", p=128), [wso], R=[wtok[l]])
            LD('sp', wao[:, :, :], wb_ao[s].rearrange("(kc p) n -> p kc n", p=128), [wao])
            LD('sp', wo[:, :, :], wb_o[s].rearrange("(kc p) n -> p kc n", p=128), [wo])
            LD('sp', nw[:, :], ssd_norm_wT[l], [nw])
            for kc in range(8):
                TS('dve' if kc % 2 else 'pool', wso[:, kc, :], wso[:, kc, :], nw[:, kc:kc + 1], None, ALU.mult, None, [wso, nw], [wso])
            gr = fw.ring(2, [128, 8, 512], BF16, "gTi")
            orr = fw.ring(2, [128, 4, 512], BF16, "oTi")
            sr = fw.ring(2, [128, 16, 512], BF16, "sgi")
            mr = fw.ring(2, [128, 8, 512], BF16, "mT")
            t1r = fw.ring(2, [128, 512], F32, "ot1")
            t2r = fw.ring(2, [128, 512], F32, "ot2")
            yr = fw.ring(2, [128, D], F32, "yo")

            def loads(nb):
                cols = slice(nb * 512, (nb + 1) * 512)
                LD('sp', gr[nb % 2][:, :, :], gnT_d.rearrange("c p s -> p c s")[:, :, cols], [gr[nb % 2]])
                LD('sp', orr[nb % 2][:, :, :], oT_d.rearrange("c p s -> p c s")[:, :, cols], [orr[nb % 2]])
                LD('sp', sr[nb % 2][:, :, :], sgT_d.rearrange("c p s -> p c s")[:, :, cols], [sr[nb % 2]])
            loads(0)
            it = 0
            for nb in range(NB):
                if nb + 1 < NB:
                    loads(nb + 1)
                gT, oT, sg, mT = gr[nb % 2], orr[nb % 2], sr[nb % 2], mr[nb % 2]
                for m in range(8):
                    P1, P2 = ps[(it % 2) * 2], ps[(it % 2) * 2 + 1]
                    t1, t2 = t1r[it % 2], t2r[it % 2]
                    it += 1
                    for kc in range(8):
                        MM(P1[:, :], wso[:, kc, m * 128:(m + 1) * 128], gT[:, kc, :], kc == 0, kc == 7, [wso, gT], [P1], inc=(kc == 7))
                    for kc in range(4):
                        MM(P2[:, :], wao[:, kc, m * 128:(m + 1) * 128], oT[:, kc, :], kc == 0, kc == 3, [wao, oT], [P2], inc=(kc == 3))
                    TT('dve', t1[:, :], P1[:, :], sg[:, m, :], ALU.mult, [P1, sg], [t1])
                    TT('dve', t2[:, :], P2[:, :], sg[:, 8 + m, :], ALU.mult, [P2, sg], [t2])
                    TT('pool', mT[:, m, :], t1[:, :], t2[:, :], ALU.add, [t1, t2], [mT])
                for j in range(4):
                    yo = yr[j % 2]
                    for hf in range(2):
                        P3 = ps[4 + (2 * j + hf) % 4]
                        for kc in range(8):
                            MM(P3[:, :], mT[:, kc, j * 128:(j + 1) * 128], wo[:, kc, hf * 512:(hf + 1) * 512], kc == 0, kc == 7, [mT, wo], [P3], inc=(kc == 7))
                        CP('act', yo[:, hf * 512:(hf + 1) * 512], P3[:, :], [P3], [yo])
                    ST('sp', y_d[(nb * 4 + j) * 128:(nb * 4 + j + 1) * 128, :], yo[:, :], [yo])

    def moe_phase(l):
        s = l % 2
        NH = 2 if S >= 2048 else 1
        SH = S // NH
        NTH = SH // 128
        NBH = SH // 512
        for half in range(NH):
            with fw.phase("moe"):
                hT = fw.sb([128, 8, SH], BF16, "h2T")
                fw.dma('sp', lambda e: [e.dma_start(out=hT[:, c, :], in_=hT_d[c, :, half * SH:(half + 1) * SH]) for c in range(8)], writes=[hT])
                wr = fw.sb([128, 8, 20], BF16, "wr")
                LD('sp', wr[:, :, :], wb_r[s].rearrange("(kc p) n -> p kc n", p=128), [wr], R=[wtok[l]])
                brt = fw.sb([128, 20], F32, "brt")
                LD('sp', brt[:, :], b_router[l].partition_broadcast(128), [brt])
                lg = fw.sb([128, NTH, 20], F32, "lg")
                for t in range(NTH):
                    P = ps[t % 2]
                    for kc in range(8):
                        MM(P[:, 0:20], hT[:, kc, t * 128:(t + 1) * 128], wr[:, kc, :], kc == 0, kc == 7, [hT, wr], [P], inc=(kc == 7))
                    TT('dve', lg[:, t, :], P[:, 0:20], brt[:, :], ALU.add, [P, brt], [lg])
                gl = lg[:, :, 0:4]
                el = lg[:, :, 4:20].rearrange("p t (g e) -> p t g e", g=4)
                R_ = fw.sb([128, NTH, 64], F32, "rt")
                gm = R_[:, :, 0:1]
                oh = R_[:, :, 4:8]
                ex = R_[:, :, 8:12]
                gs = R_[:, :, 12:13]
                ein = R_[:, :, 16:20]
                m1 = R_[:, :, 20:21]
                k1 = R_[:, :, 24:28]
                e2 = R_[:, :, 28:32]
                m2 = R_[:, :, 32:33]
                k2 = R_[:, :, 36:40]
                w1 = R_[:, :, 40:41]
                w2 = R_[:, :, 41:42]
                wg = R_[:, :, 44:48]
                tmp = R_[:, :, 48:64].rearrange("p t (g e) -> p t g e", g=4)
                RR = [R_, lg]
                fw.op('dve', lambda e: e.tensor_reduce(out=gm, in_=gl, axis=AX.X, op=ALU.max), reads=RR, writes=[R_])
                TT('dve', oh, gl, gm.to_broadcast([128, NTH, 4]), ALU.is_equal, RR, [R_])
                TT('dve', ex, gl, gm.to_broadcast([128, NTH, 4]), ALU.subtract, RR, [R_])
                ACT(ex, ex, AF.Exp, [R_], [R_])
                fw.op('dve', lambda e: e.tensor_reduce(out=gs, in_=ex, axis=AX.X, op=ALU.add), reads=RR, writes=[R_])
                fw.op('dve', lambda e: e.reciprocal(out=gs, in_=gs), reads=[R_], writes=[R_])
                TT('dve', tmp, el, oh.unsqueeze(3).to_broadcast([128, NTH, 4, 4]), ALU.mult, RR, [R_])
                fw.op('dve', lambda e: e.tensor_reduce(out=ein, in_=R_[:, :, 48:64].rearrange("p t (g e) -> p t e g", g=4), axis=AX.X, op=ALU.add), reads=RR, writes=[R_])
                fw.op('dve', lambda e: e.tensor_reduce(out=m1, in_=ein, axis=AX.X, op=ALU.max), reads=RR, writes=[R_])
                TT('dve', k1, ein, m1.to_broadcast([128, NTH, 4]), ALU.is_equal, RR, [R_])
                STT('dve', e2, k1, -1e30, ein, ALU.mult, ALU.add, RR, [R_])
                fw.op('dve', lambda e: e.tensor_reduce(out=m2, in_=e2, axis=AX.X, op=ALU.max), reads=RR, writes=[R_])
                TT('dve', k2, e2, m2.to_broadcast([128, NTH, 4]), ALU.is_equal, RR, [R_])
                TT('dve', w1, m2, m1, ALU.subtract, RR, [R_])
                ACT(w1, w1, AF.Exp, [R_], [R_])
                TS('dve', w1, w1, 1.0, None, ALU.add, None, RR, [R_])
                fw.op('dve', lambda e: e.reciprocal(out=w1, in_=w1), reads=[R_], writes=[R_])
                TS('dve', w2, w1, -1.0, 1.0, ALU.mult, ALU.add, RR, [R_])
                TT('dve', w1, w1, gs, ALU.mult, RR, [R_])
                TT('dve', w2, w2, gs, ALU.mult, RR, [R_])
                TT('dve', k1, k1, w1.to_broadcast([128, NTH, 4]), ALU.mult, RR, [R_])
                TT('dve', k2, k2, w2.to_broadcast([128, NTH, 4]), ALU.mult, RR, [R_])
                TT('dve', wg, k1, k2, ALU.add, RR, [R_])
                gates = fw.sb([128, NTH, 16], F32, "gates")
                TT('dve', gates[:, :, :].rearrange("p t (g e) -> p t g e", g=4), oh.unsqueeze(3).to_broadcast([128, NTH, 4, 4]),
                   wg.unsqueeze(2).to_broadcast([128, NTH, 4, 4]), ALU.mult, RR, [gates])
                acc = fw.sb([128, NTH, D], F32, "acc")
                wgr = fw.ring(2, [128, 8, 512], BF16, "wg")
                wur = fw.ring(2, [128, 8, 512], BF16, "wu")
                wdr = fw.ring(2, [128, 4, D], BF16, "wd")
                sgr = fw.ring(2, [128, 512], F32, "sgl")
                hdr = fw.ring(2, [128, 4, 512], BF16, "hid")

                def ldw(e_):
                    k = e_ % 2
                    LD('sp', wgr[k][:, :, :], wb_g[s, e_].rearrange("(kc p) n -> p kc n", p=128), [wgr[k]])
                    LD('sp', wur[k][:, :, :], wb_u[s, e_].rearrange("(kc p) n -> p kc n", p=128), [wur[k]])
                    LD('sp', wdr[k][:, :, :], wb_d[s, e_].rearrange("(kc p) n -> p kc n", p=128), [wdr[k]])
                ldw(0)
                it = 0
                for e_ in range(16):
                    if e_ + 1 < 16:
                        ldw(e_ + 1)
                    wgt, wut, wdt = wgr[e_ % 2], wur[e_ % 2], wdr[e_ % 2]
                    for nb in range(NBH):
                        cols = slice(nb * 512, (nb + 1) * 512)
                        hid = hdr[nb % 2]
                        for c in range(4):
                            PG, PU = ps[(it % 2) * 2], ps[(it % 2) * 2 + 1]
                            sgl = sgr[it % 2]
                            it += 1
                            for kc in range(8):
                                MM(PG[:, :], wgt[:, kc, c * 128:(c + 1) * 128], hT[:, kc, cols], kc == 0, kc == 7, [wgt, hT], [PG], inc=(kc == 7))
                            for kc in range(8):
                                MM(PU[:, :], wut[:, kc, c * 128:(c + 1) * 128], hT[:, kc, cols], kc == 0, kc == 7, [wut, hT], [PU], inc=(kc == 7))
                            ACT(sgl[:, :], PG[:, :], AF.Silu, [PG], [sgl])
                            TT('dve', hid[:, c, :], PU[:, :], sgl[:, :], ALU.mult, [PU, sgl], [hid])
                        for j in range(4):
                            t = nb * 4 + j
                            for hf in range(2):
                                PD = ps[4 + (2 * j + hf) % 4]
                                for c in range(4):
                                    MM(PD[:, :], hid[:, c, j * 128:(j + 1) * 128], wdt[:, c, hf * 512:(hf + 1) * 512], c == 0, c == 3, [hid, wdt], [PD], inc=(c == 3))
                                av = acc[:, t, hf * 512:(hf + 1) * 512]
                                if e_ == 0:
                                    TS('dve', av, PD[:, :], gates[:, t, e_:e_ + 1], None, ALU.mult, None, [PD, gates], [acc])
                                else:
                                    STT('dve', av, PD[:, :], gates[:, t, e_:e_ + 1], av, ALU.mult, ALU.add, [PD, gates, acc], [acc])
                for t in range(NTH):
                    ST('sp', y_d[half * SH + t * 128:half * SH + (t + 1) * 128, :], acc[:, t, :], [acc])

    stages = []
    for l in range(L):
        if l + 1 < L:
            cast_weights(l + 1)
        xs_src = x_in if l == 0 else xres_d
        if l == 0:
            ln_phase(l, x_in, None, None, None, None, None, 1, 0)
        win_phase(l)
        if stop_after == 'win':
            break
        attprep_phase(l)
        att_phase(l)
        if stop_after == 'att':
            break
        ssd_phase(l)
        if stop_after == 'ssd':
            break
        out_phase(l)
        if stop_after == 'out':
            break
        ln_phase(l, xs_src, y_d, xres_d, 2, ln1_g[l], ln1_b[l], 4, 3)
        if stop_after == 'ln1':
            break
        moe_phase(l)
        if stop_after == 'moe':
            break
        last = (l == L - 1)
        ln_phase(l, xres_d, y_d, out if last else xres_d, 5, ln2_g[l], ln2_b[l], None if last else 1, None if last else 0, lp=l + 1)
    fw.barrier()
    return nc, fw


def host_inputs(inputs, S=4096, L=4):
    f = lambda a: np.ascontiguousarray(np.asarray(a, dtype=np.float32))
    w_in = f(inputs['w_in'])[:L]
    ident = np.eye(128, dtype=np.float32)
    U = np.triu(np.ones((128, 128), np.float32))
    Ls = np.tril(np.ones((128, 128), np.float32), -1)
    consts = np.concatenate([ident, U, Ls, np.ones((128, 128), np.float32)], axis=1)
    shared = {
        'consts': f(consts),
        'w_mod': f(inputs['w_mod'])[:L], 'b_mod': f(inputs['b_mod'])[:L],
        'w_in': w_in,
        'w_small': f(np.concatenate([w_in[:, :, ODT:ODT + 16], w_in[:, :, OF:OF + 8]], axis=2)),
        'conv_wT': f(np.asarray(inputs['conv_w'])[:L].reshape(L, 4, 16, 128).transpose(0, 3, 2, 1)),
        'conv_bT': f(np.asarray(inputs['conv_b'])[:L].reshape(L, 16, 128).transpose(0, 2, 1)),
        'dt_bias': f(inputs['dt_bias'])[:L], 'a_log': f(inputs['a_log'])[:L], 'd_skip': f(inputs['d_skip'])[:L],
        'ssd_norm_wT': f(np.asarray(inputs['ssd_norm_w'])[:L].reshape(L, 8, 128).transpose(0, 2, 1)),
        'forget_b': f(inputs['forget_b'])[:L],
        'w_ssd_o': f(inputs['w_ssd_o'])[:L], 'w_att_o': f(inputs['w_att_o'])[:L], 'w_o': f(inputs['w_o'])[:L],
        'ln1_g': f(inputs['ln1_g'])[:L], 'ln1_b': f(inputs['ln1_b'])[:L],
        'w_router': f(np.concatenate([np.asarray(inputs['w_router_group'])[:L], np.asarray(inputs['w_router_expert'])[:L]], axis=2)),
        'b_router': f(np.concatenate([np.asarray(inputs['b_router_group'])[:L], np.asarray(inputs['b_router_expert'])[:L]], axis=1)),
        'w_gate': f(inputs['w_gate'])[:L], 'w_up': f(inputs['w_up'])[:L], 'w_down': f(inputs['w_down'])[:L],
        'ln2_g': f(inputs['ln2_g'])[:L], 'ln2_b': f(inputs['ln2_b'])[:L],
    }
    x = np.asarray(inputs['x'], dtype=np.float32)
    c = np.asarray(inputs['c'], dtype=np.float32)
    maps = []
    for b in range(x.shape[0]):
        m = dict(shared)
        m['x'] = np.ascontiguousarray(x[b, :S])
        m['cT'] = np.ascontiguousarray(c[b].reshape(8, 128).T)
        maps.append(m)
    return maps


_PROG = {}


def kernel(**inputs):
    S, L = 4096, 4
    if 'p' not in _PROG:
        _PROG['p'] = build_program(S, L)[0]
    nc = _PROG['p']
    maps = host_inputs(inputs, S, L)
    res = run_bass_kernel_spmd(nc, maps, core_ids=list(range(8)))
    return np.stack([np.asarray(r['out'], dtype=np.float32) for r in res.results], axis=0)
```

```python
import numpy as np
from contextlib import contextmanager, ExitStack
import concourse.bass as bass
import concourse.mybir as mybir
from concourse.bass_utils import run_bass_kernel_spmd

F32 = mybir.dt.float32
BF16 = mybir.dt.bfloat16
AF = mybir.ActivationFunctionType
ALU = mybir.AluOpType
AX = mybir.AxisListType

D = 1024
NIN = 6680
ALPHA = 8 ** 0.25
EPS = 1e-5
OZ, OXBC, ODT, OQ, OK_, OV, OF, OGS, OGA = 0, 1024, 3072, 3088, 3600, 4112, 4624, 4632, 5656


class Buf:
    def __init__(self, t, name):
        self.t = t
        self.name = name
        self.w = {}
        self.r = {}
        self.dsem = None

    def __getitem__(self, key):
        return self.t[key]


class FW:
    ENGS = ('pe', 'act', 'dve', 'pool', 'sp')

    def __init__(self, nc, n_dma_sems=48):
        self.nc = nc
        self.e = {'pe': nc.tensor, 'act': nc.scalar, 'dve': nc.vector, 'pool': nc.gpsimd, 'sp': nc.sync}
        self.esem = {k: nc.alloc_semaphore("es_" + k) for k in self.ENGS}
        self.ecount = {k: 0 for k in self.ENGS}
        self.known = {k: {} for k in self.ENGS}
        self.pending = {k: [] for k in self.ENGS}
        self.dsems = [nc.alloc_semaphore("ds_%d" % i) for i in range(n_dma_sems)]
        self.dtotal = {s: 0 for s in self.dsems}
        self.free_dsems = list(self.dsems)
        self.stack = None
        self.phase_bufs = []
        self.bar = nc.alloc_semaphore("bar")
        self.barcount = 0
        self.nbuf = 0
        self.ninstr = 0

    def sb(self, shape, dtype, name=None):
        self.nbuf += 1
        name = (name or "t") + "_%d" % self.nbuf
        if self.stack is not None:
            t = self.stack.enter_context(self.nc.sbuf_tensor(name, list(shape), dtype))
        else:
            t = self.nc.alloc_sbuf_tensor(name, list(shape), dtype)
        b = Buf(t, name)
        if self.stack is not None:
            self.phase_bufs.append(b)
        return b

    def ring(self, n, shape, dtype, name=None):
        return [self.sb(shape, dtype, name) for _ in range(n)]

    def token(self, name):
        b = Buf(None, name)
        if self.stack is not None:
            self.phase_bufs.append(b)
        return b

    def _dsem(self, b):
        if b.dsem is None:
            b.dsem = self.free_dsems.pop()
        return b.dsem

    @contextmanager
    def phase(self, name=""):
        assert self.stack is None
        self.stack = ExitStack()
        self.phase_bufs = []
        try:
            yield
            self.barrier()
        finally:
            for b in self.phase_bufs:
                if b.dsem is not None:
                    self.free_dsems.append(b.dsem)
                    b.dsem = None
            self.stack.close()
            self.stack = None
            self.phase_bufs = []

    def _waits(self, eng, reads, writes):
        waits = {}
        for b in reads:
            for s, v in b.w.items():
                if waits.get(s, 0) < v:
                    waits[s] = v
        for b in writes:
            for d in (b.w, b.r):
                for s, v in d.items():
                    if waits.get(s, 0) < v:
                        waits[s] = v
        e = self.e[eng]
        kn = self.known[eng]
        for s, v in waits.items():
            if eng == 'pe' and s is self.esem['pe']:
                continue
            if kn.get(s, 0) < v:
                e.wait_ge(s, v)
                self.ninstr += 1
                kn[s] = v

    def _record(self, ev, reads, writes):
        s, v = ev
        for b in writes:
            b.w = {s: v}
            b.r = {}
        for b in reads:
            if b.r.get(s, 0) < v:
                b.r[s] = v

    def op(self, eng, fn, reads=(), writes=(), inc=True):
        self._waits(eng, reads, writes)
        ins = fn(self.e[eng])
        self.ninstr += 1
        if not inc:
            self.pending[eng].append((tuple(reads), tuple(writes)))
            return ins
        self.ecount[eng] += 1
        ins.then_inc(self.esem[eng], 1)
        ev = (self.esem[eng], self.ecount[eng])
        for (r, w) in self.pending[eng]:
            self._record(ev, r, w)
        self.pending[eng] = []
        self._record(ev, reads, writes)
        return ins

    def dma(self, q, fn, reads=(), writes=(), sem_buf=None):
        self._waits(q, reads, writes)
        sbuf = sem_buf or (writes[0] if writes else reads[0])
        sem = self._dsem(sbuf)
        inss = fn(self.e[q])
        if not isinstance(inss, (list, tuple)):
            inss = [inss]
        for ins in inss:
            ins.then_inc(sem, 16)
            self.dtotal[sem] += 16
            self.ninstr += 1
        ev = (sem, self.dtotal[sem])
        self._record(ev, reads, writes)

    def barrier(self):
        sp = self.e['sp']
        kn = self.known['sp']
        for k in self.ENGS:
            assert not self.pending[k], "pending non-inc instrs at barrier on " + k
            s, v = self.esem[k], self.ecount[k]
            if k != 'sp' and kn.get(s, 0) < v:
                sp.wait_ge(s, v)
                kn[s] = v
        for s, v in self.dtotal.items():
            if kn.get(s, 0) < v:
                sp.wait_ge(s, v)
                kn[s] = v
        self.barcount += 1
        sp.sem_inc(self.bar, 1)
        for k in self.ENGS:
            if k == 'sp':
                continue
            self.e[k].wait_ge(self.bar, self.barcount)
            self.known[k] = dict(kn)
            self.known[k][self.esem[k]] = self.ecount[k]


SPARSE = True


def build_program(S=4096, L=4, debug=False, stop_after=None):
    nc = bass.Bass("TRN2", target_bir_lowering=False)
    NT = S // 128
    NB = S // 512
    fw = FW(nc)

    def inp(name, shape, dt=F32):
        return nc.dram_tensor(name, list(shape), dt, kind="ExternalInput").ap()

    dbg_kind = "ExternalOutput" if debug else "Internal"

    def scr(name, shape, dt, dbg=True):
        return nc.dram_tensor(name, list(shape), dt, kind=(dbg_kind if dbg else "Internal")).ap()

    x_in = inp("x", [S, D])
    cT = inp("cT", [128, 8])
    consts = inp("consts", [128, 768])
    w_mod = inp("w_mod", [L, D, 6 * D])
    b_mod = inp("b_mod", [L, 6 * D])
    w_in = inp("w_in", [L, D, NIN])
    w_small = inp("w_small", [L, D, 24])
    conv_wT = inp("conv_wT", [L, 128, 16, 4])
    conv_bT = inp("conv_bT", [L, 128, 16])
    dt_bias = inp("dt_bias", [L, 16])
    a_log = inp("a_log", [L, 16])
    d_skip = inp("d_skip", [L, 16])
    ssd_norm_wT = inp("ssd_norm_wT", [L, 128, 8])
    forget_b = inp("forget_b", [L, 8])
    w_ssd_o = inp("w_ssd_o", [L, D, D])
    w_att_o = inp("w_att_o", [L, 512, D])
    w_o = inp("w_o", [L, D, D])
    ln1_g = inp("ln1_g", [L, D])
    ln1_b = inp("ln1_b", [L, D])
    w_router = inp("w_router", [L, D, 20])
    b_router = inp("b_router", [L, 20])
    w_gate = inp("w_gate", [L, 16, D, 512])
    w_up = inp("w_up", [L, 16, D, 512])
    w_down = inp("w_down", [L, 16, 512, D])
    ln2_g = inp("ln2_g", [L, D])
    ln2_b = inp("ln2_b", [L, D])
    out = nc.dram_tensor("out", [S, D], F32, kind="ExternalOutput").ap()

    mod_row = scr("mod_row", [L, 6 * D], F32)
    wb_in = scr("wb_in", [2, D, NIN], BF16, dbg=False)
    wb_so = scr("wb_so", [2, D, D], BF16, dbg=False)
    wb_ao = scr("wb_ao", [2, 512, D], BF16, dbg=False)
    wb_o = scr("wb_o", [2, D, D], BF16, dbg=False)
    wb_r = scr("wb_r", [2, D, 20], BF16, dbg=False)
    wb_g = [scr("wb_g%d" % i_, [16 * 128, 4096], BF16, dbg=False) for i_ in range(2)]
    wb_u = [scr("wb_u%d" % i_, [16 * 128, 4096], BF16, dbg=False) for i_ in range(2)]
    wb_d = [scr("wb_d%d" % i_, [16 * 128, 4096], BF16, dbg=False) for i_ in range(2)]
    hT_d = scr("hT_d", [8, 128, S], BF16)
    zs_d = scr("zs_d", [S, D], BF16)
    xs_d = scr("xs_d", [S, D], BF16)
    Btm_d = scr("Btm_d", [S, 512], BF16)
    BT_d = scr("BT_d", [4, 128, S], BF16)
    CT_d = scr("CT_d", [4, 128, S], BF16)
    small_d = scr("small_d", [S, 24], F32)
    qT_d = scr("qT_d", [8, 66, S], BF16)
    kT_d = scr("kT_d", [8, 66, S], BF16)
    va_d = scr("va_d", [S, 8, 128], BF16)
    sgT_d = scr("sgT_d", [16, 128, S], BF16)
    gnT_d = scr("gnT_d", [8, 128, S], BF16)
    oT_d = scr("oT_d", [4, 128, S], BF16)
    y_d = scr("y_d", [S, D], F32)
    xres_d = scr("xres_d", [S, D], F32)
    fbias_d = scr("fbias_d", [8, 128, NB * NT], F32)
    h2tm_d = scr("h2tm_d", [S, D], BF16)
    hs_d = scr("hs_d", [(2 * NT + 16) * 128, D], BF16)
    ys_d = scr("ys_d", [(2 * NT + 16) * 128, D], BF16)

    psall = nc.alloc_psum_tensor("psall", [128, 4096], F32)
    psall_bf = psall.bitcast(BF16)
    ps = [Buf(psall[:, i * 512:(i + 1) * 512], "ps%d" % i) for i in range(8)]

    def psbf(i):
        return psall_bf[:, i * 1024:(i + 1) * 1024]

    cst = fw.sb([128, 768], F32, "cst")
    identb = fw.sb([128, 128], BF16, "identb")
    ident = cst[:, 0:128]
    Uinc = cst[:, 128:256]
    Lstr = cst[:, 256:384]
    ones = cst[:, 384:512]
    NSLT = 2 * NT + 16
    kstart = cst[:, 512:512 + NSLT]
    kidx = cst[:, 592:592 + NSLT]
    iota_p = cst[:, 672:673]
    I32 = mybir.dt.int32

    def TT(eng, o, a, b, op, R, W):
        fw.op(eng, lambda e: e.tensor_tensor(out=o, in0=a, in1=b, op=op), reads=R, writes=W)

    def TS(eng, o, a, s1, s2, op0, op1, R, W):
        if s2 is None:
            fw.op(eng, lambda e: e.tensor_scalar(out=o, in0=a, scalar1=s1, scalar2=None, op0=op0), reads=R, writes=W)
        else:
            fw.op(eng, lambda e: e.tensor_scalar(out=o, in0=a, scalar1=s1, scalar2=s2, op0=op0, op1=op1), reads=R, writes=W)

    def STT(eng, o, a, s, b, op0, op1, R, W):
        fw.op(eng, lambda e: e.scalar_tensor_tensor(out=o, in0=a, scalar=s, in1=b, op0=op0, op1=op1), reads=R, writes=W)

    def ACT(o, a, func, R, W, bias=None, scale=None, accum=None):
        kw = {}
        if bias is not None:
            kw['bias'] = bias
        if scale is not None:
            kw['scale'] = scale
        if accum is not None:
            kw['accum_out'] = accum
        fw.op('act', lambda e: e.activation(out=o, in_=a, func=func, **kw), reads=R, writes=W)

    def CP(eng, o, a, R, W):
        if eng == 'act':
            ACT(o, a, AF.Copy, R, W)
        else:
            fw.op(eng, lambda e: e.tensor_copy(out=o, in_=a), reads=R, writes=W)

    def MM(o, lhsT, rhs, start, stop, R, W, inc=True):
        fw.op('pe', lambda e: e.matmul(o, lhsT=lhsT, rhs=rhs, start=start, stop=stop), reads=R, writes=W, inc=inc)

    def TR(o, a, R, W, inc=True):
        fw.op('pe', lambda e: e.transpose(o, a, identb[:, :]), reads=list(R) + [identb], writes=W, inc=inc)

    def LD(q, o, a, W, R=(), **kw):
        fw.dma(q, lambda e: e.dma_start(out=o, in_=a, **kw), reads=R, writes=W)

    def ST(q, o, a, R, **kw):
        fw.dma(q, lambda e: e.dma_start(out=o, in_=a, **kw), reads=R)

    def MEMSET(eng, o, val, W):
        fw.op(eng, lambda e: e.memset(o, val), writes=W)

    NC_OK = dict(allow_slow_non_contiguous=True)

    LD('sp', cst[:, :], consts, [cst])
    CP('dve', identb[:, :], ident, [cst], [identb])

    wtok = [fw.token("wtok%d" % l) for l in range(L)]

    def cast_weights(l):
        s = l % 2
        tk = wtok[l]

        def go(e):
            r = []
            for k in range(8):
                r.append(e.dma_start(out=wb_in[s, k * 128:(k + 1) * 128, :], in_=w_in[l, k * 128:(k + 1) * 128, :]))
                r.append(e.dma_start(out=wb_so[s, k * 128:(k + 1) * 128, :], in_=w_ssd_o[l, k * 128:(k + 1) * 128, :]))
                r.append(e.dma_start(out=wb_o[s, k * 128:(k + 1) * 128, :], in_=w_o[l, k * 128:(k + 1) * 128, :]))
            for k in range(4):
                r.append(e.dma_start(out=wb_ao[s, k * 128:(k + 1) * 128, :], in_=w_att_o[l, k * 128:(k + 1) * 128, :]))
            r.append(e.dma_start(out=wb_r[s], in_=w_router[l]))
            for ex in range(16):
                rows = slice(ex * 128, (ex + 1) * 128)
                r.append(e.dma_start(out=wb_g[s][rows, :], in_=w_gate[l, ex].rearrange("(p kc) n -> p (kc n)", kc=8)))
                r.append(e.dma_start(out=wb_u[s][rows, :], in_=w_up[l, ex].rearrange("(p kc) n -> p (kc n)", kc=8)))
                r.append(e.dma_start(out=wb_d[s][rows, :], in_=w_down[l, ex].rearrange("(p kc) n -> p (kc n)", kc=4)))
            return r
        fw.dma('pool', go, writes=[tk])

    cast_weights(0)
    with fw.phase("mod"):
        ct = fw.sb([128, 8], F32, "ct")
        cact = fw.sb([128, 8], F32, "cact")
        LD('sp', ct[:, :], cT, [ct])
        ACT(cact[:, :], ct[:, :], AF.Silu, [ct], [cact])
        brow = fw.sb([1, 6 * D], F32, "brow")
        row = fw.sb([1, 6 * D], F32, "row")
        wring = fw.ring(3, [128, 8, 512], F32, "wmod")
        i = 0
        for l in range(L):
            LD('sp', brow[0:1, :], b_mod[l:l + 1, :], [brow])
            for nb in range(12):
                wbuf = wring[i % 3]
                P = ps[i % 2]
                i += 1
                LD('sp', wbuf[:, :, :], w_mod[l].rearrange("(kc p) n -> p kc n", p=128)[:, :, nb * 512:(nb + 1) * 512], [wbuf])
                for kc in range(8):
                    MM(P[0:1, :], cact[:, kc:kc + 1], wbuf[:, kc, :], kc == 0, kc == 7, [cact, wbuf], [P], inc=(kc == 7))
                TT('dve', row[0:1, nb * 512:(nb + 1) * 512], P[0:1, :], brow[0:1, nb * 512:(nb + 1) * 512], ALU.add, [P, brow], [row])
            ST('sp', mod_row[l:l + 1, :], row[0:1, :], [row])

    def ln_phase(l, x_src, y_src, x_dst, gt_idx, g_ap, b_ap, sc_idx, sh_idx, lp=None, want_tm=False):
        lp = l if lp is None else lp
        with fw.phase("ln"):
            post = y_src is not None
            pre = sc_idx is not None
            if post:
                gtb = fw.sb([128, D], F32, "gtb")
                gb = fw.sb([128, D], F32, "gb")
                bb = fw.sb([128, D], F32, "bb")
                LD('sp', gtb[:, :], mod_row[l, gt_idx * D:(gt_idx + 1) * D].partition_broadcast(128), [gtb])
                LD('sp', gb[:, :], g_ap.partition_broadcast(128), [gb])
                LD('sp', bb[:, :], b_ap.partition_broadcast(128), [bb])
                TS('dve', gtb[:, :], gtb[:, :], 1.0, None, ALU.add, None, [gtb], [gtb])
            if pre:
                scp = fw.sb([128, 8], F32, "scp")
                shp = fw.sb([128, 8], F32, "shp")
                LD('sp', scp[:, :], mod_row[lp, sc_idx * D:(sc_idx + 1) * D].rearrange("(c p) -> p c", p=128), [scp], **NC_OK)
                LD('sp', shp[:, :], mod_row[lp, sh_idx * D:(sh_idx + 1) * D].rearrange("(c p) -> p c", p=128), [shp], **NC_OK)
                TS('dve', scp[:, :], scp[:, :], 1.0, None, ALU.add, None, [scp], [scp])
            if want_tm:
                scb = fw.sb([128, D], F32, "scb")
                shb = fw.sb([128, D], F32, "shb")
                LD('sp', scb[:, :], mod_row[lp, sc_idx * D:(sc_idx + 1) * D].partition_broadcast(128), [scb])
                LD('sp', shb[:, :], mod_row[lp, sh_idx * D:(sh_idx + 1) * D].partition_broadcast(128), [shb])
                TS('dve', scb[:, :], scb[:, :], 1.0, None, ALU.add, None, [scb], [scb])
                h2fr = fw.ring(2, [128, D], F32, "h2f")
                h2br = fw.ring(3, [128, D], BF16, "h2b")
            xr = fw.ring(4, [128, D], F32, "xt")
            yr = fw.ring(4, [128, D], F32, "yt") if post else None
            ur = fw.ring(2, [128, D], F32, "u") if post else None
            xnr = fw.ring(3, [128, D], F32, "xnew") if post else None
            sm1 = fw.ring(3, [128, 16], F32, "sm1")
            sm2 = fw.ring(3, [128, 16], F32, "sm2")
            xbr = fw.ring(3, [128, D], BF16, "xnb") if pre else None
            hr = fw.ring(3, [128, 8, 128], BF16, "hTt") if pre else None

            def loads(t):
                LD('sp', xr[t % 4][:, :], x_src[t * 128:(t + 1) * 128, :], [xr[t % 4]])
                if post:
                    LD('sp', yr[t % 4][:, :], y_src[t * 128:(t + 1) * 128, :], [yr[t % 4]])

            def stats(src, srcb, sm):
                for hh in range(2):
                    fw.op('dve', lambda e: e.bn_stats(out=sm[:, 6 * hh:6 * hh + 6], in_=src[:, hh * 512:(hh + 1) * 512]), reads=[srcb], writes=[sm])
                fw.op('dve', lambda e: e.bn_aggr(out=sm[:, 12:14], in_=sm[:, 0:12]), reads=[sm], writes=[sm])
                ACT(sm[:, 14:15], sm[:, 13:14], AF.Ln, [sm], [sm], bias=EPS)
                ACT(sm[:, 14:15], sm[:, 14:15], AF.Exp, [sm], [sm], scale=-0.5)
                STT('dve', sm[:, 15:16], sm[:, 12:13], -1.0, sm[:, 14:15], ALU.mult, ALU.mult, [sm], [sm])

            def S1(t):
                if not post:
                    return
                xt, yt, u, xn, sm = xr[t % 4], yr[t % 4], ur[t % 2], xnr[t % 3], sm1[t % 3]
                TT('pool', u[:, :], yt[:, :], gtb[:, :], ALU.mult, [yt, gtb], [u])
                STT('dve', u[:, :], xt[:, :], ALPHA, u[:, :], ALU.mult, ALU.add, [xt, u], [u])
                stats(u[:, :], u, sm)
                ACT(xn[:, :], u[:, :], AF.Identity, [u, sm], [xn], bias=sm[:, 15:16], scale=sm[:, 14:15])

            def S2(t):
                if post:
                    xn = xnr[t % 3]
                    TT('pool', xn[:, :], xn[:, :], gb[:, :], ALU.mult, [xn, gb], [xn])
                    TT('dve', xn[:, :], xn[:, :], bb[:, :], ALU.add, [xn, bb], [xn])
                    ST('sp', x_dst[t * 128:(t + 1) * 128, :], xn[:, :], [xn])
                    cur = xn
                else:
                    cur = xr[t % 4]
                if pre:
                    xb, sm = xbr[t % 3], sm2[t % 3]
                    stats(cur[:, :], cur, sm)
                    ACT(xb[:, :], cur[:, :], AF.Identity, [cur, sm], [xb], bias=sm[:, 15:16], scale=sm[:, 14:15])
                    if want_tm:
                        h2f, h2b = h2fr[t % 2], h2br[t % 3]
                        TT('pool', h2f[:, :], xb[:, :], scb[:, :], ALU.mult, [xb, scb], [h2f])
                        TT('dve', h2b[:, :], h2f[:, :], shb[:, :], ALU.add, [h2f, shb], [h2b])
                        ST('sp', h2tm_d[t * 128:(t + 1) * 128, :], h2b[:, :], [h2b])

            def S3(t):
                if not pre:
                    return
                xb, ht = xbr[t % 3], hr[t % 3]
                P = ps[t % 2]
                for c in range(8):
                    TR(psbf(t % 2)[:, c * 128:(c + 1) * 128], xb[:, c * 128:(c + 1) * 128], [xb], [P], inc=(c == 7))
                for c in range(8):
                    ACT(ht[:, c, :], psbf(t % 2)[:, c * 128:(c + 1) * 128], AF.Identity, [P, scp, shp], [ht], bias=shp[:, c:c + 1], scale=scp[:, c:c + 1])
                ST('sp', hT_d.rearrange("c p s -> p c s")[:, :, t * 128:(t + 1) * 128], ht[:, :, :], [ht])

            loads(0)
            if NT > 1:
                loads(1)
            for step in range(NT + 2):
                if step + 2 < NT:
                    loads(step + 2)
                if step < NT:
                    S1(step)
                if 0 <= step - 1 < NT:
                    S2(step - 1)
                if 0 <= step - 2 < NT:
                    S3(step - 2)

    def win_phase(l):
        s = l % 2
        wsrc = wb_in[s].rearrange("(kc p) n -> p kc n", p=128)
        with fw.phase("win"):
            hT = fw.sb([128, 8, S], BF16, "hT")
            fw.dma('sp', lambda e: [e.dma_start(out=hT[:, c, :], in_=hT_d[c]) for c in range(8)], writes=[hT])
            wA = fw.sb([128, 8, 1560], BF16, "wA")
            LD('sp', wA[:, :, 0:1024], wsrc[:, :, OZ:OZ + 1024], [wA], R=[wtok[l]])
            LD('sp', wA[:, :, 1024:1536], wsrc[:, :, OV:OV + 512], [wA])
            LD('pool', wA[:, :, 1536:1560], w_small[l].rearrange("(kc p) n -> p kc n", p=128), [wA])
            cw = fw.sb([128, 16, 4], F32, "cw")
            cb = fw.sb([128, 16], F32, "cb")
            LD('sp', cw[:, :, :], conv_wT[l], [cw])
            LD('sp', cb[:, :], conv_bT[l], [cb])
            zr = fw.ring(2, [128, D], BF16, "zt")
            vr = fw.ring(2, [128, 8, 128], BF16, "vt")
            sr = fw.ring(2, [128, 24], F32, "smt")
            for v in vr:
                MEMSET('pool', v[:, :, :], 1.0, [v])
            for t in range(NT):
                zt, vt, st = zr[t % 2], vr[t % 2], sr[t % 2]
                tok = slice(t * 128, (t + 1) * 128)
                for cbk in range(2):
                    P = ps[cbk]
                    for kc in range(8):
                        MM(P[:, :], hT[:, kc, tok], wA[:, kc, cbk * 512:(cbk + 1) * 512], kc == 0, kc == 7, [hT, wA], [P], inc=(kc == 7))
                    ACT(zt[:, cbk * 512:(cbk + 1) * 512], P[:, :], AF.Silu, [P], [zt])
                ST('sp', zs_d[tok, :], zt[:, :], [zt])
                P = ps[2]
                for kc in range(8):
                    MM(P[:, :], hT[:, kc, tok], wA[:, kc, 1024:1536], kc == 0, kc == 7, [hT, wA], [P], inc=(kc == 7))
                CP('dve', vt[:, :, 0:64], P[:, :].rearrange("p (h d) -> p h d", h=8), [P], [vt])
                ST('sp', va_d[tok, :, :], vt[:, :, :], [vt])
                P = ps[3]
                for kc in range(8):
                    MM(P[:, 0:24], hT[:, kc, tok], wA[:, kc, 1536:1560], kc == 0, kc == 7, [hT, wA], [P], inc=(kc == 7))
                CP('dve', st[:, :], P[:, 0:24], [P], [st])
                ST('sp', small_d[tok, :], st[:, :], [st])
            chunks = [('xbc', m, OXBC + 128 * m) for m in range(16)]
            chunks += [('q', j, OQ + 128 * j) for j in range(4)] + [('k', j, OK_ + 128 * j) for j in range(4)]
            chunks += [('g', j, OGS + 128 * j) for j in range(16)]
            wBr = fw.ring(3, [128, 8, 128], BF16, "wB")
            rawr = fw.ring(2, [128, 520], F32, "raw")
            accr = fw.ring(2, [128, 512], F32, "acc")
            otr = fw.ring(3, [128, 512], BF16, "ot")
            xtr = fw.ring(2, [128, 4, 128], BF16, "xtt")
            k8 = fw.sb([128, 512], BF16, "k8")
            MEMSET('pool', k8[:, :], 1.0, [k8])
            for h in range(8):
                for nb in range(NB):
                    ST('sp', kT_d[h, 64:66, nb * 512:(nb + 1) * 512], k8[0:2, :], [k8])
            it = 0

            def ldw(ci):
                LD('sp', wBr[ci % 3][:, :, :], wsrc[:, :, chunks[ci][2]:chunks[ci][2] + 128], [wBr[ci % 3]])
            ldw(0)
            ldw(1)
            for ci, (kind, m, col) in enumerate(chunks):
                if ci + 2 < len(chunks):
                    ldw(ci + 2)
                wB = wBr[ci % 3]
                for nb in range(NB):
                    cols = slice(nb * 512, (nb + 1) * 512)
                    P = ps[4 + it % 2]
                    ot = otr[it % 3]
                    for kc in range(8):
                        MM(P[:, :], wB[:, kc, :], hT[:, kc, cols], kc == 0, kc == 7, [wB, hT], [P], inc=(kc == 7))
                    if kind == 'xbc':
                        raw, acc = rawr[it % 2], accr[it % 2]
                        praw = rawr[(it + 1) % 2]
                        CP('act', raw[:, 3:515], P[:, :], [P], [raw])
                        if nb == 0:
                            MEMSET('pool', raw[:, 0:3], 0.0, [raw])
                        else:
                            CP('pool', raw[:, 0:3], praw[:, 512:515], [praw], [raw])
                        ACT(acc[:, :], P[:, :], AF.Identity, [P, cw, cb], [acc], bias=cb[:, m:m + 1], scale=cw[:, m, 3:4])
                        STT('dve', acc[:, :], raw[:, 2:514], cw[:, m, 2:3], acc[:, :], ALU.mult, ALU.add, [raw, cw, acc], [acc])
                        STT('dve', acc[:, :], raw[:, 1:513], cw[:, m, 1:2], acc[:, :], ALU.mult, ALU.add, [raw, cw, acc], [acc])
                        STT('dve', acc[:, :], raw[:, 0:512], cw[:, m, 0:1], acc[:, :], ALU.mult, ALU.add, [raw, cw, acc], [acc])
                        ACT(ot[:, :], acc[:, :], AF.Silu, [acc], [ot])
                        if m >= 8:
                            g = (m - 8) % 4
                            dst = BT_d if m < 12 else CT_d
                            ST('sp', dst[g, :, cols], ot[:, :], [ot])
                        if m < 12:
                            xt = xtr[it % 2]
                            PT = ps[6 + it % 2]
                            for j in range(4):
                                TR(psbf(6 + it % 2)[:, j * 128:(j + 1) * 128], ot[:, j * 128:(j + 1) * 128], [ot], [PT], inc=(j == 3))
                            CP('dve', xt[:, :, :], psbf(6 + it % 2)[:, 0:512].rearrange("p (j c) -> p j c", j=4), [PT], [xt])
                            if m < 8:
                                dd = xs_d[cols, m * 128:(m + 1) * 128]
                            else:
                                dd = Btm_d[cols, (m - 8) * 128:(m - 7) * 128]
                            ST('sp', dd.rearrange("(j p) c -> p j c", p=128), xt[:, :, :], [xt])
                    elif kind in ('q', 'k'):
                        CP('dve', ot[:, :], P[:, :], [P], [ot])
                        dst = qT_d if kind == 'q' else kT_d
                        ST('sp', dst[2 * m, 0:64, cols], ot[0:64, :], [ot])
                        ST('sp', dst[2 * m + 1, 0:64, cols], ot[64:128, :], [ot])
                    else:
                        ACT(ot[:, :], P[:, :], AF.Sigmoid, [P], [ot])
                        ST('sp', sgT_d[m, :, cols], ot[:, :], [ot])
                    it += 1

    def attprep_phase(l):
        with fw.phase("attprep"):
            fr = fw.sb([128, NT, 8], F32, "fr")
            LD('sp', fr[:, :, :], small_d[:, 16:24].rearrange("(b p) h -> p b h", p=128), [fr])
            fbb = fw.sb([128, 8], F32, "fbb")
            LD('sp', fbb[:, :], forget_b[l].partition_broadcast(128), [fbb])
            lf = fw.sb([128, 8, NT], F32, "lf")
            TT('dve', lf[:, :, :], fr[:, :, :].rearrange("p b h -> p h b"), fbb[:, :].unsqueeze(2).to_broadcast([128, 8, NT]), ALU.add, [fr, fbb], [lf])
            ACT(lf[:, :, :], lf[:, :, :], AF.Exp, [lf], [lf], scale=-1.0)
            ACT(lf[:, :, :], lf[:, :, :], AF.Ln, [lf], [lf], bias=1.0)
            TS('dve', lf[:, :, :], lf[:, :, :], -1.0, None, ALU.mult, None, [lf], [lf])
            lf2 = lf[:, :, :].rearrange("p h b -> p (h b)")
            NHB = 8 * NT
            P = ps[0]
            MM(P[:, 0:NHB], Uinc, lf2, True, True, [cst, lf], [P])
            P1 = ps[1]
            MM(P1[:, 0:NHB], ones, lf2, True, True, [cst, lf], [P1])
            W = fw.sb([128, 8, NT], F32, "W")
            T = fw.sb([128, 8, NT], F32, "T")
            CP('dve', W[:, :, :].rearrange("p h b -> p (h b)"), P[:, 0:NHB], [P], [W])
            CP('dve', T[:, :, :].rearrange("p h b -> p (h b)"), P1[:, 0:NHB], [P1], [T])
            Pf = fw.sb([128, 8, NT], F32, "Pf")
            MEMSET('dve', Pf[:, :, 0:1], 0.0, [Pf])
            for b in range(1, NT):
                TT('dve', Pf[:, :, b:b + 1], Pf[:, :, b - 1:b], T[:, :, b - 1:b], ALU.add, [Pf, T], [Pf])
            Fm = fw.sb([128, 8, NT], F32, "Fm")
            TT('dve', Fm[:, :, :], W[:, :, :], Pf[:, :, :], ALU.add, [W, Pf], [Fm])
            bt = fw.sb([128, 8, NB, NT], F32, "bt")
            for h in range(8):
                TT('dve', bt[:, h, :, :], Pf[:, h, :].rearrange("p (j r) -> p j r", r=4)[:, :, 0:1].to_broadcast([128, NB, NT]),
                   Fm[:, h, :].unsqueeze(1).to_broadcast([128, NB, NT]), ALU.subtract, [Pf, Fm], [bt])
                ST('sp', fbias_d[h], bt[:, h, :, :].rearrange("p j i -> p (j i)"), [bt])
            fl = fw.sb([128, 8, NT], F32, "fl")
            for h in range(8):
                TT('dve', fl[:, h, :].rearrange("p (j r) -> p j r", r=4), Fm[:, h, :].rearrange("p (j r) -> p j r", r=4),
                   Pf[:, h, :].rearrange("p (j r) -> p j r", r=4)[:, :, 0:1].to_broadcast([128, NB, 4]), ALU.subtract, [Fm, Pf], [fl])
            TS('dve', fl[:, :, :], fl[:, :, :], 8.0, None, ALU.mult, None, [fl], [fl])
            hi = fw.sb([128, 8, NT], BF16, "hi")
            hi32 = fw.sb([128, 8, NT], F32, "hi32")
            lo = fw.sb([128, 8, NT], BF16, "lo")
            CP('dve', hi[:, :, :], fl[:, :, :], [fl], [hi])
            CP('dve', hi32[:, :, :], hi[:, :, :], [hi], [hi32])
            TT('dve', lo[:, :, :], fl[:, :, :], hi32[:, :, :], ALU.subtract, [fl, hi32], [lo])
            rows = fw.sb([128, 2, 2, 128], BF16, "rows")
            HB = 128 // NT
            for wi, src in enumerate((hi, lo)):
                s2 = src[:, :, :].rearrange("p h b -> p (h b)")
                nhalf = (8 * NT + 127) // 128
                for hf in range(nhalf):
                    n = min(128, 8 * NT - hf * 128)
                    PT = ps[2 + (wi * 2 + hf) % 2]
                    TR(psbf(2 + (wi * 2 + hf) % 2)[0:n, 0:128], s2[:, hf * 128:hf * 128 + n], [src], [PT])
                    CP('dve', rows[0:n, wi, hf, :], psbf(2 + (wi * 2 + hf) % 2)[0:n, 0:128], [PT], [rows])
            for h in range(8):
                for wi in range(2):
                    g = h * NT
                    hf, p0 = g // 128, g % 128
                    ST('sp', qT_d[h, 64 + wi, :].rearrange("(b p) -> b p", p=128), rows[p0:p0 + NT, wi, hf, :], [rows])

    def att_phase(l):
        with fw.phase("att"):
            qr = fw.ring(2, [66, S], BF16, "qa")
            kr = fw.ring(2, [66, S], BF16, "ka")
            vr = fw.ring(2, [128, NT, 128], BF16, "va")
            br = fw.ring(2, [128, NB * NT], F32, "fb")
            pr = fw.ring(4, [128, 512], BF16, "pt")
            mr = fw.ring(2, [128, 128], F32, "msk")
            rr = fw.ring(2, [64, 512], F32, "rs")
            orr = fw.ring(2, [64, 512], BF16, "on")
            negm = fw.sb([128, 128], F32, "negm")
            TS('dve', negm[:, :], Lstr, -30000.0, None, ALU.mult, None, [cst], [negm])

            def loads(h):
                LD('sp', qr[h % 2][:, :], qT_d[h], [qr[h % 2]])
                LD('sp', kr[h % 2][:, :], kT_d[h], [kr[h % 2]])
                LD('sp', vr[h % 2][:, :, :], va_d[:, h, :].rearrange("(b p) d -> p b d", p=128), [vr[h % 2]])
                LD('sp', br[h % 2][:, :], fbias_d[h], [br[h % 2]])
            loads(0)
            items = [(h, j, i) for h in range(8) for j in range(NB) for i in range(4 * j + 4)]
            LA = 2

            def s_part(n_, h, j, i):
                qa, ka, fb = qr[h % 2], kr[h % 2], br[h % 2]
                r = i - 4 * j
                c0 = 128 * r if r > 0 else 0
                n = 512 - c0
                PS_ = ps[n_ % 4]
                pt = pr[n_ % 4]
                bias = fb[:, j * NT + i:j * NT + i + 1]
                MM(PS_[:, 0:n], ka[:, i * 128:(i + 1) * 128], qa[:, j * 512 + c0:(j + 1) * 512], True, True, [ka, qa], [PS_])
                if r >= 0:
                    mk = mr[n_ % 2]
                    TT('dve', mk[:, :], PS_[:, 0:128], negm[:, :], ALU.add, [PS_, negm], [mk])
                    ACT(pt[:, 0:128], mk[:, :], AF.Exp, [mk, fb], [pt], bias=bias, scale=0.125)
                    if n > 128:
                        ACT(pt[:, 128:n], PS_[:, 128:n], AF.Exp, [PS_, fb], [pt], bias=bias, scale=0.125)
                else:
                    ACT(pt[:, 0:n], PS_[:, 0:n], AF.Exp, [PS_, fb], [pt], bias=bias, scale=0.125)

            def pv_part(n_, h, j, i):
                if j == 0 and i == 0 and h + 1 < 8:
                    loads(h + 1)
                va = vr[h % 2]
                g_ = h * NB + j
                r = i - 4 * j
                c0 = 128 * r if r > 0 else 0
                n = 512 - c0
                nk = 4 * j + 4
                PO = ps[6 + g_ % 2]
                pt = pr[n_ % 4]
                MM(PO[:, c0:512], va[:, i, :], pt[:, 0:n], i == 0, i == nk - 1, [va, pt], [PO], inc=(i == nk - 1))
                if i == nk - 1:
                    rs = rr[g_ % 2]
                    on = orr[g_ % 2]
                    CP('dve', rs[:, :], PO[64:128, :], [PO], [rs])
                    fw.op('dve', lambda e: e.reciprocal(out=rs[:, :], in_=rs[:, :]), reads=[rs], writes=[rs])
                    TT('dve', on[:, :], PO[0:64, :], rs[:, :], ALU.mult, [PO, rs], [on])
                    ST('sp', oT_d[h // 2, (h % 2) * 64:(h % 2) * 64 + 64, j * 512:(j + 1) * 512], on[:, :], [on])

            for n_ in range(len(items) + LA):
                if n_ < len(items):
                    s_part(n_, *items[n_])
                if n_ - LA >= 0:
                    pv_part(n_ - LA, *items[n_ - LA])

    def ssd_phase(l):
        with fw.phase("ssd"):
            dtb = fw.sb([128, 16], F32, "dtb")
            Ab = fw.sb([128, 16], F32, "Ab")
            dsk = fw.sb([128, 16], F32, "dsk")
            LD('sp', dtb[:, :], dt_bias[l].partition_broadcast(128), [dtb])
            LD('sp', Ab[:, :], a_log[l].partition_broadcast(128), [Ab])
            LD('sp', dsk[:, :], d_skip[l].partition_broadcast(128), [dsk])
            ACT(Ab[:, :], Ab[:, :], AF.Exp, [Ab], [Ab])
            TS('dve', Ab[:, :], Ab[:, :], -1.0, None, ALU.mult, None, [Ab], [Ab])
            st32 = fw.sb([128, D], F32, "st32")
            stb = fw.sb([128, D], BF16, "stb")
            MEMSET('dve', st32[:, :], 0.0, [st32])
            MEMSET('dve', stb[:, :], 0.0, [stb])
            xsr = fw.ring(3, [128, D], BF16, "xs")
            zsr = fw.ring(3, [128, D], BF16, "zs")
            btr = fw.ring(3, [128, 512], BF16, "btm")
            bTr = fw.ring(3, [128, 4, 128], BF16, "bT")
            cTr = fw.ring(3, [128, 4, 128], BF16, "cT")
            dtr = fw.ring(3, [128, 16], F32, "dtraw")
            smr = fw.ring(3, [128, 160], F32, "ssm")
            xdr = fw.ring(3, [128, D], BF16, "xdt")
            xer = fw.ring(3, [128, D], BF16, "xdte")
            AMr = fw.ring(2, [128, 16, 128], F32, "AM")
            Dmr = fw.ring(2, [128, 16, 128], F32, "Dm")
            cbr = fw.ring(2, [128, 4, 128], F32, "cbm")
            Mtr = fw.ring(3, [128, 16, 128], BF16, "Mt")
            t1r = fw.ring(2, [128, D], F32, "t1")
            t2r = fw.ring(2, [128, D], F32, "t2")
            gnr = fw.ring(2, [128, D], BF16, "gn")
            gTr = fw.ring(2, [128, 8, 128], BF16, "gT")
            junk = fw.sb([128, D], BF16, "junk")

            def loads(c):
                tok = slice(c * 128, (c + 1) * 128)
                k = c % 3
                LD('sp', xsr[k][:, :], xs_d[tok, :], [xsr[k]])
                LD('sp', zsr[k][:, :], zs_d[tok, :], [zsr[k]])
                LD('sp', btr[k][:, :], Btm_d[tok, :], [btr[k]])
                LD('sp', bTr[k][:, :, :], BT_d.rearrange("g p s -> p g s")[:, :, tok], [bTr[k]])
                LD('sp', cTr[k][:, :, :], CT_d.rearrange("g p s -> p g s")[:, :, tok], [cTr[k]])
                LD('sp', dtr[k][:, :], small_d[tok, 0:16], [dtr[k]])
            def SA(c):
                    k = c % 3
                    tok = slice(c * 128, (c + 1) * 128)
                    xs, zs, btm, bT, cTt, dtraw, sm = xsr[k], zsr[k], btr[k], bTr[k], cTr[k], dtr[k], smr[k]
                    xdt, xdte, AM, Dm, cbm, Mt, t1, t2, gn, gT = xdr[k], xer[k], AMr[c % 2], Dmr[c % 2], cbr[c % 2], Mtr[k], t1r[c % 2], t2r[c % 2], gnr[c % 2], gTr[c % 2]
                    dt_, a_, acs, tot, ea, te, cd, dte = [sm[:, 16 * i:16 * i + 16] for i in range(8)]
                    xs3 = xs[:, :].rearrange("p (h d) -> p h d", h=16)
                    TT('dve', dt_, dtraw[:, :], dtb[:, :], ALU.add, [dtraw, dtb], [sm])
                    ACT(dt_, dt_, AF.Exp, [sm], [sm])
                    ACT(dt_, dt_, AF.Ln, [sm], [sm], bias=1.0)
                    TT('dve', a_, dt_, Ab[:, :], ALU.mult, [sm, Ab], [sm])
                    P0 = ps[0]
                    MM(P0[:, 0:16], Uinc, a_, True, True, [cst, sm], [P0], inc=False)
                    MM(P0[:, 16:32], ones, a_, True, True, [cst, sm], [P0])
                    CP('dve', sm[:, 32:64], P0[:, 0:32], [P0], [sm])
                    ACT(ea, acs, AF.Exp, [sm], [sm])
                    TT('dve', te, tot, acs, ALU.subtract, [sm], [sm])
                    ACT(te, te, AF.Exp, [sm], [sm])
                    ACT(cd, tot, AF.Exp, [sm], [sm])
                    TT('dve', dte, dt_, te, ALU.mult, [sm], [sm])
                    TT('dve', xdt[:, :].rearrange("p (h d) -> p h d", h=16), xs3, dt_.unsqueeze(2).to_broadcast([128, 16, 64]), ALU.mult, [xs, sm], [xdt])
                    TT('pool', xdte[:, :].rearrange("p (h d) -> p h d", h=16), xs3, dte.unsqueeze(2).to_broadcast([128, 16, 64]), ALU.mult, [xs, sm], [xdte])
                    TT('dve', AM[:, :, :], Lstr.unsqueeze(1).to_broadcast([128, 16, 128]), a_.unsqueeze(2).to_broadcast([128, 16, 128]), ALU.mult, [cst, sm], [AM])
                    for half_ in range(2):
                        for hh in range(8 * half_, 8 * half_ + 8):
                            PSg = ps[1 + (hh % 8) // 4]
                            MM(PSg[:, (hh % 4) * 128:(hh % 4 + 1) * 128], AM[:, hh, :], Uinc, True, True, [AM, cst], [PSg], inc=(hh % 4 == 3))
                        for q4 in range(2):
                            hq = 8 * half_ + 4 * q4
                            ACT(Dm[:, hq:hq + 4, :].rearrange("p h s -> p (h s)"), ps[1 + q4][:, :], AF.Exp, [ps[1 + q4]], [Dm])
                    for g in range(4):
                        MM(P0[:, g * 128:(g + 1) * 128], bT[:, g, :], cTt[:, g, :], True, True, [bT, cTt], [P0], inc=(g == 3))
                    TT('dve', cbm[:, :, :], P0[:, :].rearrange("p (g s) -> p g s", g=4), Uinc.unsqueeze(1).to_broadcast([128, 4, 128]), ALU.mult, [P0, cst], [cbm])
                    TT('dve', Mt[:, :, :].rearrange("p (g r) s -> p g r s", g=4), Dm[:, :, :].rearrange("p (g r) s -> p g r s", g=4),
                       cbm[:, :, :].unsqueeze(2).to_broadcast([128, 4, 4, 128]), ALU.mult, [Dm, cbm], [Mt])

            def SB(c):
                    k = c % 3
                    tok = slice(c * 128, (c + 1) * 128)
                    xs, zs, btm, bT, cTt, dtraw, sm = xsr[k], zsr[k], btr[k], bTr[k], cTr[k], dtr[k], smr[k]
                    xdt, xdte, AM, Dm, cbm, Mt, t1, t2, gn, gT = xdr[k], xer[k], AMr[c % 2], Dmr[c % 2], cbr[c % 2], Mtr[k], t1r[c % 2], t2r[c % 2], gnr[c % 2], gTr[c % 2]
                    dt_, a_, acs, tot, ea, te, cd, dte = [sm[:, 16 * i:16 * i + 16] for i in range(8)]
                    xs3 = xs[:, :].rearrange("p (h d) -> p h d", h=16)
                    for hh in range(16):
                        PY = ps[5 + hh // 8]
                        MM(PY[:, (hh % 8) * 64:(hh % 8 + 1) * 64], Mt[:, hh, :], xdt[:, hh * 64:(hh + 1) * 64], True, True, [Mt, xdt], [PY], inc=(hh % 8 == 7))
                    for g in range(4):
                        PF = ps[3 + g // 2]
                        MM(PF[:, (g % 2) * 256:(g % 2 + 1) * 256], cTt[:, g, :], stb[:, g * 256:(g + 1) * 256], True, True, [cTt, stb], [PF], inc=(g % 2 == 1))
                    pyoff = psall[:, 1536:2560].rearrange("p (h d) -> p h d", h=16)
                    pydia = psall[:, 2560:3584]
                    TT('dve', t1[:, :].rearrange("p (h d) -> p h d", h=16), pyoff, ea.unsqueeze(2).to_broadcast([128, 16, 64]), ALU.mult, [ps[3], ps[4], sm], [t1])
                    for g in range(4):
                        PSt = ps[3 + g // 2]
                        MM(PSt[:, (g % 2) * 256:(g % 2 + 1) * 256], btm[:, g * 128:(g + 1) * 128], xdte[:, g * 256:(g + 1) * 256], True, True, [btm, xdte], [PSt], inc=(g % 2 == 1))
                    TT('pool', t2[:, :].rearrange("p (h d) -> p h d", h=16), xs3, dsk[:, :].unsqueeze(2).to_broadcast([128, 16, 64]), ALU.mult, [xs, dsk], [t2])
                    TT('dve', t1[:, :], t1[:, :], pydia, ALU.add, [t1, ps[5], ps[6]], [t1])
                    TT('pool', t1[:, :], t1[:, :], t2[:, :], ALU.add, [t1, t2], [t1])
                    TT('pool', t1[:, :], t1[:, :], zs[:, :], ALU.mult, [t1, zs], [t1])
                    MEMSET('dve', sm[:, 128:129], 0.0, [sm])
                    ACT(junk[:, :], t1[:, :], AF.Square, [t1], [junk, sm], accum=sm[:, 128:129])
                    TS('dve', sm[:, 129:130], sm[:, 128:129], 1.0 / D, EPS, ALU.mult, ALU.add, [sm], [sm])
                    ACT(sm[:, 129:130], sm[:, 129:130], AF.Ln, [sm], [sm])
                    ACT(sm[:, 129:130], sm[:, 129:130], AF.Exp, [sm], [sm], scale=-0.5)
                    ACT(gn[:, :], t1[:, :], AF.Copy, [t1, sm], [gn], scale=sm[:, 129:130])
                    PT = ps[7]
                    for cc in range(8):
                        TR(psbf(7)[:, cc * 128:(cc + 1) * 128], gn[:, cc * 128:(cc + 1) * 128], [gn], [PT], inc=(cc == 7))
                    CP('act', gT[:, :, :].rearrange("p c s -> p (c s)"), psbf(7)[:, :], [PT], [gT])
                    ST('sp', gnT_d.rearrange("c p s -> p c s")[:, :, tok], gT[:, :, :], [gT])
                    TT('pool', st32[:, :].rearrange("p (h d) -> p h d", h=16), st32[:, :].rearrange("p (h d) -> p h d", h=16),
                       cd.unsqueeze(2).to_broadcast([128, 16, 64]), ALU.mult, [st32, sm], [st32])
                    TT('dve', st32[:, :], st32[:, :], psall[:, 1536:2560], ALU.add, [st32, ps[3], ps[4], stb], [st32])
                    CP('act', stb[:, :], st32[:, :], [st32], [stb])


            loads(0)
            if NT > 1:
                loads(1)
            for step in range(NT + 1):
                if step < NT:
                    SA(step)
                if step - 1 >= 0:
                    SB(step - 1)
                if step + 2 < NT:
                    loads(step + 2)

    def out_phase(l):
        s = l % 2
        with fw.phase("out"):
            wso = fw.sb([128, 8, D], BF16, "wso")
            wao = fw.sb([128, 4, D], BF16, "wao")
            wo = fw.sb([128, 8, D], BF16, "wo")
            nw = fw.sb([128, 8], F32, "nw")
            LD('sp', wso[:, :, :], wb_so[s].rearrange("(kc p) n -> p kc n", p=128), [wso], R=[wtok[l]])
            LD('sp', wao[:, :, :], wb_ao[s].rearrange("(kc p) n -> p kc n", p=128), [wao])
            LD('sp', wo[:, :, :], wb_o[s].rearrange("(kc p) n -> p kc n", p=128), [wo])
            LD('sp', nw[:, :], ssd_norm_wT[l], [nw])
            for kc in range(8):
                TS('dve' if kc % 2 else 'pool', wso[:, kc, :], wso[:, kc, :], nw[:, kc:kc + 1], None, ALU.mult, None, [wso, nw], [wso])
            gr = fw.ring(2, [128, 8, 512], BF16, "gTi")
            orr = fw.ring(2, [128, 4, 512], BF16, "oTi")
            sr = fw.ring(2, [128, 16, 512], BF16, "sgi")
            mr = fw.ring(2, [128, 8, 512], BF16, "mT")
            t1r = fw.ring(2, [128, 512], F32, "ot1")
            t2r = fw.ring(2, [128, 512], F32, "ot2")
            yr = fw.ring(2, [128, D], F32, "yo")

            def loads(nb):
                cols = slice(nb * 512, (nb + 1) * 512)
                LD('sp', gr[nb % 2][:, :, :], gnT_d.rearrange("c p s -> p c s")[:, :, cols], [gr[nb % 2]])
                LD('sp', orr[nb % 2][:, :, :], oT_d.rearrange("c p s -> p c s")[:, :, cols], [orr[nb % 2]])
                LD('sp', sr[nb % 2][:, :, :], sgT_d.rearrange("c p s -> p c s")[:, :, cols], [sr[nb % 2]])
            loads(0)
            it = 0
            for nb in range(NB):
                if nb + 1 < NB:
                    loads(nb + 1)
                gT, oT, sg, mT = gr[nb % 2], orr[nb % 2], sr[nb % 2], mr[nb % 2]
                for m in range(8):
                    P1, P2 = ps[(it % 2) * 2], ps[(it % 2) * 2 + 1]
                    t1, t2 = t1r[it % 2], t2r[it % 2]
                    it += 1
                    for kc in range(8):
                        MM(P1[:, :], wso[:, kc, m * 128:(m + 1) * 128], gT[:, kc, :], kc == 0, kc == 7, [wso, gT], [P1], inc=(kc == 7))
                    for kc in range(4):
                        MM(P2[:, :], wao[:, kc, m * 128:(m + 1) * 128], oT[:, kc, :], kc == 0, kc == 3, [wao, oT], [P2], inc=(kc == 3))
                    TT('dve', t1[:, :], P1[:, :], sg[:, m, :], ALU.mult, [P1, sg], [t1])
                    TT('dve', t2[:, :], P2[:, :], sg[:, 8 + m, :], ALU.mult, [P2, sg], [t2])
                    TT('pool', mT[:, m, :], t1[:, :], t2[:, :], ALU.add, [t1, t2], [mT])
                for j in range(4):
                    yo = yr[j % 2]
                    for hf in range(2):
                        P3 = ps[4 + (2 * j + hf) % 4]
                        for kc in range(8):
                            MM(P3[:, :], mT[:, kc, j * 128:(j + 1) * 128], wo[:, kc, hf * 512:(hf + 1) * 512], kc == 0, kc == 7, [mT, wo], [P3], inc=(kc == 7))
                        CP('act', yo[:, hf * 512:(hf + 1) * 512], P3[:, :], [P3], [yo])
                    ST('sp', y_d[(nb * 4 + j) * 128:(nb * 4 + j + 1) * 128, :], yo[:, :], [yo])

    def moe_phase(l):
        s = l % 2
        NH = 2 if S >= 2048 else 1
        SH = S // NH
        NTH = SH // 128
        NBH = SH // 512
        for half in range(NH):
            with fw.phase("moe"):
                hT = fw.sb([128, 8, SH], BF16, "h2T")
                fw.dma('sp', lambda e: [e.dma_start(out=hT[:, c, :], in_=hT_d[c, :, half * SH:(half + 1) * SH]) for c in range(8)], writes=[hT])
                wr = fw.sb([128, 8, 20], BF16, "wr")
                LD('sp', wr[:, :, :], wb_r[s].rearrange("(kc p) n -> p kc n", p=128), [wr], R=[wtok[l]])
                brt = fw.sb([128, 20], F32, "brt")
                LD('sp', brt[:, :], b_router[l].partition_broadcast(128), [brt])
                lg = fw.sb([128, NTH, 20], F32, "lg")
                for t in range(NTH):
                    P = ps[t % 2]
                    for kc in range(8):
                        MM(P[:, 0:20], hT[:, kc, t * 128:(t + 1) * 128], wr[:, kc, :], kc == 0, kc == 7, [hT, wr], [P], inc=(kc == 7))
                    TT('dve', lg[:, t, :], P[:, 0:20], brt[:, :], ALU.add, [P, brt], [lg])
                gl = lg[:, :, 0:4]
                el = lg[:, :, 4:20].rearrange("p t (g e) -> p t g e", g=4)
                R_ = fw.sb([128, NTH, 64], F32, "rt")
                gm = R_[:, :, 0:1]
                oh = R_[:, :, 4:8]
                ex = R_[:, :, 8:12]
                gs = R_[:, :, 12:13]
                ein = R_[:, :, 16:20]
                m1 = R_[:, :, 20:21]
                k1 = R_[:, :, 24:28]
                e2 = R_[:, :, 28:32]
                m2 = R_[:, :, 32:33]
                k2 = R_[:, :, 36:40]
                w1 = R_[:, :, 40:41]
                w2 = R_[:, :, 41:42]
                wg = R_[:, :, 44:48]
                tmp = R_[:, :, 48:64].rearrange("p t (g e) -> p t g e", g=4)
                RR = [R_, lg]
                fw.op('dve', lambda e: e.tensor_reduce(out=gm, in_=gl, axis=AX.X, op=ALU.max), reads=RR, writes=[R_])
                TT('dve', oh, gl, gm.to_broadcast([128, NTH, 4]), ALU.is_equal, RR, [R_])
                TT('dve', ex, gl, gm.to_broadcast([128, NTH, 4]), ALU.subtract, RR, [R_])
                ACT(ex, ex, AF.Exp, [R_], [R_])
                fw.op('dve', lambda e: e.tensor_reduce(out=gs, in_=ex, axis=AX.X, op=ALU.add), reads=RR, writes=[R_])
                fw.op('dve', lambda e: e.reciprocal(out=gs, in_=gs), reads=[R_], writes=[R_])
                TT('dve', tmp, el, oh.unsqueeze(3).to_broadcast([128, NTH, 4, 4]), ALU.mult, RR, [R_])
                fw.op('dve', lambda e: e.tensor_reduce(out=ein, in_=R_[:, :, 48:64].rearrange("p t (g e) -> p t e g", g=4), axis=AX.X, op=ALU.add), reads=RR, writes=[R_])
                fw.op('dve', lambda e: e.tensor_reduce(out=m1, in_=ein, axis=AX.X, op=ALU.max), reads=RR, writes=[R_])
                TT('dve', k1, ein, m1.to_broadcast([128, NTH, 4]), ALU.is_equal, RR, [R_])
                STT('dve', e2, k1, -1e30, ein, ALU.mult, ALU.add, RR, [R_])
                fw.op('dve', lambda e: e.tensor_reduce(out=m2, in_=e2, axis=AX.X, op=ALU.max), reads=RR, writes=[R_])
                TT('dve', k2, e2, m2.to_broadcast([128, NTH, 4]), ALU.is_equal, RR, [R_])
                TT('dve', w1, m2, m1, ALU.subtract, RR, [R_])
                ACT(w1, w1, AF.Exp, [R_], [R_])
                TS('dve', w1, w1, 1.0, None, ALU.add, None, RR, [R_])
                fw.op('dve', lambda e: e.reciprocal(out=w1, in_=w1), reads=[R_], writes=[R_])
                TS('dve', w2, w1, -1.0, 1.0, ALU.mult, ALU.add, RR, [R_])
                TT('dve', w1, w1, gs, ALU.mult, RR, [R_])
                TT('dve', w2, w2, gs, ALU.mult, RR, [R_])
                TT('dve', k1, k1, w1.to_broadcast([128, NTH, 4]), ALU.mult, RR, [R_])
                TT('dve', k2, k2, w2.to_broadcast([128, NTH, 4]), ALU.mult, RR, [R_])
                TT('dve', wg, k1, k2, ALU.add, RR, [R_])
                gates = fw.sb([128, NTH, 16], F32, "gates")
                TT('dve', gates[:, :, :].rearrange("p t (g e) -> p t g e", g=4), oh.unsqueeze(3).to_broadcast([128, NTH, 4, 4]),
                   wg.unsqueeze(2).to_broadcast([128, NTH, 4, 4]), ALU.mult, RR, [gates])
                acc = fw.sb([128, NTH, D], F32, "acc")
                wgr = fw.ring(2, [128, 8, 512], BF16, "wg")
                wur = fw.ring(2, [128, 8, 512], BF16, "wu")
                wdr = fw.ring(2, [128, 4, D], BF16, "wd")
                sgr = fw.ring(2, [128, 512], F32, "sgl")
                hdr = fw.ring(2, [128, 4, 512], BF16, "hid")

                def ldw(e_):
                    k = e_ % 2
                    LD('sp', wgr[k][:, :, :], wb_g[s, e_].rearrange("(kc p) n -> p kc n", p=128), [wgr[k]])
                    LD('sp', wur[k][:, :, :], wb_u[s, e_].rearrange("(kc p) n -> p kc n", p=128), [wur[k]])
                    LD('sp', wdr[k][:, :, :], wb_d[s, e_].rearrange("(kc p) n -> p kc n", p=128), [wdr[k]])
                ldw(0)
                blocks = [(e_, nb) for e_ in range(16) for nb in range(NBH)]

                def GU(bi, e_, nb):
                    wgt, wut = wgr[e_ % 2], wur[e_ % 2]
                    cols = slice(nb * 512, (nb + 1) * 512)
                    hid = hdr[bi % 2]
                    for c in range(4):
                        it = bi * 4 + c
                        PG, PU = ps[(it % 2) * 2], ps[(it % 2) * 2 + 1]
                        sgl = sgr[it % 2]
                        for kc in range(8):
                            MM(PG[:, :], wgt[:, kc, c * 128:(c + 1) * 128], hT[:, kc, cols], kc == 0, kc == 7, [wgt, hT], [PG], inc=(kc == 7))
                        for kc in range(8):
                            MM(PU[:, :], wut[:, kc, c * 128:(c + 1) * 128], hT[:, kc, cols], kc == 0, kc == 7, [wut, hT], [PU], inc=(kc == 7))
                        ACT(sgl[:, :], PG[:, :], AF.Silu, [PG], [sgl])
                        TT('dve', hid[:, c, :], PU[:, :], sgl[:, :], ALU.mult, [PU, sgl], [hid])

                def DN(bi, e_, nb):
                    wdt = wdr[e_ % 2]
                    hid = hdr[bi % 2]
                    for j in range(4):
                        t = nb * 4 + j
                        for hf in range(2):
                            PD = ps[4 + (2 * j + hf) % 4]
                            for c in range(4):
                                MM(PD[:, :], hid[:, c, j * 128:(j + 1) * 128], wdt[:, c, hf * 512:(hf + 1) * 512], c == 0, c == 3, [hid, wdt], [PD], inc=(c == 3))
                            av = acc[:, t, hf * 512:(hf + 1) * 512]
                            if e_ == 0:
                                TS('dve', av, PD[:, :], gates[:, t, e_:e_ + 1], None, ALU.mult, None, [PD, gates], [acc])
                            else:
                                STT('dve', av, PD[:, :], gates[:, t, e_:e_ + 1], av, ALU.mult, ALU.add, [PD, gates, acc], [acc])

                for bi in range(len(blocks) + 1):
                    if bi < len(blocks):
                        GU(bi, *blocks[bi])
                    if bi - 1 >= 0:
                        DN(bi - 1, *blocks[bi - 1])
                    if bi < len(blocks) and blocks[bi][1] == 0 and blocks[bi][0] + 1 < 16:
                        ldw(blocks[bi][0] + 1)
                for t in range(NTH):
                    ST('sp', y_d[half * SH + t * 128:half * SH + (t + 1) * 128, :], acc[:, t, :], [acc])

    slotA_i = fw.sb([128, NT], I32, "slotA_i")
    slotB_i = fw.sb([128, NT], I32, "slotB_i")
    idxw_i = fw.sb([128, NSLT], I32, "idxw_i")
    wA_t = fw.sb([128, NT], F32, "wA_t")
    wB_t = fw.sb([128, NT], F32, "wB_t")

    def moe_sparse(l):
        s = l % 2
        with fw.phase("moeA"):
            hT = fw.sb([128, 8, S], BF16, "h2T")
            fw.dma('sp', lambda e: [e.dma_start(out=hT[:, c, :], in_=hT_d[c]) for c in range(8)], writes=[hT])
            wr = fw.sb([128, 8, 20], BF16, "wr")
            LD('sp', wr[:, :, :], wb_r[s].rearrange("(kc p) n -> p kc n", p=128), [wr], R=[wtok[l]])
            brt = fw.sb([128, 20], F32, "brt")
            LD('sp', brt[:, :], b_router[l].partition_broadcast(128), [brt])
            lg = fw.sb([128, NT, 20], F32, "lg")
            for t in range(NT):
                P = ps[t % 2]
                for kc in range(8):
                    MM(P[:, 0:20], hT[:, kc, t * 128:(t + 1) * 128], wr[:, kc, :], kc == 0, kc == 7, [hT, wr], [P], inc=(kc == 7))
                TT('dve', lg[:, t, :], P[:, 0:20], brt[:, :], ALU.add, [P, brt], [lg])
            gl = lg[:, :, 0:4]
            el = lg[:, :, 4:20].rearrange("p t (g e) -> p t g e", g=4)
            R_ = fw.sb([128, NT, 64], F32, "rt")
            gm = R_[:, :, 0:1]
            oh = R_[:, :, 4:8]
            ex = R_[:, :, 8:12]
            gs = R_[:, :, 12:13]
            ein = R_[:, :, 16:20]
            m1 = R_[:, :, 20:21]
            k1 = R_[:, :, 24:28]
            e2 = R_[:, :, 28:32]
            m2 = R_[:, :, 32:33]
            k2 = R_[:, :, 36:40]
            w1 = R_[:, :, 40:41]
            w2 = R_[:, :, 41:42]
            tmp = R_[:, :, 48:64].rearrange("p t (g e) -> p t g e", g=4)
            RR = [R_, lg]

            def RED(o, a, op, R, W):
                fw.op('dve', lambda e: e.tensor_reduce(out=o, in_=a, axis=AX.X, op=op), reads=R, writes=W)
            RED(gm, gl, ALU.max, RR, [R_])
            TT('dve', oh, gl, gm.to_broadcast([128, NT, 4]), ALU.is_equal, RR, [R_])
            TT('dve', ex, gl, gm.to_broadcast([128, NT, 4]), ALU.subtract, RR, [R_])
            ACT(ex, ex, AF.Exp, [R_], [R_])
            RED(gs, ex, ALU.add, RR, [R_])
            fw.op('dve', lambda e: e.reciprocal(out=gs, in_=gs), reads=[R_], writes=[R_])
            TT('dve', tmp, el, oh.unsqueeze(3).to_broadcast([128, NT, 4, 4]), ALU.mult, RR, [R_])
            RED(ein, R_[:, :, 48:64].rearrange("p t (g e) -> p t e g", g=4), ALU.add, RR, [R_])
            RED(m1, ein, ALU.max, RR, [R_])
            TT('dve', k1, ein, m1.to_broadcast([128, NT, 4]), ALU.is_equal, RR, [R_])
            STT('dve', e2, k1, -1e30, ein, ALU.mult, ALU.add, RR, [R_])
            RED(m2, e2, ALU.max, RR, [R_])
            TT('dve', k2, e2, m2.to_broadcast([128, NT, 4]), ALU.is_equal, RR, [R_])
            TT('dve', w1, m2, m1, ALU.subtract, RR, [R_])
            ACT(w1, w1, AF.Exp, [R_], [R_])
            TS('dve', w1, w1, 1.0, None, ALU.add, None, RR, [R_])
            fw.op('dve', lambda e: e.reciprocal(out=w1, in_=w1), reads=[R_], writes=[R_])
            TS('dve', w2, w1, -1.0, 1.0, ALU.mult, ALU.add, RR, [R_])
            TT('dve', wA_t[:, :].unsqueeze(2), w1, gs, ALU.mult, RR, [wA_t])
            TT('dve', wB_t[:, :].unsqueeze(2), w2, gs, ALU.mult, RR, [wB_t])
            mA = fw.sb([128, NT, 16], F32, "mA")
            mB = fw.sb([128, NT, 16], F32, "mB")
            Im = fw.sb([128, NT, 16], F32, "Im")
            Wc = fw.sb([128, NT, 16], F32, "Wc")
            Tc = fw.sb([128, NT, 16], F32, "Tc")
            Pf = fw.sb([128, NT, 16], F32, "Pf")
            g4 = lambda b_: b_[:, :, :].rearrange("p t (g e) -> p t g e", g=4)
            TT('dve', g4(mA), oh.unsqueeze(3).to_broadcast([128, NT, 4, 4]), k1.unsqueeze(2).to_broadcast([128, NT, 4, 4]), ALU.mult, RR, [mA])
            TT('dve', g4(mB), oh.unsqueeze(3).to_broadcast([128, NT, 4, 4]), k2.unsqueeze(2).to_broadcast([128, NT, 4, 4]), ALU.mult, RR, [mB])
            TT('dve', Im[:, :, :], mA[:, :, :], mB[:, :, :], ALU.add, [mA, mB], [Im])
            N16 = NT * 16
            I2 = Im[:, :, :].rearrange("p t e -> p (t e)")
            MM(ps[2][:, 0:N16], Uinc, I2, True, True, [cst, Im], [ps[2]])
            MM(ps[3][:, 0:N16], ones, I2, True, True, [cst, Im], [ps[3]])
            CP('dve', Wc[:, :, :].rearrange("p t e -> p (t e)"), ps[2][:, 0:N16], [ps[2]], [Wc])
            CP('dve', Tc[:, :, :].rearrange("p t e -> p (t e)"), ps[3][:, 0:N16], [ps[3]], [Tc])
            MEMSET('dve', Pf[:, 0:1, :], 0.0, [Pf])
            for t in range(1, NT):
                TT('dve', Pf[:, t:t + 1, :], Pf[:, t - 1:t, :], Tc[:, t - 1:t, :], ALU.add, [Pf, Tc], [Pf])
            sm = fw.sb([128, 128], F32, "msm")
            n_, ntl, bt, et, bs = [sm[:, 16 * i:16 * i + 16] for i in range(5)]
            TT('dve', n_, Pf[:, NT - 1, :], Tc[:, NT - 1, :], ALU.add, [Pf, Tc], [sm])
            big = fw.sb([128, 16 * NSLT], F32, "big")
            TT('dve', big[:, :].rearrange("p (e k) -> p e k", e=16), n_.unsqueeze(2).to_broadcast([128, 16, NSLT]),
               kstart.unsqueeze(1).to_broadcast([128, 16, NSLT]), ALU.is_gt, [sm, cst], [big])
            RED(ntl, big[:, :].rearrange("p (e k) -> p e k", e=16), ALU.add, [big], [sm])
            MEMSET('dve', bt[:, 0:1], 0.0, [sm])
            for e_ in range(1, 16):
                TT('dve', bt[:, e_:e_ + 1], bt[:, e_ - 1:e_], ntl[:, e_ - 1:e_], ALU.add, [sm], [sm])
            TT('dve', et, bt, ntl, ALU.add, [sm], [sm])
            TS('dve', bs, bt, 128.0, None, ALU.mult, None, [sm], [sm])
            TT('dve', big[:, :].rearrange("p (k e) -> p k e", e=16), kidx.unsqueeze(2).to_broadcast([128, NSLT, 16]),
               et.unsqueeze(1).to_broadcast([128, NSLT, 16]), ALU.is_ge, [sm, cst, big], [big])
            Ek = fw.sb([128, NSLT], F32, "Ek")
            RED(Ek[:, :], big[:, :].rearrange("p (k e) -> p k e", e=16), ALU.add, [big], [Ek])
            TS('dve', Ek[:, :], Ek[:, :], 15.0, None, ALU.min, None, [Ek], [Ek])
            TS('dve', Ek[:, :], Ek[:, :], 128.0, iota_p, ALU.mult, ALU.add, [Ek, cst], [Ek])
            CP('dve', idxw_i[:, :], Ek[:, :], [Ek], [idxw_i])
            TT('dve', Wc[:, :, :], Wc[:, :, :], Im[:, :, :], ALU.subtract, [Wc, Im], [Wc])
            TT('dve', Wc[:, :, :], Wc[:, :, :], Pf[:, :, :], ALU.add, [Wc, Pf], [Wc])
            TT('dve', Wc[:, :, :], Wc[:, :, :], bs.unsqueeze(1).to_broadcast([128, NT, 16]), ALU.add, [Wc, sm], [Wc])
            sAB = fw.sb([128, 2, NT], F32, "sAB")
            TT('dve', mA[:, :, :], mA[:, :, :], Wc[:, :, :], ALU.mult, [mA, Wc], [mA])
            TT('dve', mB[:, :, :], mB[:, :, :], Wc[:, :, :], ALU.mult, [mB, Wc], [mB])
            RED(sAB[:, 0, :], mA[:, :, :], ALU.add, [mA], [sAB])
            RED(sAB[:, 1, :], mB[:, :, :], ALU.add, [mB], [sAB])
            CP('dve', slotA_i[:, :], sAB[:, 0, :], [sAB], [slotA_i])
            CP('dve', slotB_i[:, :], sAB[:, 1, :], [sAB], [slotB_i])
            hr_ = fw.ring(3, [128, D], BF16, "h2row")
            sct = fw.token("sct")
            for t in range(NT):
                hrow = hr_[t % 3]
                LD('sp', hrow[:, :], h2tm_d[t * 128:(t + 1) * 128, :], [hrow])
                for si_ in (slotA_i, slotB_i):
                    fw.dma('pool', lambda e: e.indirect_dma_start(out=hs_d, out_offset=bass.IndirectOffsetOnAxis(ap=si_[:, t:t + 1], axis=0),
                                                                  in_=hrow[:, :], in_offset=None), reads=[hrow, si_], writes=[sct])
        with fw.phase("moeB"):
            xr_ = fw.ring(3, [128, D], BF16, "hsl")
            xTr = fw.ring(2, [128, 8, 128], BF16, "hsT")
            wgr = fw.ring(3, [128, 4096], BF16, "wg")
            wur = fw.ring(3, [128, 4096], BF16, "wu")
            wdr = fw.ring(3, [128, 4096], BF16, "wd")
            sgr = fw.ring(2, [128, 512], F32, "sgl")
            hdr = fw.ring(2, [128, 4, 128], BF16, "hid")
            yr_ = fw.ring(3, [128, D], BF16, "ysl")

            def LDk(k):
                LD('sp', xr_[k % 3][:, :], hs_d[k * 128:(k + 1) * 128, :], [xr_[k % 3]])
                for ring_, src_ in ((wgr, wb_g), (wur, wb_u), (wdr, wb_d)):
                    wt_ = ring_[k % 3]
                    fw.dma('pool', lambda e: e.indirect_dma_start(out=wt_[:, :], out_offset=None, in_=src_[s],
                                                                  in_offset=bass.IndirectOffsetOnAxis(ap=idxw_i[:, k:k + 1], axis=0)), reads=[idxw_i], writes=[wt_])

            def GU(k):
                xs_, xT = xr_[k % 3], xTr[k % 2]
                wg_ = wgr[k % 3][:, :].rearrange("p (kc n) -> p kc n", kc=8)
                wu_ = wur[k % 3][:, :].rearrange("p (kc n) -> p kc n", kc=8)
                PT = ps[6 + k % 2]
                for c in range(8):
                    TR(psbf(6 + k % 2)[:, c * 128:(c + 1) * 128], xs_[:, :].rearrange("s (p kc) -> s kc p", kc=8)[:, c, :], [xs_], [PT], inc=(c == 7))
                CP('dve', xT[:, :, :].rearrange("p c s -> p (c s)"), psbf(6 + k % 2)[:, :], [PT], [xT])
                PG, PU = ps[(k % 2) * 2], ps[(k % 2) * 2 + 1]
                sgl, hid = sgr[k % 2], hdr[k % 2]
                for c in range(4):
                    for kc in range(8):
                        MM(PG[:, c * 128:(c + 1) * 128], wg_[:, kc, :].rearrange("p (m c) -> p c m", c=4)[:, c, :], xT[:, kc, :], kc == 0, kc == 7, [wgr[k % 3], xT], [PG], inc=(kc == 7 and c == 3))
                for c in range(4):
                    for kc in range(8):
                        MM(PU[:, c * 128:(c + 1) * 128], wu_[:, kc, :].rearrange("p (m c) -> p c m", c=4)[:, c, :], xT[:, kc, :], kc == 0, kc == 7, [wur[k % 3], xT], [PU], inc=(kc == 7 and c == 3))
                ACT(sgl[:, :], PG[:, :], AF.Silu, [PG], [sgl])
                TT('dve', hid[:, :, :].rearrange("p c s -> p (c s)"), PU[:, :], sgl[:, :], ALU.mult, [PU, sgl], [hid])

            def DN(k):
                hid, yo = hdr[k % 2], yr_[k % 3]
                wd_ = wdr[k % 3][:, :].rearrange("p (c n) -> p c n", c=4)
                for hf in range(2):
                    PD = ps[4 + hf]
                    for c in range(4):
                        MM(PD[:, :], hid[:, c, :], wd_[:, c, hf * 512:(hf + 1) * 512], c == 0, c == 3, [hid, wdr[k % 3]], [PD], inc=(c == 3))
                    CP('act', yo[:, hf * 512:(hf + 1) * 512], PD[:, :], [PD], [yo])
                ST('sp', ys_d[k * 128:(k + 1) * 128, :], yo[:, :], [yo])

            LDk(0)
            LDk(1)
            for k in range(NSLT + 1):
                if k < NSLT:
                    GU(k)
                if k - 1 >= 0:
                    DN(k - 1)
                if k + 1 < NSLT and k >= 1:
                    LDk(k + 1)
        with fw.phase("moeC"):
            ar = fw.ring(3, [128, D], BF16, "ya")
            brr = fw.ring(3, [128, D], BF16, "yb")
            accr = fw.ring(2, [128, D], F32, "yacc")
            yor = fw.ring(2, [128, D], F32, "yout")

            def G(t):
                for ring_, si_ in ((ar, slotA_i), (brr, slotB_i)):
                    dst_ = ring_[t % 3]
                    fw.dma('pool', lambda e: e.indirect_dma_start(out=dst_[:, :], out_offset=None, in_=ys_d,
                                                                  in_offset=bass.IndirectOffsetOnAxis(ap=si_[:, t:t + 1], axis=0)), reads=[si_], writes=[dst_])
            G(0)
            if NT > 1:
                G(1)
            for t in range(NT):
                ya, yb, acc, yo = ar[t % 3], brr[t % 3], accr[t % 2], yor[t % 2]
                TS('dve', acc[:, :], ya[:, :], wA_t[:, t:t + 1], None, ALU.mult, None, [ya, wA_t], [acc])
                STT('dve', yo[:, :], yb[:, :], wB_t[:, t:t + 1], acc[:, :], ALU.mult, ALU.add, [yb, wB_t, acc], [yo])
                ST('sp', y_d[t * 128:(t + 1) * 128, :], yo[:, :], [yo])
                if t + 2 < NT:
                    G(t + 2)

    stages = []
    for l in range(L):
        if l + 1 < L:
            cast_weights(l + 1)
        xs_src = x_in if l == 0 else xres_d
        if l == 0:
            ln_phase(l, x_in, None, None, None, None, None, 1, 0)
        win_phase(l)
        if stop_after == 'win%d' % l:
            break
        attprep_phase(l)
        att_phase(l)
        if stop_after == 'att%d' % l:
            break
        ssd_phase(l)
        if stop_after == 'ssd%d' % l:
            break
        out_phase(l)
        if stop_after == 'out%d' % l:
            break
        ln_phase(l, xs_src, y_d, xres_d, 2, ln1_g[l], ln1_b[l], 4, 3, want_tm=SPARSE)
        if stop_after == 'ln1%d' % l:
            break
        if SPARSE:
            moe_sparse(l)
        else:
            moe_phase(l)
        if stop_after == 'moe%d' % l:
            break
        last = (l == L - 1)
        ln_phase(l, xres_d, y_d, out if last else xres_d, 5, ln2_g[l], ln2_b[l], None if last else 1, None if last else 0, lp=l + 1)
    fw.barrier()
    return nc, fw


def host_inputs(inputs, S=4096, L=4):
    f = lambda a: np.ascontiguousarray(np.asarray(a, dtype=np.float32))
    w_in = f(inputs['w_in'])[:L]
    ident = np.eye(128, dtype=np.float32)
    U = np.triu(np.ones((128, 128), np.float32))
    Ls = np.tril(np.ones((128, 128), np.float32), -1)
    kk = np.tile(np.arange(80, dtype=np.float32)[None, :], (128, 1))
    pad = np.zeros((128, 96), np.float32)
    pad[:, 0] = np.arange(128)
    consts = np.concatenate([ident, U, Ls, np.ones((128, 128), np.float32), kk * 128.0, kk, pad], axis=1)
    shared = {
        'consts': f(consts),
        'w_mod': f(inputs['w_mod'])[:L], 'b_mod': f(inputs['b_mod'])[:L],
        'w_in': w_in,
        'w_small': f(np.concatenate([w_in[:, :, ODT:ODT + 16], w_in[:, :, OF:OF + 8]], axis=2)),
        'conv_wT': f(np.asarray(inputs['conv_w'])[:L].reshape(L, 4, 16, 128).transpose(0, 3, 2, 1)),
        'conv_bT': f(np.asarray(inputs['conv_b'])[:L].reshape(L, 16, 128).transpose(0, 2, 1)),
        'dt_bias': f(inputs['dt_bias'])[:L], 'a_log': f(inputs['a_log'])[:L], 'd_skip': f(inputs['d_skip'])[:L],
        'ssd_norm_wT': f(np.asarray(inputs['ssd_norm_w'])[:L].reshape(L, 8, 128).transpose(0, 2, 1)),
        'forget_b': f(inputs['forget_b'])[:L],
        'w_ssd_o': f(inputs['w_ssd_o'])[:L], 'w_att_o': f(inputs['w_att_o'])[:L], 'w_o': f(inputs['w_o'])[:L],
        'ln1_g': f(inputs['ln1_g'])[:L], 'ln1_b': f(inputs['ln1_b'])[:L],
        'w_router': f(np.concatenate([np.asarray(inputs['w_router_group'])[:L], np.asarray(inputs['w_router_expert'])[:L]], axis=2)),
        'b_router': f(np.concatenate([np.asarray(inputs['b_router_group'])[:L], np.asarray(inputs['b_router_expert'])[:L]], axis=1)),
        'w_gate': f(inputs['w_gate'])[:L], 'w_up': f(inputs['w_up'])[:L], 'w_down': f(inputs['w_down'])[:L],
        'ln2_g': f(inputs['ln2_g'])[:L], 'ln2_b': f(inputs['ln2_b'])[:L],
    }
    x = np.asarray(inputs['x'], dtype=np.float32)
    c = np.asarray(inputs['c'], dtype=np.float32)
    maps = []
    for b in range(x.shape[0]):
        m = dict(shared)
        m['x'] = np.ascontiguousarray(x[b, :S])
        m['cT'] = np.ascontiguousarray(c[b].reshape(8, 128).T)
        maps.append(m)
    return maps


_PROG = {}


def kernel(**inputs):
    S, L = 4096, 4
    if 'p' not in _PROG:
        _PROG['p'] = build_program(S, L)[0]
    nc = _PROG['p']
    maps = host_inputs(inputs, S, L)
    res = run_bass_kernel_spmd(nc, maps, core_ids=list(range(8)))
    return np.stack([np.asarray(r['out'], dtype=np.float32) for r in res.results], axis=0)
```
